# Optimizing a Trainium2 kernel written in Bass

```python
import math
import jax, jax.numpy as jnp
from jax import lax
import numpy as np

D_MODEL = 1024
BATCH = 8
SEQ = 8192
DEPTH = 1

GRID_W = 64
WIN_H = 8
WIN_W = 16
HEAD_DIM = 64
ATTN_WIDTH = D_MODEL // 2
ATTN_HEADS = ATTN_WIDTH // HEAD_DIM
S5_WIDTH = D_MODEL - ATTN_WIDTH
S5_GROUP_CH = 16
S5_GROUPS = S5_WIDTH // S5_GROUP_CH
S5_STATE = 64
DT_MIN = 1e-3
DT_MAX = 1e-1
MIX_WIDTH = ATTN_WIDTH + S5_WIDTH
IN_WIDTH = 3 * ATTN_WIDTH + S5_WIDTH
N_EXPERT_GROUPS = 8
EXPERTS_PER_GROUP = 8
N_EXPERTS = N_EXPERT_GROUPS * EXPERTS_PER_GROUP
TOP_K_IN_GROUP = 2
D_EXPERT = D_MODEL // 2
MOE_BLOCK = 128
LN_EPS = 1e-5

kernel_name = "hybrid_natten_s5_hiermoe_deepnorm"


def layer_norm(x, g, b):
    x32 = x.astype(jnp.float32)
    mu = jnp.mean(x32, axis=-1, keepdims=True)
    var = jnp.mean(jnp.square(x32 - mu), axis=-1, keepdims=True)
    y = (x32 - mu) * lax.rsqrt(var + LN_EPS) * g.astype(jnp.float32) + b.astype(jnp.float32)
    return y.astype(x.dtype)


def neighbourhood_attention(q, k, v, rpb):
    bsz, seq, heads, dh = q.shape
    rows = seq // GRID_W
    kh = min(WIN_H, rows)
    qg = q.reshape(bsz, rows, GRID_W, heads, dh) * (dh ** -0.5)
    kg = k.reshape(bsz, rows, GRID_W, heads, dh)
    vg = v.reshape(bsz, rows, GRID_W, heads, dh)
    col = np.arange(GRID_W)
    cstart = np.clip(col - WIN_W // 2, 0, GRID_W - WIN_W)
    cidx = cstart[:, None] + np.arange(WIN_W)[None, :]
    dcol = cidx - col[:, None] + (WIN_W - 1)
    rpb_c = rpb.astype(jnp.float32)[:, :, dcol]

    def one_row(r):
        rs = jnp.clip(r - kh // 2, 0, rows - kh)
        q_r = lax.dynamic_index_in_dim(qg, r, axis=1, keepdims=False)
        k_band = lax.dynamic_slice_in_dim(kg, rs, kh, axis=1)
        v_band = lax.dynamic_slice_in_dim(vg, rs, kh, axis=1)
        k_win = k_band[:, :, cidx]
        v_win = v_band[:, :, cidx]
        s = jnp.einsum('bchd,bicjhd->bhcij', q_r, k_win).astype(jnp.float32)
        drow = rs + jnp.arange(kh) - r + (WIN_H - 1)
        bias = jnp.take(rpb_c, drow, axis=1)
        s = s + jnp.transpose(bias, (0, 2, 1, 3))[None]
        p = jax.nn.softmax(s.reshape(bsz, heads, GRID_W, kh * WIN_W), axis=-1)
        p = p.reshape(bsz, heads, GRID_W, kh, WIN_W).astype(v.dtype)
        return jnp.einsum('bhcij,bicjhd->bchd', p, v_win)

    out = lax.map(one_row, jnp.arange(rows))
    return jnp.transpose(out, (1, 0, 2, 3, 4)).reshape(bsz, seq, heads * dh)


def _ssm_combine(e1, e2):
    a1r, a1i, b1r, b1i = e1
    a2r, a2i, b2r, b2i = e2
    ar = a2r * a1r - a2i * a1i
    ai = a2r * a1i + a2i * a1r
    br = a2r * b1r - a2i * b1i + b2r
    bi = a2r * b1i + a2i * b1r + b2i
    return (ar, ai, br, bi)


def s5_bidirectional(u, a_re, a_im, log_dt, b_re, b_im, c_re, c_im, d_skip):
    f32 = jnp.float32
    u32 = u.astype(f32)
    seq = u.shape[1]
    y = d_skip.astype(f32)[None, None] * u32
    for direction in range(2):
        ar = a_re[direction].astype(f32)
        ai = a_im[direction].astype(f32)
        dt = jnp.exp(log_dt[direction].astype(f32))[:, None]
        mag = jnp.exp(ar * dt)
        lr = mag * jnp.cos(ai * dt)
        li = mag * jnp.sin(ai * dt)
        den = ar * ar + ai * ai
        zr = ((lr - 1.0) * ar + li * ai) / den
        zi = (li * ar - (lr - 1.0) * ai) / den
        br = b_re[direction].astype(f32)
        bi = b_im[direction].astype(f32)
        bbr = zr[..., None] * br - zi[..., None] * bi
        bbi = zr[..., None] * bi + zi[..., None] * br
        xr = jnp.einsum('bsgc,gpc->bsgp', u32, bbr)
        xi = jnp.einsum('bsgc,gpc->bsgp', u32, bbi)
        shp = (1, seq) + lr.shape
        lam_r = jnp.broadcast_to(lr[None, None], shp)
        lam_i = jnp.broadcast_to(li[None, None], shp)
        _, _, hr, hi = lax.associative_scan(
            _ssm_combine, (lam_r, lam_i, xr, xi), reverse=(direction == 1), axis=1)
        y = y + jnp.einsum('bsgp,gcp->bsgc', hr, c_re[direction].astype(f32)) \
              - jnp.einsum('bsgp,gcp->bsgc', hi, c_im[direction].astype(f32))
    return y


def hybrid_mixer(h, w_in, rpb, s5_a_re, s5_a_im, s5_log_dt, s5_b_re, s5_b_im,
                 s5_c_re, s5_c_im, s5_d, w_glu, b_glu, w_out):
    bsz, seq, _ = h.shape
    proj = h @ w_in
    q = proj[..., :ATTN_WIDTH].reshape(bsz, seq, ATTN_HEADS, HEAD_DIM)
    k = proj[..., ATTN_WIDTH:2 * ATTN_WIDTH].reshape(bsz, seq, ATTN_HEADS, HEAD_DIM)
    v = proj[..., 2 * ATTN_WIDTH:3 * ATTN_WIDTH].reshape(bsz, seq, ATTN_HEADS, HEAD_DIM)
    u = proj[..., 3 * ATTN_WIDTH:].reshape(bsz, seq, S5_GROUPS, S5_GROUP_CH)
    attn = neighbourhood_attention(q, k, v, rpb)
    ssm = s5_bidirectional(u, s5_a_re, s5_a_im, s5_log_dt, s5_b_re, s5_b_im,
                           s5_c_re, s5_c_im, s5_d).reshape(bsz, seq, S5_WIDTH)
    ssm = jax.nn.gelu(ssm).astype(h.dtype)
    ssm = ssm * jax.nn.sigmoid(ssm @ w_glu + b_glu)
    return jnp.concatenate([attn, ssm], axis=-1) @ w_out


def hierarchical_moe(h, w_rg, b_rg, w_re, b_re, w_gate, w_up, w_down):
    bsz, seq, dm = h.shape
    n_tok = bsz * seq
    t = h.reshape(n_tok, dm)
    g_prob = jax.nn.softmax((t @ w_rg).astype(jnp.float32) + b_rg.astype(jnp.float32), axis=-1)
    g_val, g_idx = lax.top_k(g_prob, 1)
    e_logits = ((t @ w_re).astype(jnp.float32) + b_re.astype(jnp.float32))
    e_logits = e_logits.reshape(n_tok, N_EXPERT_GROUPS, EXPERTS_PER_GROUP)
    e_in = jnp.take_along_axis(e_logits, g_idx[:, :, None], axis=1)[:, 0]
    e_top, e_loc = lax.top_k(e_in, TOP_K_IN_GROUP)
    gate = g_val * jax.nn.softmax(e_top, axis=-1)
    expert = g_idx * EXPERTS_PER_GROUP + e_loc

    n_assign = n_tok * TOP_K_IN_GROUP
    flat_e = expert.reshape(-1).astype(jnp.int32)
    flat_tok = jnp.repeat(jnp.arange(n_tok, dtype=jnp.int32), TOP_K_IN_GROUP)
    flat_w = gate.reshape(-1)
    order = jnp.argsort(flat_e)
    se, stok, sw = flat_e[order], flat_tok[order], flat_w[order]
    counts = jnp.bincount(flat_e, length=N_EXPERTS)
    starts = jnp.cumsum(counts) - counts
    padded = (counts + MOE_BLOCK - 1) // MOE_BLOCK * MOE_BLOCK
    pends = jnp.cumsum(padded)
    pstarts = pends - padded
    dest = pstarts[se] + (jnp.arange(n_assign, dtype=jnp.int32) - starts[se])
    n_blocks = (n_assign + N_EXPERTS * (MOE_BLOCK - 1) + MOE_BLOCK - 1) // MOE_BLOCK
    buf = jnp.zeros((n_blocks * MOE_BLOCK, dm), t.dtype).at[dest].set(t[stok])
    block_expert = jnp.minimum(
        jnp.searchsorted(pends, jnp.arange(n_blocks) * MOE_BLOCK, side='right'), N_EXPERTS - 1)

    def expert_block(args):
        xb, e = args
        hid = jax.nn.silu(xb @ w_gate[e]) * (xb @ w_up[e])
        return hid @ w_down[e]

    out = lax.map(expert_block, (buf.reshape(n_blocks, MOE_BLOCK, dm), block_expert))
    out = out.reshape(n_blocks * MOE_BLOCK, dm)
    contrib = out[dest].astype(jnp.float32) * sw[:, None]
    y = jnp.zeros((n_tok, dm), jnp.float32).at[stok].add(contrib)
    return y.astype(h.dtype).reshape(bsz, seq, dm)


def setup_inputs(seed: int = 0) -> dict:
    key = jax.random.key(seed)
    ks = jax.random.split(key, 24)
    beta = (8.0 * DEPTH) ** -0.25
    L, G, P, C = DEPTH, S5_GROUPS, S5_STATE, S5_GROUP_CH
    nrm = lambda k, shape: jax.random.normal(k, shape, jnp.float32)
    x = nrm(ks[0], (BATCH, SEQ, D_MODEL))
    w_in = nrm(ks[1], (L, D_MODEL, IN_WIDTH)) * D_MODEL ** -0.5
    w_in = w_in.at[:, :, 2 * ATTN_WIDTH:3 * ATTN_WIDTH].multiply(beta)
    rpb = 0.02 * nrm(ks[2], (L, ATTN_HEADS, 2 * WIN_H - 1, 2 * WIN_W - 1))
    s5_a_re = -0.5 + 0.01 * nrm(ks[3], (L, 2, G, P))
    s5_a_im = math.pi * jnp.arange(P, dtype=jnp.float32) + 0.01 * nrm(ks[4], (L, 2, G, P))
    s5_log_dt = math.log(DT_MIN) + jax.random.uniform(ks[5], (L, 2, G), jnp.float32) * (
        math.log(DT_MAX) - math.log(DT_MIN))
    s5_b_re = nrm(ks[6], (L, 2, G, P, C)) * (2.0 * C) ** -0.5
    s5_b_im = nrm(ks[7], (L, 2, G, P, C)) * (2.0 * C) ** -0.5
    s5_c_re = nrm(ks[8], (L, 2, G, C, P)) * P ** -0.5
    s5_c_im = nrm(ks[9], (L, 2, G, C, P)) * P ** -0.5
    s5_d = nrm(ks[10], (L, G, C))
    w_glu = nrm(ks[11], (L, S5_WIDTH, S5_WIDTH)) * S5_WIDTH ** -0.5
    b_glu = 0.01 * nrm(ks[12], (L, S5_WIDTH))
    w_out = nrm(ks[13], (L, MIX_WIDTH, D_MODEL)) * MIX_WIDTH ** -0.5 * beta
    ln1_g = 1.0 + 0.01 * nrm(ks[14], (L, D_MODEL))
    ln1_b = 0.01 * nrm(ks[15], (L, D_MODEL))
    w_router_group = nrm(ks[16], (L, D_MODEL, N_EXPERT_GROUPS)) * D_MODEL ** -0.5
    b_router_group = 0.01 * nrm(ks[17], (L, N_EXPERT_GROUPS))
    w_router_expert = nrm(ks[18], (L, D_MODEL, N_EXPERTS)) * D_MODEL ** -0.5
    b_router_expert = 0.01 * nrm(ks[19], (L, N_EXPERTS))
    w_gate = nrm(ks[20], (L, N_EXPERTS, D_MODEL, D_EXPERT)) * D_MODEL ** -0.5
    w_up = nrm(ks[21], (L, N_EXPERTS, D_MODEL, D_EXPERT)) * D_MODEL ** -0.5
    w_down = nrm(ks[22], (L, N_EXPERTS, D_EXPERT, D_MODEL)) * D_EXPERT ** -0.5 * beta
    ks2 = jax.random.split(ks[23], 2)
    ln2_g = 1.0 + 0.01 * nrm(ks2[0], (L, D_MODEL))
    ln2_b = 0.01 * nrm(ks2[1], (L, D_MODEL))
    return {"x": x, "w_in": w_in, "rpb": rpb, "s5_a_re": s5_a_re, "s5_a_im": s5_a_im,
            "s5_log_dt": s5_log_dt, "s5_b_re": s5_b_re, "s5_b_im": s5_b_im,
            "s5_c_re": s5_c_re, "s5_c_im": s5_c_im, "s5_d": s5_d, "w_glu": w_glu,
            "b_glu": b_glu, "w_out": w_out, "ln1_g": ln1_g, "ln1_b": ln1_b,
            "w_router_group": w_router_group, "b_router_group": b_router_group,
            "w_router_expert": w_router_expert, "b_router_expert": b_router_expert,
            "w_gate": w_gate, "w_up": w_up, "w_down": w_down,
            "ln2_g": ln2_g, "ln2_b": ln2_b}


def reference(x, w_in, rpb, s5_a_re, s5_a_im, s5_log_dt, s5_b_re, s5_b_im, s5_c_re,
              s5_c_im, s5_d, w_glu, b_glu, w_out, ln1_g, ln1_b, w_router_group,
              b_router_group, w_router_expert, b_router_expert, w_gate, w_up, w_down,
              ln2_g, ln2_b):
    alpha = (2.0 * DEPTH) ** 0.25
    h = x
    for layer in range(DEPTH):
        mix = hybrid_mixer(h, w_in[layer], rpb[layer], s5_a_re[layer], s5_a_im[layer],
                           s5_log_dt[layer], s5_b_re[layer], s5_b_im[layer], s5_c_re[layer],
                           s5_c_im[layer], s5_d[layer], w_glu[layer], b_glu[layer], w_out[layer])
        h = layer_norm(alpha * h + mix, ln1_g[layer], ln1_b[layer])
        ffn = hierarchical_moe(h, w_router_group[layer], b_router_group[layer],
                               w_router_expert[layer], b_router_expert[layer],
                               w_gate[layer], w_up[layer], w_down[layer])
        h = layer_norm(alpha * h + ffn, ln2_g[layer], ln2_b[layer])
    return h
```

```python
import os
import numpy as np
from contextlib import ExitStack
import concourse.bass as bass
import concourse.mybir as mybir
from concourse.bass_utils import run_bass_kernel_spmd

F32 = mybir.dt.float32
BF16 = mybir.dt.bfloat16
I32 = mybir.dt.int32
ALU = mybir.AluOpType
AF = mybir.ActivationFunctionType

NT = 8192
DM = 1024
NE = 64
CAP = 384
ALPHA = 2.0 ** 0.25
EPS = 1e-5
ENGINES = ("pe", "act", "dve", "pool", "sp")
DMA_POOL = {"sp": 24, "pool": 12, "act": 4}
SAME_ENGINE_SYNC = not os.environ.get("K_NOSES")
RAW_ONLY = not os.environ.get("K_ALLDEPS")
MAGIC = 12582912.0
C1 = 6.28125
C2 = float(2 * np.pi - 6.28125)
NCLS = 21


class Buf:
    __slots__ = ("name", "w", "r")

    def __init__(self, name=""):
        self.name = name
        self.w = None
        self.r = []


class Op:
    __slots__ = ("id", "eng", "fn", "deps", "dma", "needed", "val", "sem", "raw")

    def __init__(self, id, eng, fn, deps, dma):
        self.id, self.eng, self.fn, self.deps, self.dma = id, eng, fn, deps, dma
        self.needed = False
        self.val = None
        self.sem = None


class Sched:
    def __init__(self, nc):
        self.nc = nc
        self.ops = []
        self.q = {e: [] for e in ENGINES}
        self.last_dma = {}
        self.ndma = {}
        self.lane = None

    def op(self, eng, fn, reads=(), writes=(), dma=None, extra=()):
        if self.lane is not None:
            self.lane.append((eng, fn, tuple(reads), tuple(writes), dma, tuple(extra)))
            return None
        return self._op(eng, fn, reads, writes, dma, extra)

    def run_lanes(self, lanes, chunk=1):
        idx = [0] * len(lanes)
        chunks = chunk if isinstance(chunk, (list, tuple)) else [chunk] * len(lanes)
        while any(idx[i] < len(L) for i, L in enumerate(lanes)):
            for i, L in enumerate(lanes):
                for _ in range(chunks[i]):
                    if idx[i] < len(L):
                        self._op(*L[idx[i]])
                        idx[i] += 1

    def _op(self, eng, fn, reads=(), writes=(), dma=None, extra=()):
        deps = set(extra)
        raw = set(extra)
        for b in reads:
            if b.w is not None:
                deps.add(b.w)
                raw.add(b.w)
        for b in writes:
            if b.w is not None:
                deps.add(b.w)
            deps.update(b.r)
        key = None
        if dma is not None:
            n = self.ndma.get(eng, 0)
            self.ndma[eng] = n + 1
            key = ("dma", eng, n % DMA_POOL[eng])
            if key in self.last_dma:
                deps.add(self.last_dma[key])
        o = Op(len(self.ops), eng, fn, deps, key)
        o.raw = raw
        self.ops.append(o)
        self.q[eng].append(o)
        if key is not None:
            self.last_dma[key] = o.id
        for b in writes:
            b.w = o.id
            b.r = []
        for b in reads:
            b.r.append(o.id)
        return o.id

    def barrier(self):
        last = [self.q[e][-1].id for e in ENGINES if self.q[e]] + list(self.last_dma.values())
        for e in ENGINES:
            self.op(e, lambda en: en.nop(), extra=last)

    def emit(self, sems):
        ops = self.ops
        for o in ops:
            nd = set()
            for d in o.deps:
                p = ops[d]
                if p.dma is None and o.dma is None and p.eng == o.eng:
                    if o.eng == "pe" or not SAME_ENGINE_SYNC or (RAW_ONLY and o.eng != "pool" and d not in o.raw):
                        continue
                nd.add(d)
            o.deps = nd
            for d in nd:
                ops[d].needed = True
        cnt = {e: 0 for e in ENGINES}
        dcnt = {}
        for o in ops:
            if o.dma is not None:
                key = o.dma
                dcnt[key] = dcnt.get(key, 0) + 16
                o.sem, o.val = key, dcnt[key]
            elif o.needed:
                cnt[o.eng] += 1
                o.sem, o.val = o.eng, cnt[o.eng]

        def run(engname):
            def body(e):
                waited = {}
                for o in self.q[engname]:
                    need = {}
                    for d in o.deps:
                        p = ops[d]
                        if p.val > need.get(p.sem, 0):
                            need[p.sem] = p.val
                    for k, v in need.items():
                        if waited.get(k, 0) < v:
                            e.wait_ge(sems[k], v)
                            waited[k] = v
                    ins = o.fn(e)
                    if o.sem is not None:
                        ins.then_inc(sems[o.sem], 16 if o.dma is not None else 1)
            return body

        with self.nc.Block() as block:
            block.tensor(run("pe"))
            block.scalar(run("act"))
            block.vector(run("dve"))
            block.gpsimd(run("pool"))
            block.sync(run("sp"))


class Arena:
    def __init__(self, t, nbytes):
        self.t, self.cap, self.off = t, nbytes, 0

    def alloc(self, n, dt):
        bs = 2 if dt == BF16 else 4
        nb = (n * bs + 63) // 64 * 64
        a0 = self.off
        self.off += nb
        assert self.off <= self.cap, ("arena overflow", self.off, self.cap)
        v = self.t[:, a0 // 4:(a0 + nb) // 4]
        if dt == BF16:
            return v.bitcast(BF16)[:, :n]
        if dt == I32:
            return v.bitcast(I32)[:, :n]
        return v[:, :n]


def _rs(r):
    return min(max(r - 4, 0), 120)


def _classes():
    cl = [(10, 8 + i) for i in range(5)]
    for j in (0, 1):
        cl += [(j, jc) for jc in range(4)]
    for j in (62, 63):
        cl += [(j, jc) for jc in range(60, 64)]
    return cl


def build_bias_tab(rpb):
    cstart = np.clip(np.arange(64) - 8, 0, 48)
    tab = np.full((NCLS, 128, 8, 128), -30000.0, np.float32)
    for ci, (j, jc) in enumerate(_classes()):
        for kl in range(2):
            for ql in range(2):
                kr, qr = 2 * jc + kl, 2 * j + ql
                if _rs(qr) <= kr < _rs(qr) + 8:
                    drow = kr - qr + 7
                    for qc in range(64):
                        kcs = np.arange(cstart[qc], cstart[qc] + 16)
                        tab[ci, kl * 64 + kcs, :, ql * 64 + qc] = rpb[:, drow, kcs - qc + 15].T
    return tab.reshape(NCLS, 128, 1024)


def build_consts():
    c = np.zeros((128, 768), np.float32)
    c[:, 0:128] = np.eye(128, dtype=np.float32)
    c[:, 128:256] = np.triu(np.ones((128, 128), np.float32), 1)
    c[:, 256:384] = 1.0
    c[:, 384:448] = (np.arange(64) * CAP)[None, :]
    p = np.arange(128)
    c[:, 448] = ((p // 16) % 2 == 0)
    c[:, 449] = ((p // 16) % 2 == 1)
    c[:, 450] = ((p // 16) % 2 == 0) & (p >= 96)
    c[:, 451] = ((p // 16) % 2 == 1) & (p >= 96)
    c[:, 512:576] = 1.0
    c[:, 704:768] = 1.0
    return c


def build_s5_layouts(a_re, a_im, log_dt, b_re, b_im, c_re, c_im, dsk):
    def LB(a):
        return a.reshape(2, 16, 2, 64).transpose(2, 3, 0, 1).reshape(128, 32)

    def LBb(b):
        return b.reshape(2, 16, 2, 64, 16).transpose(2, 3, 0, 1, 4).reshape(128, 512)

    def LBc(c):
        return c.reshape(2, 16, 2, 16, 64).transpose(2, 4, 0, 1, 3).reshape(128, 512)

    ld3 = np.broadcast_to(log_dt[:, :, None], (2, 32, 64))
    PB = np.concatenate([LB(a_re), LB(a_im), LB(ld3), LBb(b_re), LBb(b_im), LBc(c_re), LBc(c_im)], 1)

    def LA(a):
        t = a.reshape(2, 4, 8, 64).transpose(2, 0, 1, 3)
        return np.broadcast_to(t[:, None], (8, 16, 2, 4, 64)).reshape(128, 512)

    def LAb(b):
        return b.reshape(2, 4, 8, 64, 16).transpose(2, 4, 0, 1, 3).reshape(128, 512)

    DA = dsk.reshape(4, 8, 16).transpose(1, 2, 0).reshape(128, 4)
    PA = np.concatenate([LA(a_re), LA(a_im), LA(ld3), LAb(b_re), LAb(b_im), DA], 1)
    return np.ascontiguousarray(PB, np.float32), np.ascontiguousarray(PA, np.float32)


NPB = 96 + 2048
NPA = 1536 + 1024 + 4


def build(stop=99, tap=None):
    nc = bass.Bass("TRN2", target_bir_lowering=False)

    def din(name, shape, dt=F32):
        return nc.dram_tensor(name, list(shape), dt, kind="ExternalInput").ap()

    def dscr(name, shape, dt):
        return nc.dram_tensor(name, list(shape), dt).ap()

    xT = din("xT", [DM, NT])
    x = din("x", [NT, DM])
    w_in = din("w_in", [DM, 2048])
    w_glu = din("w_glu", [512, 512])
    b_glu = din("b_glu", [128, 4])
    w_out = din("w_out", [DM, DM])
    lnp = din("lnp", [4, DM])
    w_r = din("w_r", [DM, 72])
    b_r = din("b_r", [1, 72])
    w_gate = din("w_gate", [NE, DM, 512])
    w_up = din("w_up", [NE, DM, 512])
    w_down = din("w_down", [NE, 512, DM])
    btab = din("btab", [NCLS, 128, 1024])
    consts = din("consts", [128, 768])
    PBd = din("PB", [128, NPB])
    PAd = din("PA", [128, NPA])
    out = nc.dram_tensor("out", [NT, DM], F32, kind="ExternalOutput").ap()
    uT_d = dscr("uT_d", [512, NT], BF16)
    attT_d = dscr("attT_d", [512, NT], BF16)
    hbuf = dscr("hbuf", [NT, DM], F32)
    xbuf = dscr("xbuf", [NE * CAP, DM], BF16)
    obuf = dscr("obuf", [NE * CAP, DM], BF16)
    tapo = None
    if tap is not None:
        tapo = nc.dram_tensor("tap", list(tap[1]), tap[2], kind="ExternalOutput").ap()

    S = Sched(nc)
    es = ExitStack()
    ARENA_BYTES = 206 * 1024
    arena_t = es.enter_context(nc.sbuf_tensor("arena", [128, ARENA_BYTES // 4], F32))
    A = Arena(arena_t, ARENA_BYTES)
    ps = [es.enter_context(nc.psum_tensor("ps%d" % i, [128, 512], F32)) for i in range(8)]
    PB_ = [Buf("ps%d" % i) for i in range(8)]
    sems = {e: es.enter_context(nc.semaphore("s_" + e)) for e in ENGINES}
    for q_, n_ in DMA_POOL.items():
        for i in range(n_):
            sems[("dma", q_, i)] = es.enter_context(nc.semaphore("d_%s%d" % (q_, i)))

    def MM(o, lhsT, rhs, start, stop, R, W):
        return S.op("pe", lambda e: e.matmul(o, lhsT=lhsT, rhs=rhs, start=start, stop=stop,
                                             skip_group_check=True), R, W)

    def TR(o, in_, ident, R, W):
        return S.op("pe", lambda e: e.transpose(o, in_, ident), R, W)

    def ACTF(o, in_, func, R, W, scale=1.0, bias=0.0):
        return S.op("act", lambda e: e.activation(out=o, in_=in_, func=func, bias=bias, scale=scale), R, W)

    def CP(eng, o, in_, R, W):
        if eng == "act":
            return S.op("act", lambda e: e.copy(out=o, in_=in_), R, W)
        return S.op(eng, lambda e: e.tensor_copy(out=o, in_=in_), R, W)

    def TT(eng, o, in0, in1, op, R, W):
        return S.op(eng, lambda e: e.tensor_tensor(out=o, in0=in0, in1=in1, op=op), R, W)

    def TS(eng, o, in0, s1, op0, R, W, s2=None, op1=None):
        if op1 is None:
            return S.op(eng, lambda e: e.tensor_scalar(out=o, in0=in0, scalar1=s1, scalar2=None, op0=op0), R, W)
        return S.op(eng, lambda e: e.tensor_scalar(out=o, in0=in0, scalar1=s1, scalar2=s2, op0=op0, op1=op1), R, W)

    def STT(eng, o, in0, scalar, in1, op0, op1, R, W):
        return S.op(eng, lambda e: e.scalar_tensor_tensor(out=o, in0=in0, scalar=scalar, in1=in1,
                                                          op0=op0, op1=op1), R, W)

    def MEMSET(eng, o, val, W):
        return S.op(eng, lambda e: e.memset(o, val), (), W)

    def DMA(q, o, in_, R, W, stream):
        return S.op(q, lambda e: e.dma_start(out=o, in_=in_), R, W, dma=stream)

    rr = {"n": 0}

    def alt():
        rr["n"] += 1
        return "act" if rr["n"] % 2 else "dve"

    out_writes = []

    cst = A.alloc(768, F32)
    cstb = A.alloc(640, BF16)
    Bcst, Bcstb = Buf("cst"), Buf("cstb")
    DMA("sp", cst, consts, [], [Bcst], 1)
    CP("act", cstb[:, 0:384], cst[:, 0:384], [Bcst], [Bcstb])
    CP("act", cstb[:, 384:640], cst[:, 512:768], [Bcst], [Bcstb])
    onesz = [cstb[:, 384:512], cstb[:, 512:640]]
    identf, identb = cst[:, 0:128], cstb[:, 0:128]
    ltrib, onesb = cstb[:, 128:256], cstb[:, 256:384]
    eC, maskcol, mask3 = cst[:, 384:448], cst[:, 448:450], cst[:, 450:452]
    gates_all = A.alloc(128, F32).rearrange("p (t k) -> p t k", k=2)
    dest_all = A.alloc(128, I32).rearrange("p (t k) -> p t k", k=2)
    Bgates, Bdest = Buf("gates"), Buf("dest")
    runc = A.alloc(64, F32)
    Brun = Buf("run")
    MEMSET("dve", runc, 0.0, [Brun])
    persist = A.off

    w_in_b = A.alloc(8 * 2048, BF16).rearrange("p (k n) -> p k n", k=8)
    Bwin = Buf("w_in")
    xTb = [A.alloc(8 * 512, BF16).rearrange("p (k n) -> p k n", k=8) for _ in range(2)]
    BxT = [Buf("xT0"), Buf("xT1")]
    NQ, NK = 12, 12
    qz = [A.alloc(4 * NQ * 128, BF16).rearrange("p (f n) -> p f n", f=4) for _ in range(2)]
    kT = A.alloc(4 * NK * 128, BF16).rearrange("p (f n) -> p f n", f=4)
    vz = [A.alloc(NK * 512, BF16).rearrange("p (s i c) -> p s i c", s=NK, i=4) for _ in range(2)]
    Bq = [Buf("q%d" % i) for i in range(NQ)]
    Bk = [Buf("k%d" % i) for i in range(NK)]
    Bv = [Buf("v%d" % i) for i in range(NK)]
    for par in range(2):
        MEMSET("pool", qz[par], 0.0, Bq)
        MEMSET("pool", vz[par], 0.0, Bv)
    maskt = A.alloc(NCLS * 1024, BF16).rearrange("p (c n) -> p c n", c=NCLS)
    Bmask = Buf("mask")
    mst = [A.alloc(1024, F32) for _ in range(2)]
    Bmst = [Buf("mst0"), Buf("mst1")]
    ust = [A.alloc(4 * 512, BF16).rearrange("p (f n) -> p f n", f=4) for _ in range(2)]
    Bust = [Buf("ust0"), Buf("ust1")]
    NEB = 5
    Eb = [A.alloc(512, BF16) for _ in range(NEB)]
    BE = [Buf("E%d" % i) for i in range(NEB)]
    recs = [A.alloc(512, F32) for _ in range(2)]
    Brecs = [Buf(), Buf()]
    attb = [A.alloc(512, BF16) for _ in range(2)]
    Batt = [Buf("att0"), Buf("att1")]
    Bdr_u, Bdr_att = Buf("uT_d"), Buf("attT_d")

    w_in_v = w_in.rearrange("(k p) n -> p k n", p=128)
    for kc in range(8):
        DMA("pool", w_in_b[:, kc, :], w_in_v[:, kc, :], [], [Bwin], 0)
    for c in range(NCLS):
        DMA("sp", mst[c % 2], btab[c], [], [Bmst[c % 2]], 1)
        ACTF(maskt[:, c, :], mst[c % 2], AF.Exp, [Bmst[c % 2]], [Bmask])

    xT_v = xT.rearrange("(k p) t -> p k t", p=128)
    uT_dv = uT_d.rearrange("(f p) t -> p f t", p=128)
    attT_dv = attT_d.rearrange("(f p) t -> p f t", p=128)
    cnt = {"sb": 0, "e": 0, "a": 0}

    def tile_units(j):
        if j <= 1:
            kcs = list(range(4))
            cls = [5 + 4 * j + jc for jc in kcs]
        elif j >= 62:
            kcs = list(range(60, 64))
            cls = [13 + 4 * (j - 62) + (jc - 60) for jc in kcs]
        else:
            kcs = list(range(j - 2, j + 3))
            cls = list(range(5))
        us = []
        for ci, jc in enumerate(kcs):
            for par in range(2):
                us.append({"j": j, "ks": jc % NK, "cls": cls[ci], "par": par,
                           "first": ci == 0 and par == 0, "last": ci == len(kcs) - 1 and par == 1})
        return us

    def st12(u):
        sb = 2 + cnt["sb"] % 4
        cnt["sb"] += 1
        ei = cnt["e"] % NEB
        cnt["e"] += 1
        u["ei"] = ei
        qs, ks, par = u["j"] % NQ, u["ks"], u["par"]
        for i in range(4):
            MM(ps[sb][:, i * 128:(i + 1) * 128], kT[:, i, ks * 128:(ks + 1) * 128],
               qz[par][:, i, qs * 128:(qs + 1) * 128], True, True, [Bk[ks], Bq[qs]], [PB_[sb]])
        ACTF(Eb[ei], ps[sb][:, :], AF.Exp, [PB_[sb]], [BE[ei]], scale=0.125)
        mv = maskt[:, u["cls"], :].rearrange("p (i t q) -> p i t q", i=4, t=2)[:, :, par, :]
        ev = Eb[ei].rearrange("p (i q) -> p i q", i=4)
        TT("dve", ev, ev, mv, ALU.mult, [BE[ei], Bmask], [BE[ei]])

    def st3(u):
        ei, ks, par, j = u["ei"], u["ks"], u["par"], u["j"]
        po, pm = 6, 7
        for i in range(4):
            MM(ps[po][:, i * 128:(i + 1) * 128], vz[par][:, ks, i, :], Eb[ei][:, i * 128:(i + 1) * 128],
               u["first"] and i == 0, u["last"], [Bv[ks], BE[ei]], [PB_[po]])
        MM(ps[pm][:, :], onesz[par], Eb[ei], u["first"], u["last"], [Bcstb, BE[ei]], [PB_[pm]])
        if u["last"]:
            recj = recs[j % 2]
            S.op("dve", lambda e: e.reciprocal(out=recj, in_=ps[pm][:, :]), [PB_[pm]], [Brecs[j % 2]])
            ai = cnt["a"] % 2
            cnt["a"] += 1
            TT("dve", attb[ai], ps[po][:, :], recj, ALU.mult, [PB_[po], Brecs[j % 2]], [Batt[ai]])
            DMA("sp", attT_dv[:, :, j * 128:(j + 1) * 128], attb[ai].rearrange("p (f n) -> p f n", f=4),
                [Batt[ai]], [Bdr_att], 2)

    SKEW = 3

    def attn_tiles(js):
        us = [u for j in js for u in tile_units(j)]
        for t in range(len(us) + SKEW):
            if t < len(us):
                st12(us[t])
            if t >= SKEW:
                st3(us[t - SKEW])

    pcnt = {"n": 0}

    def inproj(b):
        xb, Bx = xTb[b % 2], BxT[b % 2]
        DMA("pool", xb, xT_v[:, :, b * 512:(b + 1) * 512], [], [Bx], 0)
        for fc in range(12):
            col0 = fc * 128 if fc < 8 else 1536 + (fc - 8) * 128
            bank = pcnt["n"] % 2
            pcnt["n"] += 1
            for kc in range(8):
                MM(ps[bank][:, :], w_in_b[:, kc, col0:col0 + 128], xb[:, kc, :], kc == 0, kc == 7,
                   [Bwin, Bx], [PB_[bank]])
            if fc < 4:
                s0 = (4 * b) % NQ
                W = [Bq[s0 + i] for i in range(4)]
                CP("act", qz[0][0:64, fc, s0 * 128:s0 * 128 + 512], ps[bank][0:64, :], [PB_[bank]], W)
                CP("dve", qz[1][64:128, fc, s0 * 128:s0 * 128 + 512], ps[bank][64:128, :], [PB_[bank]], W)
                continue
            elif fc < 8:
                s0 = (4 * b) % NK
                dst, W = kT[:, fc - 4, s0 * 128:s0 * 128 + 512], [Bk[s0 + i] for i in range(4)]
            else:
                dst, W = ust[b % 2][:, fc - 8, :], [Bust[b % 2]]
            CP(alt(), dst, ps[bank][:, :], [PB_[bank]], W)
        DMA("sp", uT_dv[:, :, b * 512:(b + 1) * 512], ust[b % 2], [Bust[b % 2]], [Bdr_u], 2)
        for i in range(4):
            bank = pcnt["n"] % 2
            pcnt["n"] += 1
            for kc in range(8):
                MM(ps[bank][:, :], xb[:, kc, i * 128:(i + 1) * 128], w_in_b[:, kc, 1024:1536], kc == 0, kc == 7,
                   [Bwin, Bx], [PB_[bank]])
            sl = (4 * b + i) % NK
            pv4 = ps[bank][:, :].rearrange("p (i t d) -> p i t d", i=4, t=2)
            CP("act", vz[0][:, sl, :, 0:64], pv4[:, :, 0, :], [PB_[bank]], [Bv[sl]])
            CP("dve", vz[1][:, sl, :, 64:128], pv4[:, :, 1, :], [PB_[bank]], [Bv[sl]])

    inproj(0)
    for b in range(16):
        lanes = [[], []]
        if b < 15:
            S.lane = lanes[0]
            inproj(b + 1)
        S.lane = lanes[1]
        lo = max(0, 4 * b - 2)
        hi = 4 * b + 2 if b < 15 else 64
        attn_tiles(list(range(lo, hi)))
        S.lane = None
        na, nb_ = len(lanes[0]), len(lanes[1])
        S.run_lanes(lanes, chunk=[1, max(1, nb_ // max(na, 1))])
    S.barrier()
    A.off = persist

    def finish():
        S.op("sp", lambda e: e.nop(), extra=out_writes)
        S.emit(sems)
        es.close()
        return nc

    def do_tap(src, Bsrc):
        out_writes.append(DMA("sp", tapo, src, [Bsrc], [], 9))

    if stop == 1:
        do_tap({"uT_d": uT_d, "attT_d": attT_d}[tap[0]], {"uT_d": Bdr_u, "attT_d": Bdr_att}[tap[0]])
        return finish()

    sg_d = dscr("sg_d", [512, NT], BF16)
    sg_dv = sg_d.rearrange("(f p) t -> p f t", p=128)
    Bdr_sg = Buf("sg_d")
    PBt = A.alloc(NPB, F32)
    BPB, BPA, Bglob = Buf("PB"), Buf("PA"), Buf("glob")

    def v4(ap):
        return ap.rearrange("p (d q c) -> p d q c", d=2, q=16)

    BBr, BBi, CBr, CBi = (v4(PBt[:, 96:608]), v4(PBt[:, 608:1120]), v4(PBt[:, 1120:1632]), v4(PBt[:, 1632:2144]))
    DAc = A.alloc(4, F32)
    PW = A.alloc(17 * 64, F32).rearrange("p (k r n) -> p k r n", k=17, r=2)
    SC = A.alloc(9 * 3 * 32, F32).rearrange("p (l k c n) -> p l k c n", l=1, k=9, c=3)
    BbB = A.alloc(2 * 512, F32).rearrange("p (r d q c) -> p r d q c", r=2, d=2, q=16)
    BbA = A.alloc(2 * 512, F32).rearrange("p (r n) -> p r n", r=2)
    lamA = A.alloc(2 * 512, F32).rearrange("p (r n) -> p r n", r=2)
    p2mark = A.off
    PAt = A.alloc(NPA, F32)
    DMA("sp", PBt, PBd, [], [BPB], 1)
    DMA("sp", PAt, PAd, [], [BPA], 1)
    arB, aiB, ldB = PBt[:, 0:32], PBt[:, 32:64], PBt[:, 64:96]
    arA, aiA, ldA = PAt[:, 0:512], PAt[:, 512:1024], PAt[:, 1024:1536]
    BAr, BAi = PAt[:, 1536:2048], PAt[:, 2048:2560]

    def cmul(eng, o_r, o_i, a_r, a_i, b_r, b_i, t1, t2, R, W):
        TT(eng, t1, a_r, b_r, ALU.mult, R, W)
        TT(eng, t2, a_i, b_i, ALU.mult, R, W)
        TT(eng, o_r, t1, t2, ALU.subtract, R, W)
        TT(eng, t1, a_r, b_i, ALU.mult, R, W)
        TT(eng, t2, a_i, b_r, ALU.mult, R, W)
        TT(eng, o_i, t1, t2, ALU.add, R, W)

    def lam_z(ar, ai, ld, n, Bin, lr, li, zr, zi):
        R, W = [Bin, Bglob], [Bglob]
        dt, mag, th, kk, r, sn, cs, tmp = [A.alloc(n, F32) for _ in range(8)]
        ACTF(dt, ld, AF.Exp, R, W)
        TT("dve", mag, ar, dt, ALU.mult, R, W)
        ACTF(mag, mag, AF.Exp, R, W)
        TT("dve", th, ai, dt, ALU.mult, R, W)
        TS("dve", kk, th, float(1 / (2 * np.pi)), ALU.mult, R, W, s2=MAGIC, op1=ALU.add)
        TS("dve", kk, kk, -MAGIC, ALU.add, R, W)
        STT("dve", r, kk, -C1, th, ALU.mult, ALU.add, R, W)
        STT("dve", r, kk, -C2, r, ALU.mult, ALU.add, R, W)
        TS("dve", r, r, float(np.pi), ALU.min, R, W, s2=-float(np.pi), op1=ALU.max)
        ACTF(sn, r, AF.Sin, R, W)
        TS("dve", tmp, r, -1.0, ALU.mult, R, W)
        TT("dve", tmp, tmp, r, ALU.max, R, W)
        TS("dve", tmp, tmp, -1.0, ALU.mult, R, W, s2=float(np.pi / 2), op1=ALU.add)
        ACTF(cs, tmp, AF.Sin, R, W)
        TT("dve", lr, mag, cs, ALU.mult, R, W)
        TT("dve", li, mag, sn, ALU.mult, R, W)
        TT("dve", tmp, ar, ar, ALU.mult, R, W)
        TT("dve", kk, ai, ai, ALU.mult, R, W)
        TT("dve", tmp, tmp, kk, ALU.add, R, W)
        S.op("dve", lambda e: e.reciprocal(out=tmp, in_=tmp), R, W)
        TS("dve", r, lr, -1.0, ALU.add, R, W)
        TT("dve", kk, r, ar, ALU.mult, R, W)
        TT("dve", th, li, ai, ALU.mult, R, W)
        TT("dve", kk, kk, th, ALU.add, R, W)
        TT("dve", zr, kk, tmp, ALU.mult, R, W)
        TT("dve", kk, li, ar, ALU.mult, R, W)
        TT("dve", th, r, ai, ALU.mult, R, W)
        TT("dve", kk, kk, th, ALU.subtract, R, W)
        TT("dve", zi, kk, tmp, ALU.mult, R, W)

    RG, WG = [BPB, BPA, Bglob], [Bglob]
    CP("dve", DAc, PAt[:, 2560:2564], RG, WG)
    zB = [A.alloc(32, F32) for _ in range(2)]
    lam_z(arB, aiB, ldB, 32, BPB, PW[:, 1, 0, :], PW[:, 1, 1, :], zB[0], zB[1])
    t1b, t2b = A.alloc(512, F32), A.alloc(512, F32)
    MEMSET("dve", PW[:, 0, 0, :], 1.0, WG)
    MEMSET("dve", PW[:, 0, 1, :], 0.0, WG)
    for k in range(2, 17):
        cmul("dve", PW[:, k, 0, :], PW[:, k, 1, :], PW[:, k - 1, 0, :], PW[:, k - 1, 1, :],
             PW[:, 1, 0, :], PW[:, 1, 1, :], t1b[:, 0:32], t2b[:, 0:32], RG, WG)
    CP("dve", SC[:, 0, 0, 0:2, :], PW[:, 16, 0:2, :], RG, WG)
    for k in range(1, 9):
        cmul("dve", SC[:, 0, k, 0, :], SC[:, 0, k, 1, :], SC[:, 0, k - 1, 0, :], SC[:, 0, k - 1, 1, :],
             SC[:, 0, k - 1, 0, :], SC[:, 0, k - 1, 1, :], t1b[:, 0:32], t2b[:, 0:32], RG, WG)
    TS("dve", SC[:, 0, :, 2, :], SC[:, 0, :, 1, :], -1.0, ALU.mult, RG, WG)

    def bc(ap, shape):
        return ap.to_broadcast(shape)

    zrb = bc(zB[0].unsqueeze(2), [128, 32, 16])
    zib = bc(zB[1].unsqueeze(2), [128, 32, 16])
    f3 = lambda ap: ap.rearrange("p d q c -> p (d q) c")
    f3o = lambda ap: ap.rearrange("p (n c) -> p n c", c=16)
    cmul("dve", f3(BbB[:, 0]), f3(BbB[:, 1]), zrb, zib, f3(BBr), f3(BBi), f3o(t1b), f3o(t2b), RG, WG)
    zA = [A.alloc(512, F32) for _ in range(2)]
    lam_z(arA, aiA, ldA, 512, BPA, lamA[:, 0, :], lamA[:, 1, :], zA[0], zA[1])
    cmul("dve", BbA[:, 0, :], BbA[:, 1, :], zA[0], zA[1], BAr, BAi, t1b, t2b, RG, WG)
    S.barrier()
    A.off = p2mark

    uTf = [A.alloc(NT, BF16)]
    uds = A.alloc(NT, BF16).rearrange("p (s j) -> p s j", s=16)
    Buds = Buf("uds")
    BuT = [[Buf() for _ in range(16)]]
    TXs = [A.alloc(16 * 2 * 128, BF16).rearrange("p (k r g c) -> p k r g c", k=16, r=2, g=2) for _ in range(2)]
    TX3s = [A.alloc(16 * 2 * 128, BF16).rearrange("p (k r g c) -> p k r g c", k=16, r=2, g=2) for _ in range(2)]
    BTXs = [Buf(), Buf()]
    Wb = A.alloc(2 * 2 * 4 * 17 * 16, BF16).rearrange("p (r d q t c) -> p r d q t c", r=2, d=2, q=4, t=17)
    TC = A.alloc(2 * 4 * 2 * 512, BF16).rearrange("p (d q r n) -> p d q r n", d=2, q=4, r=2)
    Bp = A.alloc(2 * 8 * 2 * 128, BF16).rearrange("p (d g r n) -> p d g r n", d=2, g=8, r=2)
    FIRt = A.alloc(2 * 16 * 128, BF16).rearrange("p (d t n) -> p d t n", d=2, t=16)
    Sf = A.alloc(2 * 4 * 2 * 514, BF16).rearrange("p (d q r n) -> p d q r n", d=2, q=4, r=2)
    BTX, BWf, BWb, BTC, BBp, BFIR, BEp = [Buf() for _ in range(7)]
    BSf = [[Buf() for _ in range(4)] for _ in range(2)]
    MEMSET("pool", TC, 0.0, [BTC])
    MEMSET("pool", Bp, 0.0, [BBp])
    MEMSET("pool", Sf, 0.0, [b for r in BSf for b in r])
    Xs = [A.alloc(1024, F32).rearrange("p (r n) -> p r n", r=2) for _ in range(4)]
    BXs = [Buf() for _ in range(4)]
    scr0 = A.off
    EpA = A.alloc(16 * 128, F32).rearrange("p (k r c) -> p k r c", k=16, r=2)
    VA = A.alloc(16 * 128, F32).rearrange("p (k r c) -> p k r c", k=16, r=2)
    tA = A.alloc(16 * 64, F32)
    Wf = A.alloc(2 * 4 * 17 * 16, F32).rearrange("p (r q t c) -> p r q t c", r=2, q=4, t=17)
    lpw = A.alloc(3 * 128, F32).rearrange("p (m r c) -> p m r c", m=3, r=2)
    tW = EpA.rearrange("p k r c -> p (k r c)")[:, 0:4 * 17 * 16]
    tW2 = VA.rearrange("p k r c -> p (k r c)")[:, 0:4 * 17 * 16]
    scr1 = A.off
    A.off = scr0
    Yi = [A.alloc(16 * 128, BF16).rearrange("p (k n) -> p k n", k=16) for _ in range(2)]
    BYi = [Buf(), Buf()]
    Yt = A.alloc(2048, F32)
    BYt = Buf()
    ytm = [A.alloc(512, F32) for _ in range(2)]
    gt1 = [A.alloc(512, F32) for _ in range(2)]
    sgo = [A.alloc(512, BF16) for _ in range(2)]
    Bytm = [Buf(), Buf()]
    A.off = max(A.off, scr1)
    lamA4 = lamA.rearrange("p r (d f c) -> p r d f c", d=2, f=4)
    BbA4 = BbA.rearrange("p r (d f c) -> p r d f c", d=2, f=4)

    def cmac(eng, dst, src, l, k, col, R, W):
        a, b, nb = SC[:, l, k, 0, col:col + 1], SC[:, l, k, 1, col:col + 1], SC[:, l, k, 2, col:col + 1]
        STT(eng, dst[:, 0, :], src[:, 0, :], a, dst[:, 0, :], ALU.mult, ALU.add, R, W)
        STT(eng, dst[:, 0, :], src[:, 1, :], nb, dst[:, 0, :], ALU.mult, ALU.add, R, W)
        STT(eng, dst[:, 1, :], src[:, 0, :], b, dst[:, 1, :], ALU.mult, ALU.add, R, W)
        STT(eng, dst[:, 1, :], src[:, 1, :], a, dst[:, 1, :], ALU.mult, ALU.add, R, W)

    def scan(eng, X, n, l, rev, col, R, W):
        nlev = n.bit_length() - 1
        for k in range(nlev):
            st, h = 2 << k, 1 << k
            if not rev:
                cmac(eng, X[:, :, st - 1::st], X[:, :, h - 1::st], 0, k, col, R, W)
            else:
                cmac(eng, X[:, :, 0::st], X[:, :, h::st], 0, k, col, R, W)
        for k in range(nlev - 2, -1, -1):
            st, h = 2 << k, 1 << k
            if not rev:
                cmac(eng, X[:, :, st - 1 + h::st], X[:, :, st - 1:n - st:st], 0, k, col, R, W)
            else:
                cmac(eng, X[:, :, h:n - st:st], X[:, :, st::st], 0, k, col, R, W)

    pdn = {"n": 0, "pin": 0, "pt": 0, "pf": 0}
    for f in range(4):
        uf, Buf_f = uTf[0], BuT[0]
        DMA("sp", uf, uT_dv[:, f, :], [Bdr_u], Buf_f, 1)
        ufv = uf.rearrange("p (j t) -> p t j", t=16)
        CP("act", uds[:, 0:8, :], ufv[:, 0:8, :], Buf_f, [Buds])
        CP("dve", uds[:, 8:16, :], ufv[:, 8:16, :], Buf_f, [Buds])
        for d in range(2):
            Cr = bc(CBr[:, d, 4 * f:4 * f + 4, :].unsqueeze(2), [128, 4, 17, 16])
            Ci = bc(CBi[:, d, 4 * f:4 * f + 4, :].unsqueeze(2), [128, 4, 17, 16])
            pr = bc(PW[:, :, 0, d * 16 + 4 * f:d * 16 + 4 * f + 4].rearrange("p t q -> p q t").unsqueeze(3),
                    [128, 4, 17, 16])
            pi_ = bc(PW[:, :, 1, d * 16 + 4 * f:d * 16 + 4 * f + 4].rearrange("p t q -> p q t").unsqueeze(3),
                     [128, 4, 17, 16])
            t1v = tW.rearrange("p (q t c) -> p q t c", q=4, t=17)
            t2v = tW2.rearrange("p (q t c) -> p q t c", q=4, t=17)
            cmul("dve", Wf[:, 0], Wf[:, 1], Cr, Ci, pr, pi_, t1v, t2v, [BPB, Bglob, BWf], [BWf])
            for ri in range(2):
                CP("act", Wb[:, ri, d].rearrange("p q t c -> p (q t c)"), Wf[:, ri].rearrange("p q t c -> p (q t c)"),
                   [BWf], [BWb])
            for g in range(2):
                for ri in range(2):
                    o = TC[64 * g:64 * g + 64, d, :, ri, :].rearrange("p q (k g c) -> p q k g c", k=16, g=2)[:, :, :, g, :]
                    i_ = Wf[64 * g:64 * g + 64, ri, :, 1:17, :]
                    ACTF(o, i_, AF.Copy, [BWf], [BTC], scale=1.0 if ri == 0 else -1.0)
        for g8 in range(8):
            g, qq = g8 % 2, g8 // 2
            for d in range(2):
                for ri in range(2):
                    ACTF(Bp[64 * g:64 * g + 64, d, g8, ri, g8 * 16:(g8 + 1) * 16],
                         BbB[64 * g:64 * g + 64, ri, d, 4 * f + qq, :], AF.Copy, [Bglob], [BBp],
                         scale=1.0 if ri == 0 else -1.0)
        for d in range(2):
            for bk in range(4):
                ov = ps[bk][:, :].rearrange("p (t n) -> p t n", t=4)
                for g8 in range(8):
                    for ri in range(2):
                        MM(ov[:, :, g8 * 16:(g8 + 1) * 16], Bp[:, d, g8, ri, :], Wb[:, ri, d, g8 // 2, 4 * bk:4 * bk + 4, :],
                           g8 == 0 and ri == 0, g8 == 7 and ri == 1, [BBp, BWb], [PB_[bk]])
                CP(alt(), FIRt[:, d, 4 * bk:4 * bk + 4, :], ov, [PB_[bk]], [BFIR])
                if d == 0 and bk == 0:
                    STT("dve", FIRt[:, 0, 0, :], identf, DAc[:, f:f + 1], ps[0][:, 0:128], ALU.mult, ALU.add,
                        [PB_[0], Bcst, Bglob], [BFIR])
        def tx_gen(d):
            TX, TX3, BTX = TXs[d], TX3s[d], BTXs[d]
            RA, WA = [Bglob, BEp], [BEp]
            lr_ = lamA4[:, 0, d, f, :]
            li_ = lamA4[:, 1, d, f, :]
            MEMSET("dve", EpA[:, 0, 0, :], 1.0, WA)
            MEMSET("dve", EpA[:, 0, 1, :], 0.0, WA)
            CP("dve", EpA[:, 1, 0, :], lr_, RA, WA)
            CP("dve", EpA[:, 1, 1, :], li_, RA, WA)
            pr_, pi_ = lr_, li_
            for lv, m in enumerate((2, 4, 8)):
                cmul("dve", lpw[:, lv, 0, :], lpw[:, lv, 1, :], pr_, pi_, pr_, pi_, tA[:, 0:64], tA[:, 64:128], RA, WA)
                pr_, pi_ = lpw[:, lv, 0, :], lpw[:, lv, 1, :]
                t1 = tA[:, 0:m * 64].rearrange("p (k c) -> p k c", k=m)
                t2 = tA[:, 512:512 + m * 64].rearrange("p (k c) -> p k c", k=m)
                cmul("dve", EpA[:, m:2 * m, 0, :], EpA[:, m:2 * m, 1, :], EpA[:, 0:m, 0, :], EpA[:, 0:m, 1, :],
                     bc(pr_.unsqueeze(1), [128, m, 64]), bc(pi_.unsqueeze(1), [128, m, 64]), t1, t2, RA, WA)
            Br_ = bc(BbA4[:, 0, d, f, :].unsqueeze(1), [128, 16, 64])
            Bi_ = bc(BbA4[:, 1, d, f, :].unsqueeze(1), [128, 16, 64])
            tAv = tA.rearrange("p (k c) -> p k c", k=16)
            TT("dve", VA[:, :, 0, :], EpA[:, :, 0, :], Br_, ALU.mult, RA, WA)
            TT("dve", tAv, EpA[:, :, 1, :], Bi_, ALU.mult, RA, WA)
            TT("dve", VA[:, :, 0, :], VA[:, :, 0, :], tAv, ALU.subtract, RA, WA)
            TT("dve", VA[:, :, 1, :], EpA[:, :, 0, :], Bi_, ALU.mult, RA, WA)
            TT("dve", tAv, EpA[:, :, 1, :], Br_, ALU.mult, RA, WA)
            TT("dve", VA[:, :, 1, :], VA[:, :, 1, :], tAv, ALU.add, RA, WA)
            for ri in range(2):
                for g in range(2):
                    TS("dve", TX[:, :, ri, g, :], VA[:, :, ri, :], maskcol[:, g:g + 1], ALU.mult, [BEp, Bcst], [BTX])
                    ACTF(TX3[64:128, :, ri, g, :], VA[64:128, :, ri, :], AF.Identity, [BEp, Bcst], [BTX],
                         scale=mask3[64:128, g:g + 1])

        def x_mm(d):
            TX, TX3, BTX = TXs[d], TX3s[d], BTXs[d]
            for qq in range(4):
                for ri in range(2):
                    bank = 2 * qq + ri
                    for s_ in range(16):
                        k = 15 - s_ if d == 0 else s_
                        if qq < 3:
                            lt = TX[32 * qq:32 * qq + 32, k, ri, :, :].rearrange("p g c -> p (g c)")
                            rh = uds[32 * qq:32 * qq + 32, s_, :]
                        else:
                            lt = TX3[64:128, k, ri, :, :].rearrange("p g c -> p (g c)")
                            rh = uds[64:128, s_, :]
                        MM(ps[bank][:, :], lt, rh, s_ == 0, s_ == 15, [BTX, Buds], [PB_[bank]])

        def x_evac(d):
            for qq in range(4):
                for ri in range(2):
                    bank = 2 * qq + ri
                    CP(alt(), Xs[qq][:, ri, :], ps[bank][:, :], [PB_[bank]], [BXs[qq]])

        def scans(d):
            lanes = [[] for _ in range(4)]
            for qq in range(4):
                S.lane = lanes[qq]
                col = d * 16 + 4 * f + qq
                scan("dve", Xs[qq], 512, 0, d == 1, col, [BXs[qq], Bglob], [BXs[qq]])
                CP("act", Sf[:, d, qq, :, 1:513], Xs[qq], [BXs[qq]], [BSf[d][qq]])
            S.lane = None
            S.run_lanes(lanes)

        tx_gen(0)
        x_mm(0)
        x_evac(0)
        tx_gen(1)
        x_mm(1)
        scans(0)
        x_evac(1)
        scans(1)
        S.barrier()
        for ct in range(4):
            for d in range(2):
                off = 128 * ct if d == 0 else 128 * ct + 2
                for qq in range(4):
                    pin = 4 + pdn["pin"] % 2
                    pdn["pin"] += 1
                    for ri in range(2):
                        MM(ps[pin][:, :], Sf[:, d, qq, ri, off:off + 128], TC[:, d, qq, ri, :], ri == 0, ri == 1,
                           [BSf[d][qq], BTC], [PB_[pin]])
                    CP(alt(), Yi[d][:, :, 32 * qq:32 * qq + 32], ps[pin][:, :].rearrange("p (k n) -> p k n", k=16),
                       [PB_[pin]], [BYi[d]])
            for tg in range(4):
                pt = 6 + pdn["pt"] % 2
                pdn["pt"] += 1
                for t4 in range(4):
                    tau = 4 * tg + t4
                    MM(ps[pt][:, t4 * 128:(t4 + 1) * 128], Yi[0][:, tau, :], identb, True, False,
                       [BYi[0], Bcstb], [PB_[pt]])
                    MM(ps[pt][:, t4 * 128:(t4 + 1) * 128], Yi[1][:, 15 - tau, :], identb, False, True,
                       [BYi[1], Bcstb], [PB_[pt]])
                CP(alt(), Yt.rearrange("p (tb s jl) -> p s tb jl", tb=4, s=16)[:, 4 * tg:4 * tg + 4, :, :],
                   ps[pt][:, :].rearrange("p (t tb jl) -> p t tb jl", t=4, tb=4), [PB_[pt]], [BYt])
            for tb in range(4):
                blk = 4 * ct + tb
                pf = pdn["pf"] % 2
                pdn["pf"] += 1
                ub = uds[:, :, 32 * blk:32 * blk + 32]
                pv = ps[pf][:, :].rearrange("p (s j) -> p s j", s=16)
                RB = [BFIR, Buds]
                MM(pv, FIRt[:, 0, 0, :], ub, True, False, RB, [PB_[pf]])
                for tau in range(1, 16):
                    MM(pv[:, tau:16, :], FIRt[:, 0, tau, :], ub[:, 0:16 - tau, :], False, False, RB, [PB_[pf]])
                for tau in range(16):
                    MM(pv[:, 0:16 - tau, :], FIRt[:, 1, tau, :], ub[:, tau:16, :], False, tau == 15, RB, [PB_[pf]])
                y, g1, so, By = ytm[pf], gt1[pf], sgo[pf], Bytm[pf]
                TT("dve", y, ps[pf][:, :], Yt[:, tb * 512:(tb + 1) * 512], ALU.add, [PB_[pf], BYt], [By])
                ACTF(g1, y, AF.Square, [By], [By])
                TS("dve", g1, g1, 0.044715, ALU.mult, [By], [By], s2=1.0, op1=ALU.add)
                TT("dve", g1, g1, y, ALU.mult, [By], [By])
                ACTF(g1, g1, AF.Sigmoid, [By], [By], scale=1.5957691216057308)
                TT("dve", so.rearrange("p (jl s) -> p s jl", s=16), y.rearrange("p (s jl) -> p s jl", s=16),
                   g1.rearrange("p (s jl) -> p s jl", s=16), ALU.mult, [By], [By])
                DMA("sp", sg_dv[:, f, blk * 512:(blk + 1) * 512], so, [By], [Bdr_sg], 2)
        S.barrier()
    A.off = persist

    if stop == 2:
        do_tap(sg_d, Bdr_sg)
        return finish()

    AX = mybir.AxisListType.X
    lnt = A.alloc(4 * DM, F32).rearrange("p (i n) -> p i n", i=4)
    Bln = Buf("ln")
    for i in range(4):
        DMA("sp", lnt[:, i, :], lnp[i].partition_broadcast(128), [], [Bln], 1)
    epsc = A.alloc(1, F32)
    MEMSET("dve", epsc, EPS, [Bln])
    persist2 = A.off
    wglu_b = A.alloc(4 * 512, BF16).rearrange("p (k n) -> p k n", k=4)
    bglu = A.alloc(4, F32)
    wout_b = A.alloc(8 * DM, BF16).rearrange("p (k n) -> p k n", k=8)
    wr_f = A.alloc(8 * 72, F32).rearrange("p (k n) -> p k n", k=8)
    brb = A.alloc(72, F32)
    Bw3 = Buf("w3")
    DMA("pool", wglu_b, w_glu.rearrange("(k p) n -> p k n", p=128), [], [Bw3], 0)
    for kc in range(8):
        DMA("pool", wout_b[:, kc, :], w_out.rearrange("(k p) n -> p k n", p=128)[:, kc, :], [], [Bw3], 0)
    DMA("sp", bglu, b_glu, [], [Bw3], 1)
    DMA("sp", wr_f, w_r.rearrange("(k p) n -> p k n", p=128), [], [Bw3], 1)
    DMA("sp", brb, b_r[0].partition_broadcast(128), [], [Bw3], 1)
    sgb = [A.alloc(4 * 512, BF16).rearrange("p (k n) -> p k n", k=4) for _ in range(2)]
    atb = [A.alloc(4 * 512, BF16).rearrange("p (k n) -> p k n", k=4) for _ in range(2)]
    sTg = [A.alloc(4 * 512, BF16).rearrange("p (k n) -> p k n", k=4) for _ in range(2)]
    gtb = [A.alloc(512, BF16) for _ in range(2)]
    Bsgb, Batb, BsTg, Bgtb = ([Buf(), Buf()] for _ in range(4))
    NL = 4
    xt = [A.alloc(DM, F32) for _ in range(NL)]
    rt = [A.alloc(DM, F32) for _ in range(NL)]
    hbt2 = [[A.alloc(DM, BF16) for _ in range(NL)] for _ in range(2)]
    lnscr = [A.alloc(16, F32) for _ in range(NL)]
    Blns = [Buf() for _ in range(NL)]
    hTs = [A.alloc(8 * 128, F32).rearrange("p (k n) -> p k n", k=8) for _ in range(NL)]
    sms = [A.alloc(512, F32) for _ in range(NL)]
    twbs = [A.alloc(64, BF16) for _ in range(NL)]
    Bxt, Brt, BhTs, Bsms = ([Buf() for _ in range(NL)] for _ in range(4))
    Bhbt2 = [[Buf() for _ in range(NL)] for _ in range(2)]
    Bhbuf, Bxbuf, Bobuf = Buf("hbuf"), Buf("xbuf"), Buf("obuf")

    def layer_norm(r, gi, R, W, scr, Bs):
        st, mv, sd, nb = scr[:, 0:12], scr[:, 12:14], scr[:, 14:15], scr[:, 15:16]
        S.op("dve", lambda e: e.bn_stats(out=st[:, 0:6], in_=r[:, 0:512]), R + [Bs], [Bs])
        S.op("dve", lambda e: e.bn_stats(out=st[:, 6:12], in_=r[:, 512:1024]), R + [Bs], [Bs])
        S.op("dve", lambda e: e.bn_aggr(out=mv, in_=st), [Bs], [Bs])
        ACTF(sd, mv[:, 1:2], AF.Sqrt, [Bs, Bln], [Bs], bias=epsc[:, 0:1])
        S.op("dve", lambda e: e.reciprocal(out=sd, in_=sd), [Bs], [Bs])
        STT("dve", nb, mv[:, 0:1], -1.0, sd, ALU.mult, ALU.mult, [Bs], [Bs])
        ACTF(r, r, AF.Identity, R + [Bs], W, scale=sd, bias=nb)
        TT("dve", r, r, lnt[:, gi, :], ALU.mult, R + [Bln], W)
        TT("dve", r, r, lnt[:, gi + 1, :], ALU.add, R + [Bln], W)

    def p3_f1(b, i):
        bi, tt = b % 2, 4 * b + i
        sm, Bsm = sms[i], Bsms[i]
        for half in range(2):
            bank = half
            for kc in range(8):
                lt = atb[bi][:, kc, i * 128:(i + 1) * 128] if kc < 4 else sTg[bi][:, kc - 4, i * 128:(i + 1) * 128]
                MM(ps[bank][:, :], lt, wout_b[:, kc, half * 512:(half + 1) * 512], kc == 0, kc == 7,
                   [Batb[bi], BsTg[bi], Bw3], [PB_[bank]])
            STT("dve", rt[i][:, half * 512:(half + 1) * 512], xt[i][:, half * 512:(half + 1) * 512], ALPHA,
                ps[bank][:, :], ALU.mult, ALU.add, [Bxt[i], PB_[bank]], [Brt[i]])
        h = rt[i]
        layer_norm(h, 0, [Brt[i]], [Brt[i]], lnscr[i], Blns[i])
        DMA("sp", hbuf[tt * 128:(tt + 1) * 128, :], h, [Brt[i]], [Bhbuf], 2)
        CP("act", hbt2[b % 2][i], h, [Brt[i]], [Bhbt2[b % 2][i]])

    def p3_f2(b, i):
        h, hT, BhT, mb = rt[i], hTs[i], BhTs[i], 4 + i
        for kc in range(8):
            bank = 2 + kc // 4
            TR(ps[bank][:, (kc % 4) * 128:(kc % 4 + 1) * 128], h[:, kc * 128:(kc + 1) * 128], identf,
               [Brt[i], Bcst], [PB_[bank]])
        CP("act", hT[:, 0:4, :], ps[2][:, :].rearrange("p (k n) -> p k n", k=4), [PB_[2]], [BhT])
        CP("act", hT[:, 4:8, :], ps[3][:, :].rearrange("p (k n) -> p k n", k=4), [PB_[3]], [BhT])
        for kc in range(8):
            MM(ps[mb][:, 0:72], hT[:, kc, :], wr_f[:, kc, :], kc == 0, kc == 7, [BhT, Bw3], [PB_[mb]])

    def p3_router(b, i):
        tt = 4 * b + i
        sm, Bsm, twb, mb = sms[i], Bsms[i], twbs[i], 4 + i
        lg = sm[:, 0:72]
        el = sm[:, 8:72].rearrange("p (g e) -> p g e", g=8)
        gmax, ngm, gsum, gval = sm[:, 72:73], sm[:, 73:74], sm[:, 74:75], sm[:, 75:76]
        ge, ohg = sm[:, 80:88], sm[:, 88:96]
        prod = sm[:, 96:160].rearrange("p (g e) -> p g e", g=8)
        ein, oh1, e2, oh2 = sm[:, 160:168], sm[:, 168:176], sm[:, 176:184], sm[:, 184:192]
        m1, m2, dm, w1 = sm[:, 192:193], sm[:, 193:194], sm[:, 194:195], sm[:, 195:196]
        o64 = [sm[:, 200:264], sm[:, 264:328]]
        R_, W_ = [Bsm], [Bsm]
        TT("dve", lg, ps[mb][:, 0:72], brb, ALU.add, [PB_[mb], Bw3, Bsm], W_)
        S.op("dve", lambda e: e.reduce_max(out=gmax, in_=lg[:, 0:8], axis=AX), R_, W_)
        TS("dve", ngm, gmax, -1.0, ALU.mult, R_, W_)
        ACTF(ge, lg[:, 0:8], AF.Exp, R_, W_, bias=ngm)
        S.op("dve", lambda e: e.reduce_sum(out=gsum, in_=ge, axis=AX), R_, W_)
        S.op("dve", lambda e: e.reciprocal(out=gval, in_=gsum), R_, W_)
        TS("dve", ohg, lg[:, 0:8], gmax, ALU.is_equal, R_, W_)
        TT("dve", prod, el, bc(ohg.unsqueeze(2), [128, 8, 8]), ALU.mult, R_, W_)
        S.op("dve", lambda e: e.tensor_reduce(out=ein, in_=prod.rearrange("p g e -> p e g"), axis=AX,
                                              op=ALU.add), R_, W_)
        S.op("dve", lambda e: e.reduce_max(out=m1, in_=ein, axis=AX), R_, W_)
        TS("dve", oh1, ein, m1, ALU.is_equal, R_, W_)
        STT("dve", e2, oh1, -1e30, ein, ALU.mult, ALU.add, R_, W_)
        S.op("dve", lambda e: e.reduce_max(out=m2, in_=e2, axis=AX), R_, W_)
        TS("dve", oh2, e2, m2, ALU.is_equal, R_, W_)
        TT("dve", dm, m2, m1, ALU.subtract, R_, W_)
        ACTF(dm, dm, AF.Exp, R_, W_)
        TS("dve", dm, dm, 1.0, ALU.add, R_, W_)
        S.op("dve", lambda e: e.reciprocal(out=w1, in_=dm), R_, W_)
        TT("dve", gates_all[:, tt, 0:1], gval, w1, ALU.mult, R_, [Bgates])
        TT("dve", gates_all[:, tt, 1:2], gval, gates_all[:, tt, 0:1], ALU.subtract, R_ + [Bgates], [Bgates])
        for k, ohk in enumerate((oh1, oh2)):
            TT("dve", o64[k].rearrange("p (g e) -> p g e", g=8), bc(ohg.unsqueeze(2), [128, 8, 8]),
               bc(ohk.unsqueeze(1), [128, 8, 8]), ALU.mult, R_, W_)
        TT("dve", twb, o64[0], o64[1], ALU.add, R_, W_)

    def p3_dispatch(b, i):
        tt = 4 * b + i
        sm, Bsm, twb, mb = sms[i], Bsms[i], twbs[i], 4 + i
        o64 = [sm[:, 200:264], sm[:, 264:328]]
        posC, tmp64, dkf = sm[:, 328:392], sm[:, 420:484], sm[:, 392:394]
        R_, W_ = [Bsm], [Bsm]
        MM(ps[mb][:, 128:192], ltrib, twb, True, True, [Bsm, Bcstb], [PB_[mb]])
        MM(ps[mb][:, 192:256], onesb, twb, True, True, [Bsm, Bcstb], [PB_[mb]])
        TT("dve", posC, ps[mb][:, 128:192], runc, ALU.add, [PB_[mb], Brun, Bsm], W_)
        TT("dve", posC, posC, eC, ALU.add, R_ + [Bcst], W_)
        TT("dve", runc, runc, ps[mb][:, 192:256], ALU.add, [PB_[mb], Brun], [Brun])
        for k in range(2):
            TT("dve", tmp64, o64[k], posC, ALU.mult, R_, W_)
            S.op("dve", lambda e, k=k: e.reduce_sum(out=dkf[:, k:k + 1], in_=tmp64, axis=AX), R_, W_)
            CP("dve", dest_all[:, tt, k:k + 1], dkf[:, k:k + 1], R_, [Bdest])
            S.op("pool", lambda e, k=k: e.indirect_dma_start(
                out=xbuf, out_offset=bass.IndirectOffsetOnAxis(ap=dest_all[:, tt, k:k + 1], axis=0),
                in_=hbt2[b % 2][i], in_offset=None), [Bhbt2[b % 2][i], Bdest], [Bxbuf], dma=3)

    def p3_loads(b):
        bi = b % 2
        DMA("sp", sgb[bi], sg_dv[:, :, b * 512:(b + 1) * 512], [Bdr_sg], [Bsgb[bi]], 1)
        DMA("sp", atb[bi], attT_dv[:, :, b * 512:(b + 1) * 512], [Bdr_att], [Batb[bi]], 1)

    def p3_glu(b):
        bi = b % 2
        for oc in range(4):
            bank = oc % 2
            for kc in range(4):
                MM(ps[bank][:, :], wglu_b[:, kc, oc * 128:(oc + 1) * 128], sgb[bi][:, kc, :], kc == 0, kc == 3,
                   [Bw3, Bsgb[bi]], [PB_[bank]])
            ACTF(gtb[bank], ps[bank][:, :], AF.Sigmoid, [PB_[bank], Bw3], [Bgtb[bank]], bias=bglu[:, oc:oc + 1])
            TT("dve", sTg[bi][:, oc, :], sgb[bi][:, oc, :], gtb[bank], ALU.mult, [Bsgb[bi], Bgtb[bank]], [BsTg[bi]])

    p3_loads(0)
    p3_glu(0)
    for b in range(16):
        bi = b % 2
        for i in range(NL):
            tt_ = 4 * b + i
            DMA("sp", xt[i], x[tt_ * 128:(tt_ + 1) * 128, :], [], [Bxt[i]], 1)
        if b < 15:
            p3_loads(b + 1)
        def dsp(i):
            if b > 0:
                p3_dispatch(b - 1, i)
        p3_f1(b, 0)
        dsp(0)
        p3_f1(b, 1)
        dsp(1)
        p3_f2(b, 0)
        p3_f1(b, 2)
        dsp(2)
        p3_f2(b, 1)
        p3_f1(b, 3)
        dsp(3)
        p3_f2(b, 2)
        p3_f2(b, 3)
        if b < 15:
            p3_glu(b + 1)
        lanes = [[] for _ in range(NL)]
        for i in range(NL):
            S.lane = lanes[i]
            p3_router(b, i)
        S.lane = None
        S.run_lanes(lanes)
    for i in range(NL):
        p3_dispatch(15, i)
    S.barrier()
    A.off = persist2

    if stop == 3:
        do_tap(hbuf, Bhbuf)
        return finish()

    NWB = 3
    wg = [A.alloc(8 * 512, BF16).rearrange("p (k n) -> p k n", k=8) for _ in range(NWB)]
    wu = [A.alloc(8 * 512, BF16).rearrange("p (k n) -> p k n", k=8) for _ in range(NWB)]
    wd = [A.alloc(4 * DM, BF16).rearrange("p (k n) -> p k n", k=4) for _ in range(NWB)]
    xgt = [A.alloc(3 * DM, BF16).rearrange("p (s n) -> p s n", s=3) for _ in range(NWB)]
    xgT = [A.alloc(8 * CAP, BF16).rearrange("p (k n) -> p k n", k=8) for _ in range(NWB)]
    hidT = [A.alloc(4 * CAP, BF16).rearrange("p (k n) -> p k n", k=4) for _ in range(NWB)]
    sgt = [A.alloc(CAP, BF16) for _ in range(2)]
    obt = [A.alloc(DM, BF16) for _ in range(3)]
    Bwg, Bwu, Bwd, Bxgt, BxgT, BhidT, Bsgt, Bobt = ([Buf() for _ in range(3)] for _ in range(8))
    c4 = {"t": 0, "g": 0, "d": 0, "o": 0}

    def p4_load(e_):
        bi = e_ % NWB
        DMA("pool", wg[bi], w_gate[e_].rearrange("(k p) n -> p k n", p=128), [], [Bwg[bi]], 5)
        DMA("pool", wu[bi], w_up[e_].rearrange("(k p) n -> p k n", p=128), [], [Bwu[bi]], 5)
        DMA("pool", wd[bi], w_down[e_].rearrange("(k p) n -> p k n", p=128), [], [Bwd[bi]], 5)
        DMA("sp", xgt[bi], xbuf[e_ * CAP:(e_ + 1) * CAP, :].rearrange("(s p) n -> p s n", p=128),
            [Bxbuf], [Bxgt[bi]], 6)

    def p4_A(e_):
        bi = e_ % NWB
        for s_ in range(3):
            bank = c4["t"] % 2
            c4["t"] += 1
            pb16 = ps[bank][:, :].bitcast(BF16)
            for kc in range(8):
                TR(pb16[:, kc * 128:(kc + 1) * 128], xgt[bi][:, s_, kc * 128:(kc + 1) * 128], identb,
                   [Bxgt[bi], Bcstb], [PB_[bank]])
            CP(alt(), xgT[bi][:, :, s_ * 128:(s_ + 1) * 128], pb16.rearrange("p (k n) -> p k n", k=8),
               [PB_[bank]], [BxgT[bi]])

    def p4_B(e_):
        bi = e_ % NWB
        for hc in range(4):
            gi = c4["g"] % 2
            c4["g"] += 1
            bg, bu = 2 + gi, 4 + gi
            for kc in range(8):
                MM(ps[bg][:, 0:CAP], wg[bi][:, kc, hc * 128:(hc + 1) * 128], xgT[bi][:, kc, :], kc == 0, kc == 7,
                   [Bwg[bi], BxgT[bi]], [PB_[bg]])
            for kc in range(8):
                MM(ps[bu][:, 0:CAP], wu[bi][:, kc, hc * 128:(hc + 1) * 128], xgT[bi][:, kc, :], kc == 0, kc == 7,
                   [Bwu[bi], BxgT[bi]], [PB_[bu]])
            ACTF(sgt[gi], ps[bg][:, 0:CAP], AF.Silu, [PB_[bg]], [Bsgt[gi]])
            TT("dve", hidT[bi][:, hc, :], sgt[gi], ps[bu][:, 0:CAP], ALU.mult, [Bsgt[gi], PB_[bu]], [BhidT[bi]])

    def p4_C(e_):
        bi = e_ % NWB
        for s_ in range(3):
            oi = c4["o"] % 3
            c4["o"] += 1
            for half in range(2):
                bank = 6 + c4["d"] % 2
                c4["d"] += 1
                for hc in range(4):
                    MM(ps[bank][:, :], hidT[bi][:, hc, s_ * 128:(s_ + 1) * 128], wd[bi][:, hc, half * 512:(half + 1) * 512],
                       hc == 0, hc == 3, [BhidT[bi], Bwd[bi]], [PB_[bank]])
                CP(alt(), obt[oi][:, half * 512:(half + 1) * 512], ps[bank][:, :], [PB_[bank]], [Bobt[oi]])
            r0 = e_ * CAP + s_ * 128
            DMA("sp", obuf[r0:r0 + 128, :], obt[oi], [Bobt[oi]], [Bobuf], 7)

    p4_load(0)
    p4_load(1)
    p4_A(0)
    p4_B(0)
    p4_A(1)
    for e_ in range(NE):
        if e_ + 2 < NE:
            p4_load(e_ + 2)
        if e_ + 1 < NE:
            p4_B(e_ + 1)
        p4_C(e_)
        if e_ + 2 < NE:
            p4_A(e_ + 2)
    S.barrier()
    A.off = persist2

    N5 = 4
    o1 = [A.alloc(DM, BF16) for _ in range(N5)]
    o2 = [A.alloc(DM, BF16) for _ in range(N5)]
    ht = [A.alloc(DM, F32) for _ in range(N5)]
    yt5 = [A.alloc(DM, F32) for _ in range(N5)]
    Bo1, Bo2, Bht, Byt5 = ([Buf() for _ in range(N5)] for _ in range(4))
    ln5 = [A.alloc(16, F32) for _ in range(N5)]
    Bln5 = [Buf() for _ in range(N5)]
    Bout = Buf("out")
    def p5_load(tt):
        ti = tt % N5
        for k, (ob_, Bo_) in enumerate(((o1[ti], Bo1[ti]), (o2[ti], Bo2[ti]))):
            S.op("pool", lambda e, k=k, ob_=ob_: e.indirect_dma_start(
                out=ob_, out_offset=None, in_=obuf,
                in_offset=bass.IndirectOffsetOnAxis(ap=dest_all[:, tt, k:k + 1], axis=0)),
                [Bobuf, Bdest], [Bo_], dma=4)
        DMA("sp", ht[ti], hbuf[tt * 128:(tt + 1) * 128, :], [Bhbuf], [Bht[ti]], 1)

    def p5_compute(tt):
        ti = tt % N5
        y5 = yt5[ti]
        S.op("act", lambda e: e.mul(out=y5, in_=ht[ti], mul=ALPHA), [Bht[ti]], [Byt5[ti]])
        STT("dve", y5, o1[ti], gates_all[:, tt, 0:1], y5, ALU.mult, ALU.add, [Bo1[ti], Bgates, Byt5[ti]], [Byt5[ti]])
        STT("dve", y5, o2[ti], gates_all[:, tt, 1:2], y5, ALU.mult, ALU.add, [Bo2[ti], Bgates, Byt5[ti]], [Byt5[ti]])
        layer_norm(y5, 2, [Byt5[ti]], [Byt5[ti]], ln5[ti], Bln5[ti])
        out_writes.append(DMA("sp", out[tt * 128:(tt + 1) * 128, :], y5, [Byt5[ti]], [Bout], 8))

    for tt in range(3):
        p5_load(tt)
    for tt in range(64):
        if tt + 3 < 64:
            p5_load(tt + 3)
        p5_compute(tt)
    return finish()


def prep_shared(inp):
    sh = {}
    sh["w_in"] = np.ascontiguousarray(inp["w_in"][0], np.float32)
    sh["w_glu"] = np.ascontiguousarray(inp["w_glu"][0], np.float32)
    sh["b_glu"] = np.ascontiguousarray(inp["b_glu"][0].reshape(4, 128).T, np.float32)
    sh["w_out"] = np.ascontiguousarray(inp["w_out"][0], np.float32)
    sh["lnp"] = np.ascontiguousarray(np.stack([inp["ln1_g"][0], inp["ln1_b"][0], inp["ln2_g"][0], inp["ln2_b"][0]]),
                                     np.float32)
    sh["w_r"] = np.ascontiguousarray(np.concatenate([inp["w_router_group"][0], inp["w_router_expert"][0]], 1),
                                     np.float32)
    sh["b_r"] = np.ascontiguousarray(np.concatenate([inp["b_router_group"][0], inp["b_router_expert"][0]])[None],
                                     np.float32)
    sh["w_gate"] = np.ascontiguousarray(inp["w_gate"][0], np.float32)
    sh["w_up"] = np.ascontiguousarray(inp["w_up"][0], np.float32)
    sh["w_down"] = np.ascontiguousarray(inp["w_down"][0], np.float32)
    sh["btab"] = build_bias_tab(np.asarray(inp["rpb"][0], np.float32))
    sh["consts"] = build_consts()
    PB, PA = build_s5_layouts(*[np.asarray(inp[k][0], np.float32) for k in
                                ("s5_a_re", "s5_a_im", "s5_log_dt", "s5_b_re", "s5_b_im", "s5_c_re", "s5_c_im",
                                 "s5_d")])
    sh["PB"], sh["PA"] = PB, PA
    return sh


def prep_core(inp, b, sh):
    m = dict(sh)
    xb = np.asarray(inp["x"][b], np.float32)
    m["x"] = np.ascontiguousarray(xb)
    m["xT"] = np.ascontiguousarray(xb.T)
    return m


def kernel(**inputs):
    sh = prep_shared(inputs)
    nc = build()
    in_maps = [prep_core(inputs, b, sh) for b in range(8)]
    res = run_bass_kernel_spmd(nc, in_maps, core_ids=list(range(8)))
    return np.stack([np.asarray(r["out"], np.float32) for r in res.results], 0)
```

```python
import os
import numpy as np
from contextlib import ExitStack
import concourse.bass as bass
import concourse.mybir as mybir
from concourse.bass_utils import run_bass_kernel_spmd

F32 = mybir.dt.float32
BF16 = mybir.dt.bfloat16
I32 = mybir.dt.int32
ALU = mybir.AluOpType
AF = mybir.ActivationFunctionType

NT = 8192
DM = 1024
NE = 64
CAP = 384
ALPHA = 2.0 ** 0.25
EPS = 1e-5
ENGINES = ("pe", "act", "dve", "pool", "sp")
DMA_POOL = {"sp": 24, "pool": 12, "act": 4}
SAME_ENGINE_SYNC = not os.environ.get("K_NOSES")
RAW_ONLY = not os.environ.get("K_ALLDEPS")
MAGIC = 12582912.0
C1 = 6.28125
C2 = float(2 * np.pi - 6.28125)
NCLS = 21


class Buf:
    __slots__ = ("name", "w", "r")

    def __init__(self, name=""):
        self.name = name
        self.w = None
        self.r = []


class Op:
    __slots__ = ("id", "eng", "fn", "deps", "dma", "needed", "val", "sem", "raw")

    def __init__(self, id, eng, fn, deps, dma):
        self.id, self.eng, self.fn, self.deps, self.dma = id, eng, fn, deps, dma
        self.needed = False
        self.val = None
        self.sem = None


class Sched:
    def __init__(self, nc):
        self.nc = nc
        self.ops = []
        self.q = {e: [] for e in ENGINES}
        self.last_dma = {}
        self.ndma = {}
        self.lane = None

    def op(self, eng, fn, reads=(), writes=(), dma=None, extra=()):
        if self.lane is not None:
            self.lane.append((eng, fn, tuple(reads), tuple(writes), dma, tuple(extra)))
            return None
        return self._op(eng, fn, reads, writes, dma, extra)

    def run_lanes(self, lanes, chunk=1):
        idx = [0] * len(lanes)
        chunks = chunk if isinstance(chunk, (list, tuple)) else [chunk] * len(lanes)
        while any(idx[i] < len(L) for i, L in enumerate(lanes)):
            for i, L in enumerate(lanes):
                for _ in range(chunks[i]):
                    if idx[i] < len(L):
                        self._op(*L[idx[i]])
                        idx[i] += 1

    def _op(self, eng, fn, reads=(), writes=(), dma=None, extra=()):
        deps = set(extra)
        raw = set(extra)
        for b in reads:
            if b.w is not None:
                deps.add(b.w)
                raw.add(b.w)
        for b in writes:
            if b.w is not None:
                deps.add(b.w)
            deps.update(b.r)
        key = None
        if dma is not None:
            n = self.ndma.get(eng, 0)
            self.ndma[eng] = n + 1
            key = ("dma", eng, n % DMA_POOL[eng])
            if key in self.last_dma:
                deps.add(self.last_dma[key])
        o = Op(len(self.ops), eng, fn, deps, key)
        o.raw = raw
        self.ops.append(o)
        self.q[eng].append(o)
        if key is not None:
            self.last_dma[key] = o.id
        for b in writes:
            b.w = o.id
            b.r = []
        for b in reads:
            b.r.append(o.id)
        return o.id

    def barrier(self):
        last = [self.q[e][-1].id for e in ENGINES if self.q[e]] + list(self.last_dma.values())
        for e in ENGINES:
            self.op(e, lambda en: en.nop(), extra=last)

    def emit(self, sems):
        ops = self.ops
        for o in ops:
            nd = set()
            for d in o.deps:
                p = ops[d]
                if p.dma is None and o.dma is None and p.eng == o.eng:
                    if o.eng == "pe" or not SAME_ENGINE_SYNC or (RAW_ONLY and o.eng != "pool" and d not in o.raw):
                        continue
                nd.add(d)
            o.deps = nd
            for d in nd:
                ops[d].needed = True
        cnt = {e: 0 for e in ENGINES}
        dcnt = {}
        for o in ops:
            if o.dma is not None:
                key = o.dma
                dcnt[key] = dcnt.get(key, 0) + 16
                o.sem, o.val = key, dcnt[key]
            elif o.needed:
                cnt[o.eng] += 1
                o.sem, o.val = o.eng, cnt[o.eng]

        def run(engname):
            def body(e):
                waited = {}
                for o in self.q[engname]:
                    need = {}
                    for d in o.deps:
                        p = ops[d]
                        if p.val > need.get(p.sem, 0):
                            need[p.sem] = p.val
                    for k, v in need.items():
                        if waited.get(k, 0) < v:
                            e.wait_ge(sems[k], v)
                            waited[k] = v
                    ins = o.fn(e)
                    if o.sem is not None:
                        ins.then_inc(sems[o.sem], 16 if o.dma is not None else 1)
            return body

        with self.nc.Block() as block:
            block.tensor(run("pe"))
            block.scalar(run("act"))
            block.vector(run("dve"))
            block.gpsimd(run("pool"))
            block.sync(run("sp"))


class Arena:
    def __init__(self, t, nbytes):
        self.t, self.cap, self.off = t, nbytes, 0

    def alloc(self, n, dt):
        bs = 2 if dt == BF16 else 4
        nb = (n * bs + 63) // 64 * 64
        a0 = self.off
        self.off += nb
        assert self.off <= self.cap, ("arena overflow", self.off, self.cap)
        v = self.t[:, a0 // 4:(a0 + nb) // 4]
        if dt == BF16:
            return v.bitcast(BF16)[:, :n]
        if dt == I32:
            return v.bitcast(I32)[:, :n]
        return v[:, :n]


def _rs(r):
    return min(max(r - 4, 0), 120)


def _classes():
    cl = [(10, 8 + i) for i in range(5)]
    for j in (0, 1):
        cl += [(j, jc) for jc in range(4)]
    for j in (62, 63):
        cl += [(j, jc) for jc in range(60, 64)]
    return cl


def build_bias_tab(rpb):
    cstart = np.clip(np.arange(64) - 8, 0, 48)
    tab = np.full((NCLS, 128, 8, 128), -30000.0, np.float32)
    for ci, (j, jc) in enumerate(_classes()):
        for kl in range(2):
            for ql in range(2):
                kr, qr = 2 * jc + kl, 2 * j + ql
                if _rs(qr) <= kr < _rs(qr) + 8:
                    drow = kr - qr + 7
                    for qc in range(64):
                        kcs = np.arange(cstart[qc], cstart[qc] + 16)
                        tab[ci, kl * 64 + kcs, :, ql * 64 + qc] = rpb[:, drow, kcs - qc + 15].T
    return tab.reshape(NCLS, 128, 1024)


def build_consts():
    c = np.zeros((128, 768), np.float32)
    c[:, 0:128] = np.eye(128, dtype=np.float32)
    c[:, 128:256] = np.triu(np.ones((128, 128), np.float32), 1)
    c[:, 256:384] = 1.0
    c[:, 384:448] = (np.arange(64) * CAP)[None, :]
    p = np.arange(128)
    c[:, 448] = ((p // 16) % 2 == 0)
    c[:, 449] = ((p // 16) % 2 == 1)
    c[:, 450] = ((p // 16) % 2 == 0) & (p >= 96)
    c[:, 451] = ((p // 16) % 2 == 1) & (p >= 96)
    c[:, 512:576] = 1.0
    c[:, 704:768] = 1.0
    return c


def build_s5_layouts(a_re, a_im, log_dt, b_re, b_im, c_re, c_im, dsk):
    def LB(a):
        return a.reshape(2, 16, 2, 64).transpose(2, 3, 0, 1).reshape(128, 32)

    def LBb(b):
        return b.reshape(2, 16, 2, 64, 16).transpose(2, 3, 0, 1, 4).reshape(128, 512)

    def LBc(c):
        return c.reshape(2, 16, 2, 16, 64).transpose(2, 4, 0, 1, 3).reshape(128, 512)

    ld3 = np.broadcast_to(log_dt[:, :, None], (2, 32, 64))
    PB = np.concatenate([LB(a_re), LB(a_im), LB(ld3), LBb(b_re), LBb(b_im), LBc(c_re), LBc(c_im)], 1)

    def LA(a):
        t = a.reshape(2, 4, 8, 64).transpose(2, 0, 1, 3)
        return np.broadcast_to(t[:, None], (8, 16, 2, 4, 64)).reshape(128, 512)

    def LAb(b):
        return b.reshape(2, 4, 8, 64, 16).transpose(2, 4, 0, 1, 3).reshape(128, 512)

    DA = dsk.reshape(4, 8, 16).transpose(1, 2, 0).reshape(128, 4)
    PA = np.concatenate([LA(a_re), LA(a_im), LA(ld3), LAb(b_re), LAb(b_im), DA], 1)
    return np.ascontiguousarray(PB, np.float32), np.ascontiguousarray(PA, np.float32)


NPB = 96 + 2048
NPA = 1536 + 1024 + 4


def build(stop=99, tap=None):
    nc = bass.Bass("TRN2", target_bir_lowering=False)

    def din(name, shape, dt=F32):
        return nc.dram_tensor(name, list(shape), dt, kind="ExternalInput").ap()

    def dscr(name, shape, dt):
        return nc.dram_tensor(name, list(shape), dt).ap()

    xT = din("xT", [DM, NT])
    x = din("x", [NT, DM])
    w_in = din("w_in", [DM, 2048])
    w_glu = din("w_glu", [512, 512])
    b_glu = din("b_glu", [128, 4])
    w_out = din("w_out", [DM, DM])
    lnp = din("lnp", [4, DM])
    w_r = din("w_r", [DM, 72])
    b_r = din("b_r", [1, 72])
    w_gate = din("w_gate", [NE, DM, 512])
    w_up = din("w_up", [NE, DM, 512])
    w_down = din("w_down", [NE, 512, DM])
    btab = din("btab", [NCLS, 128, 1024])
    consts = din("consts", [128, 768])
    PBd = din("PB", [128, NPB])
    PAd = din("PA", [128, NPA])
    out = nc.dram_tensor("out", [NT, DM], F32, kind="ExternalOutput").ap()
    uT_d = dscr("uT_d", [512, NT], BF16)
    attT_d = dscr("attT_d", [512, NT], BF16)
    hbuf = dscr("hbuf", [NT, DM], F32)
    xbuf = dscr("xbuf", [NE * CAP, DM], BF16)
    obuf = dscr("obuf", [NE * CAP, DM], BF16)
    tapo = None
    if tap is not None:
        tapo = nc.dram_tensor("tap", list(tap[1]), tap[2], kind="ExternalOutput").ap()

    S = Sched(nc)
    es = ExitStack()
    ARENA_BYTES = 206 * 1024
    arena_t = es.enter_context(nc.sbuf_tensor("arena", [128, ARENA_BYTES // 4], F32))
    A = Arena(arena_t, ARENA_BYTES)
    ps = [es.enter_context(nc.psum_tensor("ps%d" % i, [128, 512], F32)) for i in range(8)]
    PB_ = [Buf("ps%d" % i) for i in range(8)]
    sems = {e: es.enter_context(nc.semaphore("s_" + e)) for e in ENGINES}
    for q_, n_ in DMA_POOL.items():
        for i in range(n_):
            sems[("dma", q_, i)] = es.enter_context(nc.semaphore("d_%s%d" % (q_, i)))

    def MM(o, lhsT, rhs, start, stop, R, W):
        return S.op("pe", lambda e: e.matmul(o, lhsT=lhsT, rhs=rhs, start=start, stop=stop,
                                             skip_group_check=True), R, W)

    def TR(o, in_, ident, R, W):
        return S.op("pe", lambda e: e.transpose(o, in_, ident), R, W)

    def ACTF(o, in_, func, R, W, scale=1.0, bias=0.0):
        return S.op("act", lambda e: e.activation(out=o, in_=in_, func=func, bias=bias, scale=scale), R, W)

    def CP(eng, o, in_, R, W):
        if eng == "act":
            return S.op("act", lambda e: e.copy(out=o, in_=in_), R, W)
        return S.op(eng, lambda e: e.tensor_copy(out=o, in_=in_), R, W)

    def TT(eng, o, in0, in1, op, R, W):
        return S.op(eng, lambda e: e.tensor_tensor(out=o, in0=in0, in1=in1, op=op), R, W)

    def TS(eng, o, in0, s1, op0, R, W, s2=None, op1=None):
        if op1 is None:
            return S.op(eng, lambda e: e.tensor_scalar(out=o, in0=in0, scalar1=s1, scalar2=None, op0=op0), R, W)
        return S.op(eng, lambda e: e.tensor_scalar(out=o, in0=in0, scalar1=s1, scalar2=s2, op0=op0, op1=op1), R, W)

    def STT(eng, o, in0, scalar, in1, op0, op1, R, W):
        return S.op(eng, lambda e: e.scalar_tensor_tensor(out=o, in0=in0, scalar=scalar, in1=in1,
                                                          op0=op0, op1=op1), R, W)

    def MEMSET(eng, o, val, W):
        return S.op(eng, lambda e: e.memset(o, val), (), W)

    def DMA(q, o, in_, R, W, stream):
        return S.op(q, lambda e: e.dma_start(out=o, in_=in_), R, W, dma=stream)

    rr = {"n": 0}

    def alt():
        rr["n"] += 1
        return "act" if rr["n"] % 2 else "dve"

    out_writes = []

    cst = A.alloc(768, F32)
    cstb = A.alloc(640, BF16)
    Bcst, Bcstb = Buf("cst"), Buf("cstb")
    DMA("sp", cst, consts, [], [Bcst], 1)
    CP("act", cstb[:, 0:384], cst[:, 0:384], [Bcst], [Bcstb])
    CP("act", cstb[:, 384:640], cst[:, 512:768], [Bcst], [Bcstb])
    onesz = [cstb[:, 384:512], cstb[:, 512:640]]
    identf, identb = cst[:, 0:128], cstb[:, 0:128]
    ltrib, onesb = cstb[:, 128:256], cstb[:, 256:384]
    eC, maskcol, mask3 = cst[:, 384:448], cst[:, 448:450], cst[:, 450:452]
    gates_all = A.alloc(128, F32).rearrange("p (t k) -> p t k", k=2)
    dest_all = A.alloc(128, I32).rearrange("p (t k) -> p t k", k=2)
    Bgates, Bdest = Buf("gates"), Buf("dest")
    runc = A.alloc(64, F32)
    Brun = Buf("run")
    MEMSET("dve", runc, 0.0, [Brun])
    persist = A.off

    w_in_b = A.alloc(8 * 2048, BF16).rearrange("p (k n) -> p k n", k=8)
    Bwin = Buf("w_in")
    xTb = [A.alloc(8 * 512, BF16).rearrange("p (k n) -> p k n", k=8) for _ in range(2)]
    BxT = [Buf("xT0"), Buf("xT1")]
    NQ, NK = 12, 12
    qz = [A.alloc(4 * NQ * 128, BF16).rearrange("p (f n) -> p f n", f=4) for _ in range(2)]
    kT = A.alloc(4 * NK * 128, BF16).rearrange("p (f n) -> p f n", f=4)
    vz = [A.alloc(NK * 512, BF16).rearrange("p (s i c) -> p s i c", s=NK, i=4) for _ in range(2)]
    Bq = [Buf("q%d" % i) for i in range(NQ)]
    Bk = [Buf("k%d" % i) for i in range(NK)]
    Bv = [Buf("v%d" % i) for i in range(NK)]
    for par in range(2):
        MEMSET("pool", qz[par], 0.0, Bq)
        MEMSET("pool", vz[par], 0.0, Bv)
    maskt = A.alloc(NCLS * 1024, BF16).rearrange("p (c n) -> p c n", c=NCLS)
    Bmask = Buf("mask")
    mst = [A.alloc(1024, F32) for _ in range(2)]
    Bmst = [Buf("mst0"), Buf("mst1")]
    ust = [A.alloc(4 * 512, BF16).rearrange("p (f n) -> p f n", f=4) for _ in range(2)]
    Bust = [Buf("ust0"), Buf("ust1")]
    NEB = 5
    Eb = [A.alloc(512, BF16) for _ in range(NEB)]
    BE = [Buf("E%d" % i) for i in range(NEB)]
    recs = [A.alloc(512, F32) for _ in range(2)]
    Brecs = [Buf(), Buf()]
    attb = [A.alloc(512, BF16) for _ in range(2)]
    Batt = [Buf("att0"), Buf("att1")]
    Bdr_u, Bdr_att = Buf("uT_d"), Buf("attT_d")

    w_in_v = w_in.rearrange("(k p) n -> p k n", p=128)
    for kc in range(8):
        DMA("pool", w_in_b[:, kc, :], w_in_v[:, kc, :], [], [Bwin], 0)
    for c in range(NCLS):
        DMA("sp", mst[c % 2], btab[c], [], [Bmst[c % 2]], 1)
        ACTF(maskt[:, c, :], mst[c % 2], AF.Exp, [Bmst[c % 2]], [Bmask])

    xT_v = xT.rearrange("(k p) t -> p k t", p=128)
    uT_dv = uT_d.rearrange("(f p) t -> p f t", p=128)
    attT_dv = attT_d.rearrange("(f p) t -> p f t", p=128)
    cnt = {"sb": 0, "e": 0, "a": 0}

    def tile_units(j):
        if j <= 1:
            kcs = list(range(4))
            cls = [5 + 4 * j + jc for jc in kcs]
        elif j >= 62:
            kcs = list(range(60, 64))
            cls = [13 + 4 * (j - 62) + (jc - 60) for jc in kcs]
        else:
            kcs = list(range(j - 2, j + 3))
            cls = list(range(5))
        us = []
        for ci, jc in enumerate(kcs):
            for par in range(2):
                us.append({"j": j, "ks": jc % NK, "cls": cls[ci], "par": par,
                           "first": ci == 0 and par == 0, "last": ci == len(kcs) - 1 and par == 1})
        return us

    def st12(u):
        sb = 2 + cnt["sb"] % 4
        cnt["sb"] += 1
        ei = cnt["e"] % NEB
        cnt["e"] += 1
        u["ei"] = ei
        qs, ks, par = u["j"] % NQ, u["ks"], u["par"]
        for i in range(4):
            MM(ps[sb][:, i * 128:(i + 1) * 128], kT[:, i, ks * 128:(ks + 1) * 128],
               qz[par][:, i, qs * 128:(qs + 1) * 128], True, True, [Bk[ks], Bq[qs]], [PB_[sb]])
        ACTF(Eb[ei], ps[sb][:, :], AF.Exp, [PB_[sb]], [BE[ei]], scale=0.125)
        mv = maskt[:, u["cls"], :].rearrange("p (i t q) -> p i t q", i=4, t=2)[:, :, par, :]
        ev = Eb[ei].rearrange("p (i q) -> p i q", i=4)
        TT("dve", ev, ev, mv, ALU.mult, [BE[ei], Bmask], [BE[ei]])

    def st3(u):
        ei, ks, par, j = u["ei"], u["ks"], u["par"], u["j"]
        po, pm = 6, 7
        for i in range(4):
            MM(ps[po][:, i * 128:(i + 1) * 128], vz[par][:, ks, i, :], Eb[ei][:, i * 128:(i + 1) * 128],
               u["first"] and i == 0, u["last"], [Bv[ks], BE[ei]], [PB_[po]])
        MM(ps[pm][:, :], onesz[par], Eb[ei], u["first"], u["last"], [Bcstb, BE[ei]], [PB_[pm]])
        if u["last"]:
            recj = recs[j % 2]
            S.op("dve", lambda e: e.reciprocal(out=recj, in_=ps[pm][:, :]), [PB_[pm]], [Brecs[j % 2]])
            ai = cnt["a"] % 2
            cnt["a"] += 1
            TT("dve", attb[ai], ps[po][:, :], recj, ALU.mult, [PB_[po], Brecs[j % 2]], [Batt[ai]])
            DMA("sp", attT_dv[:, :, j * 128:(j + 1) * 128], attb[ai].rearrange("p (f n) -> p f n", f=4),
                [Batt[ai]], [Bdr_att], 2)

    SKEW = 3

    def attn_tiles(js):
        us = [u for j in js for u in tile_units(j)]
        for t in range(len(us) + SKEW):
            if t < len(us):
                st12(us[t])
            if t >= SKEW:
                st3(us[t - SKEW])

    pcnt = {"n": 0}

    def inproj(b):
        xb, Bx = xTb[b % 2], BxT[b % 2]
        DMA("pool", xb, xT_v[:, :, b * 512:(b + 1) * 512], [], [Bx], 0)
        for fc in range(12):
            col0 = fc * 128 if fc < 8 else 1536 + (fc - 8) * 128
            bank = pcnt["n"] % 2
            pcnt["n"] += 1
            for kc in range(8):
                MM(ps[bank][:, :], w_in_b[:, kc, col0:col0 + 128], xb[:, kc, :], kc == 0, kc == 7,
                   [Bwin, Bx], [PB_[bank]])
            if fc < 4:
                s0 = (4 * b) % NQ
                W = [Bq[s0 + i] for i in range(4)]
                CP("act", qz[0][0:64, fc, s0 * 128:s0 * 128 + 512], ps[bank][0:64, :], [PB_[bank]], W)
                CP("dve", qz[1][64:128, fc, s0 * 128:s0 * 128 + 512], ps[bank][64:128, :], [PB_[bank]], W)
                continue
            elif fc < 8:
                s0 = (4 * b) % NK
                dst, W = kT[:, fc - 4, s0 * 128:s0 * 128 + 512], [Bk[s0 + i] for i in range(4)]
            else:
                dst, W = ust[b % 2][:, fc - 8, :], [Bust[b % 2]]
            CP(alt(), dst, ps[bank][:, :], [PB_[bank]], W)
        DMA("sp", uT_dv[:, :, b * 512:(b + 1) * 512], ust[b % 2], [Bust[b % 2]], [Bdr_u], 2)
        for i in range(4):
            bank = pcnt["n"] % 2
            pcnt["n"] += 1
            for kc in range(8):
                MM(ps[bank][:, :], xb[:, kc, i * 128:(i + 1) * 128], w_in_b[:, kc, 1024:1536], kc == 0, kc == 7,
                   [Bwin, Bx], [PB_[bank]])
            sl = (4 * b + i) % NK
            pv4 = ps[bank][:, :].rearrange("p (i t d) -> p i t d", i=4, t=2)
            CP("act", vz[0][:, sl, :, 0:64], pv4[:, :, 0, :], [PB_[bank]], [Bv[sl]])
            CP("dve", vz[1][:, sl, :, 64:128], pv4[:, :, 1, :], [PB_[bank]], [Bv[sl]])

    inproj(0)
    for b in range(16):
        lanes = [[], []]
        if b < 15:
            S.lane = lanes[0]
            inproj(b + 1)
        S.lane = lanes[1]
        lo = max(0, 4 * b - 2)
        hi = 4 * b + 2 if b < 15 else 64
        attn_tiles(list(range(lo, hi)))
        S.lane = None
        na, nb_ = len(lanes[0]), len(lanes[1])
        S.run_lanes(lanes, chunk=[1, max(1, nb_ // max(na, 1))])
    S.barrier()
    A.off = persist

    def finish():
        S.op("sp", lambda e: e.nop(), extra=out_writes)
        S.emit(sems)
        es.close()
        return nc

    def do_tap(src, Bsrc):
        out_writes.append(DMA("sp", tapo, src, [Bsrc], [], 9))

    if stop == 1:
        do_tap({"uT_d": uT_d, "attT_d": attT_d}[tap[0]], {"uT_d": Bdr_u, "attT_d": Bdr_att}[tap[0]])
        return finish()

    sg_d = dscr("sg_d", [512, NT], BF16)
    sg_dv = sg_d.rearrange("(f p) t -> p f t", p=128)
    Bdr_sg = Buf("sg_d")
    PBt = A.alloc(NPB, F32)
    BPB, BPA, Bglob = Buf("PB"), Buf("PA"), Buf("glob")

    def v4(ap):
        return ap.rearrange("p (d q c) -> p d q c", d=2, q=16)

    BBr, BBi, CBr, CBi = (v4(PBt[:, 96:608]), v4(PBt[:, 608:1120]), v4(PBt[:, 1120:1632]), v4(PBt[:, 1632:2144]))
    DAc = A.alloc(4, F32)
    PW = A.alloc(17 * 64, F32).rearrange("p (k r n) -> p k r n", k=17, r=2)
    SC = A.alloc(9 * 3 * 32, F32).rearrange("p (l k c n) -> p l k c n", l=1, k=9, c=3)
    BbB = A.alloc(2 * 512, F32).rearrange("p (r d q c) -> p r d q c", r=2, d=2, q=16)
    BbA = A.alloc(2 * 512, F32).rearrange("p (r n) -> p r n", r=2)
    lamA = A.alloc(2 * 512, F32).rearrange("p (r n) -> p r n", r=2)
    p2mark = A.off
    PAt = A.alloc(NPA, F32)
    DMA("sp", PBt, PBd, [], [BPB], 1)
    DMA("sp", PAt, PAd, [], [BPA], 1)
    arB, aiB, ldB = PBt[:, 0:32], PBt[:, 32:64], PBt[:, 64:96]
    arA, aiA, ldA = PAt[:, 0:512], PAt[:, 512:1024], PAt[:, 1024:1536]
    BAr, BAi = PAt[:, 1536:2048], PAt[:, 2048:2560]

    def cmul(eng, o_r, o_i, a_r, a_i, b_r, b_i, t1, t2, R, W):
        TT(eng, t1, a_r, b_r, ALU.mult, R, W)
        TT(eng, t2, a_i, b_i, ALU.mult, R, W)
        TT(eng, o_r, t1, t2, ALU.subtract, R, W)
        TT(eng, t1, a_r, b_i, ALU.mult, R, W)
        TT(eng, t2, a_i, b_r, ALU.mult, R, W)
        TT(eng, o_i, t1, t2, ALU.add, R, W)

    def lam_z(ar, ai, ld, n, Bin, lr, li, zr, zi):
        R, W = [Bin, Bglob], [Bglob]
        dt, mag, th, kk, r, sn, cs, tmp = [A.alloc(n, F32) for _ in range(8)]
        ACTF(dt, ld, AF.Exp, R, W)
        TT("dve", mag, ar, dt, ALU.mult, R, W)
        ACTF(mag, mag, AF.Exp, R, W)
        TT("dve", th, ai, dt, ALU.mult, R, W)
        TS("dve", kk, th, float(1 / (2 * np.pi)), ALU.mult, R, W, s2=MAGIC, op1=ALU.add)
        TS("dve", kk, kk, -MAGIC, ALU.add, R, W)
        STT("dve", r, kk, -C1, th, ALU.mult, ALU.add, R, W)
        STT("dve", r, kk, -C2, r, ALU.mult, ALU.add, R, W)
        TS("dve", r, r, float(np.pi), ALU.min, R, W, s2=-float(np.pi), op1=ALU.max)
        ACTF(sn, r, AF.Sin, R, W)
        TS("dve", tmp, r, -1.0, ALU.mult, R, W)
        TT("dve", tmp, tmp, r, ALU.max, R, W)
        TS("dve", tmp, tmp, -1.0, ALU.mult, R, W, s2=float(np.pi / 2), op1=ALU.add)
        ACTF(cs, tmp, AF.Sin, R, W)
        TT("dve", lr, mag, cs, ALU.mult, R, W)
        TT("dve", li, mag, sn, ALU.mult, R, W)
        TT("dve", tmp, ar, ar, ALU.mult, R, W)
        TT("dve", kk, ai, ai, ALU.mult, R, W)
        TT("dve", tmp, tmp, kk, ALU.add, R, W)
        S.op("dve", lambda e: e.reciprocal(out=tmp, in_=tmp), R, W)
        TS("dve", r, lr, -1.0, ALU.add, R, W)
        TT("dve", kk, r, ar, ALU.mult, R, W)
        TT("dve", th, li, ai, ALU.mult, R, W)
        TT("dve", kk, kk, th, ALU.add, R, W)
        TT("dve", zr, kk, tmp, ALU.mult, R, W)
        TT("dve", kk, li, ar, ALU.mult, R, W)
        TT("dve", th, r, ai, ALU.mult, R, W)
        TT("dve", kk, kk, th, ALU.subtract, R, W)
        TT("dve", zi, kk, tmp, ALU.mult, R, W)

    RG, WG = [BPB, BPA, Bglob], [Bglob]
    CP("dve", DAc, PAt[:, 2560:2564], RG, WG)
    zB = [A.alloc(32, F32) for _ in range(2)]
    lam_z(arB, aiB, ldB, 32, BPB, PW[:, 1, 0, :], PW[:, 1, 1, :], zB[0], zB[1])
    t1b, t2b = A.alloc(512, F32), A.alloc(512, F32)
    MEMSET("dve", PW[:, 0, 0, :], 1.0, WG)
    MEMSET("dve", PW[:, 0, 1, :], 0.0, WG)
    for k in range(2, 17):
        cmul("dve", PW[:, k, 0, :], PW[:, k, 1, :], PW[:, k - 1, 0, :], PW[:, k - 1, 1, :],
             PW[:, 1, 0, :], PW[:, 1, 1, :], t1b[:, 0:32], t2b[:, 0:32], RG, WG)
    CP("dve", SC[:, 0, 0, 0:2, :], PW[:, 16, 0:2, :], RG, WG)
    for k in range(1, 9):
        cmul("dve", SC[:, 0, k, 0, :], SC[:, 0, k, 1, :], SC[:, 0, k - 1, 0, :], SC[:, 0, k - 1, 1, :],
             SC[:, 0, k - 1, 0, :], SC[:, 0, k - 1, 1, :], t1b[:, 0:32], t2b[:, 0:32], RG, WG)
    TS("dve", SC[:, 0, :, 2, :], SC[:, 0, :, 1, :], -1.0, ALU.mult, RG, WG)

    def bc(ap, shape):
        return ap.to_broadcast(shape)

    zrb = bc(zB[0].unsqueeze(2), [128, 32, 16])
    zib = bc(zB[1].unsqueeze(2), [128, 32, 16])
    f3 = lambda ap: ap.rearrange("p d q c -> p (d q) c")
    f3o = lambda ap: ap.rearrange("p (n c) -> p n c", c=16)
    cmul("dve", f3(BbB[:, 0]), f3(BbB[:, 1]), zrb, zib, f3(BBr), f3(BBi), f3o(t1b), f3o(t2b), RG, WG)
    zA = [A.alloc(512, F32) for _ in range(2)]
    lam_z(arA, aiA, ldA, 512, BPA, lamA[:, 0, :], lamA[:, 1, :], zA[0], zA[1])
    cmul("dve", BbA[:, 0, :], BbA[:, 1, :], zA[0], zA[1], BAr, BAi, t1b, t2b, RG, WG)
    S.barrier()
    A.off = p2mark

    uTf = [A.alloc(NT, BF16)]
    uds = A.alloc(NT, BF16).rearrange("p (s j) -> p s j", s=16)
    Buds = Buf("uds")
    BuT = [[Buf() for _ in range(16)]]
    TXs = [A.alloc(16 * 2 * 128, BF16).rearrange("p (k r g c) -> p k r g c", k=16, r=2, g=2) for _ in range(2)]
    TX3s = [A.alloc(16 * 2 * 128, BF16).rearrange("p (k r g c) -> p k r g c", k=16, r=2, g=2) for _ in range(2)]
    BTXs = [Buf(), Buf()]
    Wb = A.alloc(2 * 2 * 4 * 17 * 16, BF16).rearrange("p (r d q t c) -> p r d q t c", r=2, d=2, q=4, t=17)
    TC = A.alloc(2 * 4 * 2 * 512, BF16).rearrange("p (d q r n) -> p d q r n", d=2, q=4, r=2)
    Bp = A.alloc(2 * 8 * 2 * 128, BF16).rearrange("p (d g r n) -> p d g r n", d=2, g=8, r=2)
    FIRt = A.alloc(2 * 16 * 128, BF16).rearrange("p (d t n) -> p d t n", d=2, t=16)
    Sf = A.alloc(2 * 4 * 2 * 514, BF16).rearrange("p (d q r n) -> p d q r n", d=2, q=4, r=2)
    BTX, BWf, BWb, BTC, BBp, BFIR, BEp = [Buf() for _ in range(7)]
    BSf = [[Buf() for _ in range(4)] for _ in range(2)]
    MEMSET("pool", TC, 0.0, [BTC])
    MEMSET("pool", Bp, 0.0, [BBp])
    MEMSET("pool", Sf, 0.0, [b for r in BSf for b in r])
    Xs = [A.alloc(1024, F32).rearrange("p (r n) -> p r n", r=2) for _ in range(4)]
    BXs = [Buf() for _ in range(4)]
    scr0 = A.off
    EpA = A.alloc(16 * 128, F32).rearrange("p (k r c) -> p k r c", k=16, r=2)
    VA = A.alloc(16 * 128, F32).rearrange("p (k r c) -> p k r c", k=16, r=2)
    tA = A.alloc(16 * 64, F32)
    Wf = A.alloc(2 * 4 * 17 * 16, F32).rearrange("p (r q t c) -> p r q t c", r=2, q=4, t=17)
    lpw = A.alloc(3 * 128, F32).rearrange("p (m r c) -> p m r c", m=3, r=2)
    tW = EpA.rearrange("p k r c -> p (k r c)")[:, 0:4 * 17 * 16]
    tW2 = VA.rearrange("p k r c -> p (k r c)")[:, 0:4 * 17 * 16]
    scr1 = A.off
    A.off = scr0
    Yi = [A.alloc(16 * 128, BF16).rearrange("p (k n) -> p k n", k=16) for _ in range(2)]
    BYi = [Buf(), Buf()]
    Yt = A.alloc(2048, F32)
    BYt = Buf()
    ytm = [A.alloc(512, F32) for _ in range(2)]
    gt1 = [A.alloc(512, F32) for _ in range(2)]
    sgo = [A.alloc(512, BF16) for _ in range(2)]
    Bytm = [Buf(), Buf()]
    A.off = max(A.off, scr1)
    lamA4 = lamA.rearrange("p r (d f c) -> p r d f c", d=2, f=4)
    BbA4 = BbA.rearrange("p r (d f c) -> p r d f c", d=2, f=4)

    def cmac(eng, dst, src, l, k, col, R, W):
        a, b, nb = SC[:, l, k, 0, col:col + 1], SC[:, l, k, 1, col:col + 1], SC[:, l, k, 2, col:col + 1]
        STT(eng, dst[:, 0, :], src[:, 0, :], a, dst[:, 0, :], ALU.mult, ALU.add, R, W)
        STT(eng, dst[:, 0, :], src[:, 1, :], nb, dst[:, 0, :], ALU.mult, ALU.add, R, W)
        STT(eng, dst[:, 1, :], src[:, 0, :], b, dst[:, 1, :], ALU.mult, ALU.add, R, W)
        STT(eng, dst[:, 1, :], src[:, 1, :], a, dst[:, 1, :], ALU.mult, ALU.add, R, W)

    def scan(eng, X, n, l, rev, col, R, W):
        nlev = n.bit_length() - 1
        for k in range(nlev):
            st, h = 2 << k, 1 << k
            if not rev:
                cmac(eng, X[:, :, st - 1::st], X[:, :, h - 1::st], 0, k, col, R, W)
            else:
                cmac(eng, X[:, :, 0::st], X[:, :, h::st], 0, k, col, R, W)
        for k in range(nlev - 2, -1, -1):
            st, h = 2 << k, 1 << k
            if not rev:
                cmac(eng, X[:, :, st - 1 + h::st], X[:, :, st - 1:n - st:st], 0, k, col, R, W)
            else:
                cmac(eng, X[:, :, h:n - st:st], X[:, :, st::st], 0, k, col, R, W)

    pdn = {"n": 0, "pin": 0, "pt": 0, "pf": 0}
    for f in range(4):
        uf, Buf_f = uTf[0], BuT[0]
        DMA("sp", uf, uT_dv[:, f, :], [Bdr_u], Buf_f, 1)
        ufv = uf.rearrange("p (j t) -> p t j", t=16)
        CP("act", uds[:, 0:8, :], ufv[:, 0:8, :], Buf_f, [Buds])
        CP("dve", uds[:, 8:16, :], ufv[:, 8:16, :], Buf_f, [Buds])
        for d in range(2):
            Cr = bc(CBr[:, d, 4 * f:4 * f + 4, :].unsqueeze(2), [128, 4, 17, 16])
            Ci = bc(CBi[:, d, 4 * f:4 * f + 4, :].unsqueeze(2), [128, 4, 17, 16])
            pr = bc(PW[:, :, 0, d * 16 + 4 * f:d * 16 + 4 * f + 4].rearrange("p t q -> p q t").unsqueeze(3),
                    [128, 4, 17, 16])
            pi_ = bc(PW[:, :, 1, d * 16 + 4 * f:d * 16 + 4 * f + 4].rearrange("p t q -> p q t").unsqueeze(3),
                     [128, 4, 17, 16])
            t1v = tW.rearrange("p (q t c) -> p q t c", q=4, t=17)
            t2v = tW2.rearrange("p (q t c) -> p q t c", q=4, t=17)
            cmul("dve", Wf[:, 0], Wf[:, 1], Cr, Ci, pr, pi_, t1v, t2v, [BPB, Bglob, BWf], [BWf])
            for ri in range(2):
                CP("act", Wb[:, ri, d].rearrange("p q t c -> p (q t c)"), Wf[:, ri].rearrange("p q t c -> p (q t c)"),
                   [BWf], [BWb])
            for g in range(2):
                for ri in range(2):
                    o = TC[64 * g:64 * g + 64, d, :, ri, :].rearrange("p q (k g c) -> p q k g c", k=16, g=2)[:, :, :, g, :]
                    i_ = Wf[64 * g:64 * g + 64, ri, :, 1:17, :]
                    ACTF(o, i_, AF.Copy, [BWf], [BTC], scale=1.0 if ri == 0 else -1.0)
        for g8 in range(8):
            g, qq = g8 % 2, g8 // 2
            for d in range(2):
                for ri in range(2):
                    ACTF(Bp[64 * g:64 * g + 64, d, g8, ri, g8 * 16:(g8 + 1) * 16],
                         BbB[64 * g:64 * g + 64, ri, d, 4 * f + qq, :], AF.Copy, [Bglob], [BBp],
                         scale=1.0 if ri == 0 else -1.0)
        for d in range(2):
            for bk in range(4):
                for g8 in range(8):
                    for ri in range(2):
                        MM(ps[bk][:, g8 * 64:(g8 + 1) * 64], Bp[:, d, g8, ri, :],
                           Wb[:, ri, d, g8 // 2, 4 * bk:4 * bk + 4, :],
                           g8 == 0 and ri == 0, g8 == 7 and ri == 1, [BBp, BWb], [PB_[bk]])
                ov = ps[bk][:, :].rearrange("p (g t c) -> p t g c", g=8, t=4)
                CP(alt(), FIRt[:, d, 4 * bk:4 * bk + 4, :].rearrange("p t (g c) -> p t g c", g=8), ov,
                   [PB_[bk]], [BFIR])
                if d == 0 and bk == 0:
                    STT("dve", FIRt[:, 0, 0, :].rearrange("p (g c) -> p g c", g=8),
                        identf.rearrange("p (g c) -> p g c", g=8), DAc[:, f:f + 1],
                        ps[0][:, :].rearrange("p (g t c) -> p g t c", g=8, t=4)[:, :, 0, :], ALU.mult, ALU.add,
                        [PB_[0], Bcst, Bglob], [BFIR])
        def tx_gen(d):
            TX, TX3, BTX = TXs[d], TX3s[d], BTXs[d]
            RA, WA = [Bglob, BEp], [BEp]
            lr_ = lamA4[:, 0, d, f, :]
            li_ = lamA4[:, 1, d, f, :]
            MEMSET("dve", EpA[:, 0, 0, :], 1.0, WA)
            MEMSET("dve", EpA[:, 0, 1, :], 0.0, WA)
            CP("dve", EpA[:, 1, 0, :], lr_, RA, WA)
            CP("dve", EpA[:, 1, 1, :], li_, RA, WA)
            pr_, pi_ = lr_, li_
            for lv, m in enumerate((2, 4, 8)):
                cmul("dve", lpw[:, lv, 0, :], lpw[:, lv, 1, :], pr_, pi_, pr_, pi_, tA[:, 0:64], tA[:, 64:128], RA, WA)
                pr_, pi_ = lpw[:, lv, 0, :], lpw[:, lv, 1, :]
                t1 = tA[:, 0:m * 64].rearrange("p (k c) -> p k c", k=m)
                t2 = tA[:, 512:512 + m * 64].rearrange("p (k c) -> p k c", k=m)
                cmul("dve", EpA[:, m:2 * m, 0, :], EpA[:, m:2 * m, 1, :], EpA[:, 0:m, 0, :], EpA[:, 0:m, 1, :],
                     bc(pr_.unsqueeze(1), [128, m, 64]), bc(pi_.unsqueeze(1), [128, m, 64]), t1, t2, RA, WA)
            Br_ = bc(BbA4[:, 0, d, f, :].unsqueeze(1), [128, 16, 64])
            Bi_ = bc(BbA4[:, 1, d, f, :].unsqueeze(1), [128, 16, 64])
            tAv = tA.rearrange("p (k c) -> p k c", k=16)
            TT("dve", VA[:, :, 0, :], EpA[:, :, 0, :], Br_, ALU.mult, RA, WA)
            TT("dve", tAv, EpA[:, :, 1, :], Bi_, ALU.mult, RA, WA)
            TT("dve", VA[:, :, 0, :], VA[:, :, 0, :], tAv, ALU.subtract, RA, WA)
            TT("dve", VA[:, :, 1, :], EpA[:, :, 0, :], Bi_, ALU.mult, RA, WA)
            TT("dve", tAv, EpA[:, :, 1, :], Br_, ALU.mult, RA, WA)
            TT("dve", VA[:, :, 1, :], VA[:, :, 1, :], tAv, ALU.add, RA, WA)
            for ri in range(2):
                for g in range(2):
                    TS("dve", TX[:, :, ri, g, :], VA[:, :, ri, :], maskcol[:, g:g + 1], ALU.mult, [BEp, Bcst], [BTX])
                    ACTF(TX3[64:128, :, ri, g, :], VA[64:128, :, ri, :], AF.Identity, [BEp, Bcst], [BTX],
                         scale=mask3[64:128, g:g + 1])

        def x_mm(d):
            TX, TX3, BTX = TXs[d], TX3s[d], BTXs[d]
            for qq in range(4):
                for ri in range(2):
                    bank = 2 * qq + ri
                    for s_ in range(16):
                        k = 15 - s_ if d == 0 else s_
                        if qq < 3:
                            lt = TX[32 * qq:32 * qq + 32, k, ri, :, :].rearrange("p g c -> p (g c)")
                            rh = uds[32 * qq:32 * qq + 32, s_, :]
                        else:
                            lt = TX3[64:128, k, ri, :, :].rearrange("p g c -> p (g c)")
                            rh = uds[64:128, s_, :]
                        MM(ps[bank][:, :], lt, rh, s_ == 0, s_ == 15, [BTX, Buds], [PB_[bank]])

        def x_evac(d):
            for qq in range(4):
                for ri in range(2):
                    bank = 2 * qq + ri
                    CP(alt(), Xs[qq][:, ri, :], ps[bank][:, :], [PB_[bank]], [BXs[qq]])

        def scans(d):
            lanes = [[] for _ in range(4)]
            for qq in range(4):
                S.lane = lanes[qq]
                col = d * 16 + 4 * f + qq
                scan("dve", Xs[qq], 512, 0, d == 1, col, [BXs[qq], Bglob], [BXs[qq]])
                CP("act", Sf[:, d, qq, :, 1:513], Xs[qq], [BXs[qq]], [BSf[d][qq]])
            S.lane = None
            S.run_lanes(lanes)

        tx_gen(0)
        x_mm(0)
        x_evac(0)
        tx_gen(1)
        x_mm(1)
        scans(0)
        x_evac(1)
        scans(1)
        S.barrier()
        for ct in range(4):
            for d in range(2):
                off = 128 * ct if d == 0 else 128 * ct + 2
                for qq in range(4):
                    pin = 4 + pdn["pin"] % 2
                    pdn["pin"] += 1
                    for ri in range(2):
                        MM(ps[pin][:, :], Sf[:, d, qq, ri, off:off + 128], TC[:, d, qq, ri, :], ri == 0, ri == 1,
                           [BSf[d][qq], BTC], [PB_[pin]])
                    CP(alt(), Yi[d][:, :, 32 * qq:32 * qq + 32], ps[pin][:, :].rearrange("p (k n) -> p k n", k=16),
                       [PB_[pin]], [BYi[d]])
            for tg in range(4):
                pt = 6 + pdn["pt"] % 2
                pdn["pt"] += 1
                for t4 in range(4):
                    tau = 4 * tg + t4
                    MM(ps[pt][:, t4 * 128:(t4 + 1) * 128], Yi[0][:, tau, :], identb, True, False,
                       [BYi[0], Bcstb], [PB_[pt]])
                    MM(ps[pt][:, t4 * 128:(t4 + 1) * 128], Yi[1][:, 15 - tau, :], identb, False, True,
                       [BYi[1], Bcstb], [PB_[pt]])
                CP(alt(), Yt.rearrange("p (tb s jl) -> p s tb jl", tb=4, s=16)[:, 4 * tg:4 * tg + 4, :, :],
                   ps[pt][:, :].rearrange("p (t tb jl) -> p t tb jl", t=4, tb=4), [PB_[pt]], [BYt])
            for tb in range(4):
                blk = 4 * ct + tb
                pf = pdn["pf"] % 2
                pdn["pf"] += 1
                ub = uds[:, :, 32 * blk:32 * blk + 32]
                RB = [BFIR, Buds]
                MM(ps[pf][:, :], FIRt[:, 0, 0, :], ub, True, False, RB, [PB_[pf]])
                for tau in range(1, 16):
                    MM(ps[pf][:, tau * 32:512], FIRt[:, 0, tau, :], ub[:, 0:16 - tau, :], False, False, RB, [PB_[pf]])
                for tau in range(16):
                    MM(ps[pf][:, 0:(16 - tau) * 32], FIRt[:, 1, tau, :], ub[:, tau:16, :], False, tau == 15, RB,
                       [PB_[pf]])
                y, g1, so, By = ytm[pf], gt1[pf], sgo[pf], Bytm[pf]
                TT("dve", y, ps[pf][:, :], Yt[:, tb * 512:(tb + 1) * 512], ALU.add, [PB_[pf], BYt], [By])
                ACTF(g1, y, AF.Square, [By], [By])
                TS("dve", g1, g1, 0.044715, ALU.mult, [By], [By], s2=1.0, op1=ALU.add)
                TT("dve", g1, g1, y, ALU.mult, [By], [By])
                ACTF(g1, g1, AF.Sigmoid, [By], [By], scale=1.5957691216057308)
                TT("dve", so.rearrange("p (jl s) -> p s jl", s=16), y.rearrange("p (s jl) -> p s jl", s=16),
                   g1.rearrange("p (s jl) -> p s jl", s=16), ALU.mult, [By], [By])
                DMA("sp", sg_dv[:, f, blk * 512:(blk + 1) * 512], so, [By], [Bdr_sg], 2)
        S.barrier()
    A.off = persist

    if stop == 2:
        do_tap(sg_d, Bdr_sg)
        return finish()

    AX = mybir.AxisListType.X
    lnt = A.alloc(4 * DM, F32).rearrange("p (i n) -> p i n", i=4)
    Bln = Buf("ln")
    for i in range(4):
        DMA("sp", lnt[:, i, :], lnp[i].partition_broadcast(128), [], [Bln], 1)
    epsc = A.alloc(1, F32)
    MEMSET("dve", epsc, EPS, [Bln])
    persist2 = A.off
    wglu_b = A.alloc(4 * 512, BF16).rearrange("p (k n) -> p k n", k=4)
    bglu = A.alloc(4, F32)
    wout_b = A.alloc(8 * DM, BF16).rearrange("p (k n) -> p k n", k=8)
    wr_f = A.alloc(8 * 72, F32).rearrange("p (k n) -> p k n", k=8)
    brb = A.alloc(72, F32)
    Bw3 = Buf("w3")
    DMA("pool", wglu_b, w_glu.rearrange("(k p) n -> p k n", p=128), [], [Bw3], 0)
    for kc in range(8):
        DMA("pool", wout_b[:, kc, :], w_out.rearrange("(k p) n -> p k n", p=128)[:, kc, :], [], [Bw3], 0)
    DMA("sp", bglu, b_glu, [], [Bw3], 1)
    DMA("sp", wr_f, w_r.rearrange("(k p) n -> p k n", p=128), [], [Bw3], 1)
    DMA("sp", brb, b_r[0].partition_broadcast(128), [], [Bw3], 1)
    sgb = [A.alloc(4 * 512, BF16).rearrange("p (k n) -> p k n", k=4) for _ in range(2)]
    atb = [A.alloc(4 * 512, BF16).rearrange("p (k n) -> p k n", k=4) for _ in range(2)]
    sTg = [A.alloc(4 * 512, BF16).rearrange("p (k n) -> p k n", k=4) for _ in range(2)]
    gtb = [A.alloc(512, BF16) for _ in range(2)]
    Bsgb, Batb, BsTg, Bgtb = ([Buf(), Buf()] for _ in range(4))
    NL = 4
    xt = [A.alloc(DM, F32) for _ in range(NL)]
    rt = [A.alloc(DM, F32) for _ in range(NL)]
    hbt2 = [[A.alloc(DM, BF16) for _ in range(NL)] for _ in range(2)]
    lnscr = [A.alloc(16, F32) for _ in range(NL)]
    Blns = [Buf() for _ in range(NL)]
    hTs = [A.alloc(8 * 128, F32).rearrange("p (k n) -> p k n", k=8) for _ in range(NL)]
    sms = [A.alloc(512, F32) for _ in range(NL)]
    twbs = [A.alloc(64, BF16) for _ in range(NL)]
    Bxt, Brt, BhTs, Bsms = ([Buf() for _ in range(NL)] for _ in range(4))
    Bhbt2 = [[Buf() for _ in range(NL)] for _ in range(2)]
    Bhbuf, Bxbuf, Bobuf = Buf("hbuf"), Buf("xbuf"), Buf("obuf")

    def layer_norm(r, gi, R, W, scr, Bs):
        st, mv, sd, nb = scr[:, 0:12], scr[:, 12:14], scr[:, 14:15], scr[:, 15:16]
        S.op("dve", lambda e: e.bn_stats(out=st[:, 0:6], in_=r[:, 0:512]), R + [Bs], [Bs])
        S.op("dve", lambda e: e.bn_stats(out=st[:, 6:12], in_=r[:, 512:1024]), R + [Bs], [Bs])
        S.op("dve", lambda e: e.bn_aggr(out=mv, in_=st), [Bs], [Bs])
        ACTF(sd, mv[:, 1:2], AF.Sqrt, [Bs, Bln], [Bs], bias=epsc[:, 0:1])
        S.op("dve", lambda e: e.reciprocal(out=sd, in_=sd), [Bs], [Bs])
        STT("dve", nb, mv[:, 0:1], -1.0, sd, ALU.mult, ALU.mult, [Bs], [Bs])
        ACTF(r, r, AF.Identity, R + [Bs], W, scale=sd, bias=nb)
        TT("dve", r, r, lnt[:, gi, :], ALU.mult, R + [Bln], W)
        TT("dve", r, r, lnt[:, gi + 1, :], ALU.add, R + [Bln], W)

    def p3_f1a(b, i):
        bi, tt = b % 2, 4 * b + i
        for half in range(2):
            bank = half
            for kc in range(8):
                lt = atb[bi][:, kc, i * 128:(i + 1) * 128] if kc < 4 else sTg[bi][:, kc - 4, i * 128:(i + 1) * 128]
                MM(ps[bank][:, :], lt, wout_b[:, kc, half * 512:(half + 1) * 512], kc == 0, kc == 7,
                   [Batb[bi], BsTg[bi], Bw3], [PB_[bank]])
            STT("dve", rt[i][:, half * 512:(half + 1) * 512], xt[i][:, half * 512:(half + 1) * 512], ALPHA,
                ps[bank][:, :], ALU.mult, ALU.add, [Bxt[i], PB_[bank]], [Brt[i]])

    def p3_f1b(b, i):
        tt = 4 * b + i
        h = rt[i]
        layer_norm(h, 0, [Brt[i]], [Brt[i]], lnscr[i], Blns[i])
        DMA("sp", hbuf[tt * 128:(tt + 1) * 128, :], h, [Brt[i]], [Bhbuf], 2)
        CP("act", hbt2[b % 2][i], h, [Brt[i]], [Bhbt2[b % 2][i]])

    def p3_f2(b, i):
        h, hT, BhT, mb = rt[i], hTs[i], BhTs[i], 4 + i
        for kc in range(8):
            bank = 2 + kc // 4
            TR(ps[bank][:, (kc % 4) * 128:(kc % 4 + 1) * 128], h[:, kc * 128:(kc + 1) * 128], identf,
               [Brt[i], Bcst], [PB_[bank]])
        CP("act", hT[:, 0:4, :], ps[2][:, :].rearrange("p (k n) -> p k n", k=4), [PB_[2]], [BhT])
        CP("act", hT[:, 4:8, :], ps[3][:, :].rearrange("p (k n) -> p k n", k=4), [PB_[3]], [BhT])
        for kc in range(8):
            MM(ps[mb][:, 0:72], hT[:, kc, :], wr_f[:, kc, :], kc == 0, kc == 7, [BhT, Bw3], [PB_[mb]])

    def p3_router(b, i):
        tt = 4 * b + i
        sm, Bsm, twb, mb = sms[i], Bsms[i], twbs[i], 4 + i
        lg = sm[:, 0:72]
        el = sm[:, 8:72].rearrange("p (g e) -> p g e", g=8)
        gmax, ngm, gsum, gval = sm[:, 72:73], sm[:, 73:74], sm[:, 74:75], sm[:, 75:76]
        ge, ohg = sm[:, 80:88], sm[:, 88:96]
        prod = sm[:, 96:160].rearrange("p (g e) -> p g e", g=8)
        ein, oh1, e2, oh2 = sm[:, 160:168], sm[:, 168:176], sm[:, 176:184], sm[:, 184:192]
        m1, m2, dm, w1 = sm[:, 192:193], sm[:, 193:194], sm[:, 194:195], sm[:, 195:196]
        o64 = [sm[:, 200:264], sm[:, 264:328]]
        R_, W_ = [Bsm], [Bsm]
        TT("dve", lg, ps[mb][:, 0:72], brb, ALU.add, [PB_[mb], Bw3, Bsm], W_)
        S.op("dve", lambda e: e.reduce_max(out=gmax, in_=lg[:, 0:8], axis=AX), R_, W_)
        TS("dve", ngm, gmax, -1.0, ALU.mult, R_, W_)
        ACTF(ge, lg[:, 0:8], AF.Exp, R_, W_, bias=ngm)
        S.op("dve", lambda e: e.reduce_sum(out=gsum, in_=ge, axis=AX), R_, W_)
        S.op("dve", lambda e: e.reciprocal(out=gval, in_=gsum), R_, W_)
        TS("dve", ohg, lg[:, 0:8], gmax, ALU.is_equal, R_, W_)
        TT("dve", prod, el, bc(ohg.unsqueeze(2), [128, 8, 8]), ALU.mult, R_, W_)
        S.op("dve", lambda e: e.tensor_reduce(out=ein, in_=prod.rearrange("p g e -> p e g"), axis=AX,
                                              op=ALU.add), R_, W_)
        S.op("dve", lambda e: e.reduce_max(out=m1, in_=ein, axis=AX), R_, W_)
        TS("dve", oh1, ein, m1, ALU.is_equal, R_, W_)
        STT("dve", e2, oh1, -1e30, ein, ALU.mult, ALU.add, R_, W_)
        S.op("dve", lambda e: e.reduce_max(out=m2, in_=e2, axis=AX), R_, W_)
        TS("dve", oh2, e2, m2, ALU.is_equal, R_, W_)
        TT("dve", dm, m2, m1, ALU.subtract, R_, W_)
        ACTF(dm, dm, AF.Exp, R_, W_)
        TS("dve", dm, dm, 1.0, ALU.add, R_, W_)
        S.op("dve", lambda e: e.reciprocal(out=w1, in_=dm), R_, W_)
        TT("dve", gates_all[:, tt, 0:1], gval, w1, ALU.mult, R_, [Bgates])
        TT("dve", gates_all[:, tt, 1:2], gval, gates_all[:, tt, 0:1], ALU.subtract, R_ + [Bgates], [Bgates])
        for k, ohk in enumerate((oh1, oh2)):
            TT("dve", o64[k].rearrange("p (g e) -> p g e", g=8), bc(ohg.unsqueeze(2), [128, 8, 8]),
               bc(ohk.unsqueeze(1), [128, 8, 8]), ALU.mult, R_, W_)
        TT("dve", twb, o64[0], o64[1], ALU.add, R_, W_)

    def p3_dispatch(b, i):
        tt = 4 * b + i
        sm, Bsm, twb, mb = sms[i], Bsms[i], twbs[i], 4 + i
        o64 = [sm[:, 200:264], sm[:, 264:328]]
        posC, tmp64, dkf = sm[:, 328:392], sm[:, 420:484], sm[:, 392:394]
        R_, W_ = [Bsm], [Bsm]
        MM(ps[mb][:, 128:192], ltrib, twb, True, True, [Bsm, Bcstb], [PB_[mb]])
        MM(ps[mb][:, 192:256], onesb, twb, True, True, [Bsm, Bcstb], [PB_[mb]])
        TT("dve", posC, ps[mb][:, 128:192], runc, ALU.add, [PB_[mb], Brun, Bsm], W_)
        TT("dve", posC, posC, eC, ALU.add, R_ + [Bcst], W_)
        TT("dve", runc, runc, ps[mb][:, 192:256], ALU.add, [PB_[mb], Brun], [Brun])
        for k in range(2):
            TT("dve", tmp64, o64[k], posC, ALU.mult, R_, W_)
            S.op("dve", lambda e, k=k: e.reduce_sum(out=dkf[:, k:k + 1], in_=tmp64, axis=AX), R_, W_)
            CP("dve", dest_all[:, tt, k:k + 1], dkf[:, k:k + 1], R_, [Bdest])
            S.op("pool", lambda e, k=k: e.indirect_dma_start(
                out=xbuf, out_offset=bass.IndirectOffsetOnAxis(ap=dest_all[:, tt, k:k + 1], axis=0),
                in_=hbt2[b % 2][i], in_offset=None), [Bhbt2[b % 2][i], Bdest], [Bxbuf], dma=3)

    def p3_loads(b):
        bi = b % 2
        DMA("sp", sgb[bi], sg_dv[:, :, b * 512:(b + 1) * 512], [Bdr_sg], [Bsgb[bi]], 1)
        DMA("sp", atb[bi], attT_dv[:, :, b * 512:(b + 1) * 512], [Bdr_att], [Batb[bi]], 1)

    def p3_glu(b):
        bi = b % 2
        for oc in range(4):
            bank = oc % 2
            for kc in range(4):
                MM(ps[bank][:, :], wglu_b[:, kc, oc * 128:(oc + 1) * 128], sgb[bi][:, kc, :], kc == 0, kc == 3,
                   [Bw3, Bsgb[bi]], [PB_[bank]])
            ACTF(gtb[bank], ps[bank][:, :], AF.Sigmoid, [PB_[bank], Bw3], [Bgtb[bank]], bias=bglu[:, oc:oc + 1])
            TT("dve", sTg[bi][:, oc, :], sgb[bi][:, oc, :], gtb[bank], ALU.mult, [Bsgb[bi], Bgtb[bank]], [BsTg[bi]])

    def p3_xloads(b):
        for i in range(NL):
            tt_ = 4 * b + i
            DMA("sp", xt[i], x[tt_ * 128:(tt_ + 1) * 128, :], [], [Bxt[i]], 1)

    p3_loads(0)
    p3_xloads(0)
    p3_glu(0)
    for i in range(NL):
        p3_f1a(0, i)
    for b in range(16):
        if b < 15:
            p3_loads(b + 1)
            p3_xloads(b + 1)
        lanes = [[] for _ in range(NL)]
        for i in range(NL):
            S.lane = lanes[i]
            p3_f1b(b, i)
        S.lane = None
        S.run_lanes(lanes)
        if b < 15:
            p3_glu(b + 1)
        for i in range(NL):
            if b > 0:
                p3_dispatch(b - 1, i)
            p3_f2(b, i)
        lanes = [[] for _ in range(NL + 1)]
        for i in range(NL):
            S.lane = lanes[i]
            p3_router(b, i)
        if b < 15:
            S.lane = lanes[NL]
            for i in range(NL):
                p3_f1a(b + 1, i)
        S.lane = None
        S.run_lanes(lanes)
    for i in range(NL):
        p3_dispatch(15, i)
    S.barrier()
    A.off = persist2

    if stop == 3:
        do_tap(hbuf, Bhbuf)
        return finish()

    NWB = 3
    wg = [A.alloc(8 * 512, BF16).rearrange("p (k n) -> p k n", k=8) for _ in range(NWB)]
    wu = [A.alloc(8 * 512, BF16).rearrange("p (k n) -> p k n", k=8) for _ in range(NWB)]
    wd = [A.alloc(4 * DM, BF16).rearrange("p (k n) -> p k n", k=4) for _ in range(NWB)]
    xgt = [A.alloc(3 * DM, BF16).rearrange("p (s n) -> p s n", s=3) for _ in range(NWB)]
    xgT = [A.alloc(8 * CAP, BF16).rearrange("p (k n) -> p k n", k=8) for _ in range(NWB)]
    hidT = [A.alloc(4 * CAP, BF16).rearrange("p (k n) -> p k n", k=4) for _ in range(NWB)]
    sgt = [A.alloc(CAP, BF16) for _ in range(2)]
    obt = [A.alloc(DM, BF16) for _ in range(3)]
    Bwg, Bwu, Bwd, Bxgt, BxgT, BhidT, Bsgt, Bobt = ([Buf() for _ in range(3)] for _ in range(8))
    c4 = {"t": 0, "g": 0, "d": 0, "o": 0}

    def p4_load(e_):
        bi = e_ % NWB
        DMA("pool", wg[bi], w_gate[e_].rearrange("(k p) n -> p k n", p=128), [], [Bwg[bi]], 5)
        DMA("pool", wu[bi], w_up[e_].rearrange("(k p) n -> p k n", p=128), [], [Bwu[bi]], 5)
        DMA("pool", wd[bi], w_down[e_].rearrange("(k p) n -> p k n", p=128), [], [Bwd[bi]], 5)
        DMA("sp", xgt[bi], xbuf[e_ * CAP:(e_ + 1) * CAP, :].rearrange("(s p) n -> p s n", p=128),
            [Bxbuf], [Bxgt[bi]], 6)

    def p4_A(e_):
        bi = e_ % NWB
        for s_ in range(3):
            bank = c4["t"] % 2
            c4["t"] += 1
            pb16 = ps[bank][:, :].bitcast(BF16)
            for kc in range(8):
                TR(pb16[:, kc * 128:(kc + 1) * 128], xgt[bi][:, s_, kc * 128:(kc + 1) * 128], identb,
                   [Bxgt[bi], Bcstb], [PB_[bank]])
            CP(alt(), xgT[bi][:, :, s_ * 128:(s_ + 1) * 128], pb16.rearrange("p (k n) -> p k n", k=8),
               [PB_[bank]], [BxgT[bi]])

    def p4_B(e_):
        bi = e_ % NWB
        for hc in range(4):
            gi = c4["g"] % 2
            c4["g"] += 1
            bg, bu = 2 + gi, 4 + gi
            for kc in range(8):
                MM(ps[bg][:, 0:CAP], wg[bi][:, kc, hc * 128:(hc + 1) * 128], xgT[bi][:, kc, :], kc == 0, kc == 7,
                   [Bwg[bi], BxgT[bi]], [PB_[bg]])
            for kc in range(8):
                MM(ps[bu][:, 0:CAP], wu[bi][:, kc, hc * 128:(hc + 1) * 128], xgT[bi][:, kc, :], kc == 0, kc == 7,
                   [Bwu[bi], BxgT[bi]], [PB_[bu]])
            ACTF(sgt[gi], ps[bg][:, 0:CAP], AF.Silu, [PB_[bg]], [Bsgt[gi]])
            TT("dve", hidT[bi][:, hc, :], sgt[gi], ps[bu][:, 0:CAP], ALU.mult, [Bsgt[gi], PB_[bu]], [BhidT[bi]])

    def p4_C(e_):
        bi = e_ % NWB
        for s_ in range(3):
            oi = c4["o"] % 3
            c4["o"] += 1
            for half in range(2):
                bank = 6 + c4["d"] % 2
                c4["d"] += 1
                for hc in range(4):
                    MM(ps[bank][:, :], hidT[bi][:, hc, s_ * 128:(s_ + 1) * 128], wd[bi][:, hc, half * 512:(half + 1) * 512],
                       hc == 0, hc == 3, [BhidT[bi], Bwd[bi]], [PB_[bank]])
                CP(alt(), obt[oi][:, half * 512:(half + 1) * 512], ps[bank][:, :], [PB_[bank]], [Bobt[oi]])
            r0 = e_ * CAP + s_ * 128
            DMA("sp", obuf[r0:r0 + 128, :], obt[oi], [Bobt[oi]], [Bobuf], 7)

    p4_load(0)
    p4_load(1)
    p4_A(0)
    p4_B(0)
    p4_A(1)
    for e_ in range(NE):
        if e_ + 2 < NE:
            p4_load(e_ + 2)
        if e_ + 1 < NE:
            p4_B(e_ + 1)
        p4_C(e_)
        if e_ + 2 < NE:
            p4_A(e_ + 2)
    S.barrier()
    A.off = persist2

    N5 = 4
    o1 = [A.alloc(DM, BF16) for _ in range(N5)]
    o2 = [A.alloc(DM, BF16) for _ in range(N5)]
    ht = [A.alloc(DM, F32) for _ in range(N5)]
    yt5 = [A.alloc(DM, F32) for _ in range(N5)]
    Bo1, Bo2, Bht, Byt5 = ([Buf() for _ in range(N5)] for _ in range(4))
    ln5 = [A.alloc(16, F32) for _ in range(N5)]
    Bln5 = [Buf() for _ in range(N5)]
    Bout = Buf("out")
    def p5_load(tt):
        ti = tt % N5
        for k, (ob_, Bo_) in enumerate(((o1[ti], Bo1[ti]), (o2[ti], Bo2[ti]))):
            S.op("pool", lambda e, k=k, ob_=ob_: e.indirect_dma_start(
                out=ob_, out_offset=None, in_=obuf,
                in_offset=bass.IndirectOffsetOnAxis(ap=dest_all[:, tt, k:k + 1], axis=0)),
                [Bobuf, Bdest], [Bo_], dma=4)
        DMA("sp", ht[ti], hbuf[tt * 128:(tt + 1) * 128, :], [Bhbuf], [Bht[ti]], 1)

    def p5_compute(tt):
        ti = tt % N5
        y5 = yt5[ti]
        S.op("act", lambda e: e.mul(out=y5, in_=ht[ti], mul=ALPHA), [Bht[ti]], [Byt5[ti]])
        STT("dve", y5, o1[ti], gates_all[:, tt, 0:1], y5, ALU.mult, ALU.add, [Bo1[ti], Bgates, Byt5[ti]], [Byt5[ti]])
        STT("dve", y5, o2[ti], gates_all[:, tt, 1:2], y5, ALU.mult, ALU.add, [Bo2[ti], Bgates, Byt5[ti]], [Byt5[ti]])
        layer_norm(y5, 2, [Byt5[ti]], [Byt5[ti]], ln5[ti], Bln5[ti])
        out_writes.append(DMA("sp", out[tt * 128:(tt + 1) * 128, :], y5, [Byt5[ti]], [Bout], 8))

    for tt in range(3):
        p5_load(tt)
    for tt in range(64):
        if tt + 3 < 64:
            p5_load(tt + 3)
        p5_compute(tt)
    return finish()


def prep_shared(inp):
    sh = {}
    sh["w_in"] = np.ascontiguousarray(inp["w_in"][0], np.float32)
    sh["w_glu"] = np.ascontiguousarray(inp["w_glu"][0], np.float32)
    sh["b_glu"] = np.ascontiguousarray(inp["b_glu"][0].reshape(4, 128).T, np.float32)
    sh["w_out"] = np.ascontiguousarray(inp["w_out"][0], np.float32)
    sh["lnp"] = np.ascontiguousarray(np.stack([inp["ln1_g"][0], inp["ln1_b"][0], inp["ln2_g"][0], inp["ln2_b"][0]]),
                                     np.float32)
    sh["w_r"] = np.ascontiguousarray(np.concatenate([inp["w_router_group"][0], inp["w_router_expert"][0]], 1),
                                     np.float32)
    sh["b_r"] = np.ascontiguousarray(np.concatenate([inp["b_router_group"][0], inp["b_router_expert"][0]])[None],
                                     np.float32)
    sh["w_gate"] = np.ascontiguousarray(inp["w_gate"][0], np.float32)
    sh["w_up"] = np.ascontiguousarray(inp["w_up"][0], np.float32)
    sh["w_down"] = np.ascontiguousarray(inp["w_down"][0], np.float32)
    sh["btab"] = build_bias_tab(np.asarray(inp["rpb"][0], np.float32))
    sh["consts"] = build_consts()
    PB, PA = build_s5_layouts(*[np.asarray(inp[k][0], np.float32) for k in
                                ("s5_a_re", "s5_a_im", "s5_log_dt", "s5_b_re", "s5_b_im", "s5_c_re", "s5_c_im",
                                 "s5_d")])
    sh["PB"], sh["PA"] = PB, PA
    return sh


def prep_core(inp, b, sh):
    m = dict(sh)
    xb = np.asarray(inp["x"][b], np.float32)
    m["x"] = np.ascontiguousarray(xb)
    m["xT"] = np.ascontiguousarray(xb.T)
    return m


def kernel(**inputs):
    sh = prep_shared(inputs)
    nc = build()
    in_maps = [prep_core(inputs, b, sh) for b in range(8)]
    res = run_bass_kernel_spmd(nc, in_maps, core_ids=list(range(8)))
    return np.stack([np.asarray(r["out"], np.float32) for r in res.results], 0)
```

```python
import os
import numpy as np
from contextlib import ExitStack
import concourse.bass as bass
import concourse.mybir as mybir
from concourse.bass_utils import run_bass_kernel_spmd

F32 = mybir.dt.float32
BF16 = mybir.dt.bfloat16
I32 = mybir.dt.int32
ALU = mybir.AluOpType
AF = mybir.ActivationFunctionType

NT = 8192
DM = 1024
NE = 64
CAP = 384
ALPHA = 2.0 ** 0.25
EPS = 1e-5
ENGINES = ("pe", "act", "dve", "pool", "sp")
DMA_POOL = {"sp": 24, "pool": 12, "act": 4}
SAME_ENGINE_SYNC = not os.environ.get("K_NOSES")
RAW_ONLY = not os.environ.get("K_ALLDEPS")
MAGIC = 12582912.0
C1 = 6.28125
C2 = float(2 * np.pi - 6.28125)
NCLS = 21


class Buf:
    __slots__ = ("name", "w", "r")

    def __init__(self, name=""):
        self.name = name
        self.w = None
        self.r = []


class Op:
    __slots__ = ("id", "eng", "fn", "deps", "dma", "needed", "val", "sem", "raw")

    def __init__(self, id, eng, fn, deps, dma):
        self.id, self.eng, self.fn, self.deps, self.dma = id, eng, fn, deps, dma
        self.needed = False
        self.val = None
        self.sem = None


class Sched:
    def __init__(self, nc):
        self.nc = nc
        self.ops = []
        self.q = {e: [] for e in ENGINES}
        self.last_dma = {}
        self.ndma = {}
        self.lane = None

    def op(self, eng, fn, reads=(), writes=(), dma=None, extra=()):
        if self.lane is not None:
            self.lane.append((eng, fn, tuple(reads), tuple(writes), dma, tuple(extra)))
            return None
        return self._op(eng, fn, reads, writes, dma, extra)

    def run_lanes(self, lanes, chunk=1):
        idx = [0] * len(lanes)
        chunks = chunk if isinstance(chunk, (list, tuple)) else [chunk] * len(lanes)
        while any(idx[i] < len(L) for i, L in enumerate(lanes)):
            for i, L in enumerate(lanes):
                for _ in range(chunks[i]):
                    if idx[i] < len(L):
                        self._op(*L[idx[i]])
                        idx[i] += 1

    def _op(self, eng, fn, reads=(), writes=(), dma=None, extra=()):
        deps = set(extra)
        raw = set(extra)
        for b in reads:
            if b.w is not None:
                deps.add(b.w)
                raw.add(b.w)
        for b in writes:
            if b.w is not None:
                deps.add(b.w)
            deps.update(b.r)
        key = None
        if dma is not None:
            n = self.ndma.get(eng, 0)
            self.ndma[eng] = n + 1
            key = ("dma", eng, n % DMA_POOL[eng])
            if key in self.last_dma:
                deps.add(self.last_dma[key])
        o = Op(len(self.ops), eng, fn, deps, key)
        o.raw = raw
        self.ops.append(o)
        self.q[eng].append(o)
        if key is not None:
            self.last_dma[key] = o.id
        for b in writes:
            b.w = o.id
            b.r = []
        for b in reads:
            b.r.append(o.id)
        return o.id

    def barrier(self):
        last = [self.q[e][-1].id for e in ENGINES if self.q[e]] + list(self.last_dma.values())
        for e in ENGINES:
            self.op(e, lambda en: en.nop(), extra=last)

    def emit(self, sems):
        ops = self.ops
        for o in ops:
            nd = set()
            for d in o.deps:
                p = ops[d]
                if p.dma is None and o.dma is None and p.eng == o.eng:
                    if o.eng == "pe" or not SAME_ENGINE_SYNC or (RAW_ONLY and o.eng != "pool" and d not in o.raw):
                        continue
                nd.add(d)
            o.deps = nd
            for d in nd:
                ops[d].needed = True
        cnt = {e: 0 for e in ENGINES}
        dcnt = {}
        for o in ops:
            if o.dma is not None:
                key = o.dma
                dcnt[key] = dcnt.get(key, 0) + 16
                o.sem, o.val = key, dcnt[key]
            elif o.needed:
                cnt[o.eng] += 1
                o.sem, o.val = o.eng, cnt[o.eng]

        def run(engname):
            def body(e):
                waited = {}
                for o in self.q[engname]:
                    need = {}
                    for d in o.deps:
                        p = ops[d]
                        if p.val > need.get(p.sem, 0):
                            need[p.sem] = p.val
                    for k, v in need.items():
                        if waited.get(k, 0) < v:
                            e.wait_ge(sems[k], v)
                            waited[k] = v
                    ins = o.fn(e)
                    if o.sem is not None:
                        ins.then_inc(sems[o.sem], 16 if o.dma is not None else 1)
            return body

        with self.nc.Block() as block:
            block.tensor(run("pe"))
            block.scalar(run("act"))
            block.vector(run("dve"))
            block.gpsimd(run("pool"))
            block.sync(run("sp"))


class Arena:
    def __init__(self, t, nbytes):
        self.t, self.cap, self.off = t, nbytes, 0

    def alloc(self, n, dt):
        bs = 2 if dt == BF16 else 4
        nb = (n * bs + 63) // 64 * 64
        a0 = self.off
        self.off += nb
        assert self.off <= self.cap, ("arena overflow", self.off, self.cap)
        v = self.t[:, a0 // 4:(a0 + nb) // 4]
        if dt == BF16:
            return v.bitcast(BF16)[:, :n]
        if dt == I32:
            return v.bitcast(I32)[:, :n]
        return v[:, :n]


def _rs(r):
    return min(max(r - 4, 0), 120)


def _classes():
    cl = [(10, 8 + i) for i in range(5)]
    for j in (0, 1):
        cl += [(j, jc) for jc in range(4)]
    for j in (62, 63):
        cl += [(j, jc) for jc in range(60, 64)]
    return cl


def build_bias_tab(rpb):
    cstart = np.clip(np.arange(64) - 8, 0, 48)
    tab = np.full((NCLS, 128, 8, 128), -30000.0, np.float32)
    for ci, (j, jc) in enumerate(_classes()):
        for kl in range(2):
            for ql in range(2):
                kr, qr = 2 * jc + kl, 2 * j + ql
                if _rs(qr) <= kr < _rs(qr) + 8:
                    drow = kr - qr + 7
                    for qc in range(64):
                        kcs = np.arange(cstart[qc], cstart[qc] + 16)
                        tab[ci, kl * 64 + kcs, :, ql * 64 + qc] = rpb[:, drow, kcs - qc + 15].T
    return tab.reshape(NCLS, 128, 1024)


def build_consts():
    c = np.zeros((128, 768), np.float32)
    c[:, 0:128] = np.eye(128, dtype=np.float32)
    c[:, 128:256] = np.triu(np.ones((128, 128), np.float32), 1)
    c[:, 256:384] = 1.0
    c[:, 384:448] = (np.arange(64) * CAP)[None, :]
    p = np.arange(128)
    c[:, 448] = ((p // 16) % 2 == 0)
    c[:, 449] = ((p // 16) % 2 == 1)
    c[:, 450] = ((p // 16) % 2 == 0) & (p >= 96)
    c[:, 451] = ((p // 16) % 2 == 1) & (p >= 96)
    c[:, 512:576] = 1.0
    c[:, 704:768] = 1.0
    return c


def build_s5_layouts(a_re, a_im, log_dt, b_re, b_im, c_re, c_im, dsk):
    def LB(a):
        return a.reshape(2, 16, 2, 64).transpose(2, 3, 0, 1).reshape(128, 32)

    def LBb(b):
        return b.reshape(2, 16, 2, 64, 16).transpose(2, 3, 0, 1, 4).reshape(128, 512)

    def LBc(c):
        return c.reshape(2, 16, 2, 16, 64).transpose(2, 4, 0, 1, 3).reshape(128, 512)

    ld3 = np.broadcast_to(log_dt[:, :, None], (2, 32, 64))
    PB = np.concatenate([LB(a_re), LB(a_im), LB(ld3), LBb(b_re), LBb(b_im), LBc(c_re), LBc(c_im)], 1)

    def LA(a):
        t = a.reshape(2, 4, 8, 64).transpose(2, 0, 1, 3)
        return np.broadcast_to(t[:, None], (8, 16, 2, 4, 64)).reshape(128, 512)

    def LAb(b):
        return b.reshape(2, 4, 8, 64, 16).transpose(2, 4, 0, 1, 3).reshape(128, 512)

    DA = dsk.reshape(4, 8, 16).transpose(1, 2, 0).reshape(128, 4)
    PA = np.concatenate([LA(a_re), LA(a_im), LA(ld3), LAb(b_re), LAb(b_im), DA], 1)
    return np.ascontiguousarray(PB, np.float32), np.ascontiguousarray(PA, np.float32)


NPB = 96 + 2048
NPA = 1536 + 1024 + 4


def build(stop=99, tap=None):
    nc = bass.Bass("TRN2", target_bir_lowering=False)

    def din(name, shape, dt=F32):
        return nc.dram_tensor(name, list(shape), dt, kind="ExternalInput").ap()

    def dscr(name, shape, dt):
        return nc.dram_tensor(name, list(shape), dt).ap()

    xT = din("xT", [DM, NT])
    x = din("x", [NT, DM])
    w_in = din("w_in", [DM, 2048])
    w_glu = din("w_glu", [512, 512])
    b_glu = din("b_glu", [128, 4])
    w_out = din("w_out", [DM, DM])
    lnp = din("lnp", [4, DM])
    w_r = din("w_r", [DM, 72])
    b_r = din("b_r", [1, 72])
    w_gate = din("w_gate", [NE, DM, 512])
    w_up = din("w_up", [NE, DM, 512])
    w_down = din("w_down", [NE, 512, DM])
    btab = din("btab", [NCLS, 128, 1024])
    consts = din("consts", [128, 768])
    PBd = din("PB", [128, NPB])
    PAd = din("PA", [128, NPA])
    out = nc.dram_tensor("out", [NT, DM], F32, kind="ExternalOutput").ap()
    uT_d = dscr("uT_d", [512, NT], BF16)
    attT_d = dscr("attT_d", [512, NT], BF16)
    hbuf = dscr("hbuf", [NT, DM], F32)
    xbuf = dscr("xbuf", [NE * CAP, DM], BF16)
    obuf = dscr("obuf", [NE * CAP, DM], BF16)
    tapo = None
    if tap is not None:
        tapo = nc.dram_tensor("tap", list(tap[1]), tap[2], kind="ExternalOutput").ap()

    S = Sched(nc)
    es = ExitStack()
    ARENA_BYTES = 206 * 1024
    arena_t = es.enter_context(nc.sbuf_tensor("arena", [128, ARENA_BYTES // 4], F32))
    A = Arena(arena_t, ARENA_BYTES)
    ps = [es.enter_context(nc.psum_tensor("ps%d" % i, [128, 512], F32)) for i in range(8)]
    PB_ = [Buf("ps%d" % i) for i in range(8)]
    sems = {e: es.enter_context(nc.semaphore("s_" + e)) for e in ENGINES}
    for q_, n_ in DMA_POOL.items():
        for i in range(n_):
            sems[("dma", q_, i)] = es.enter_context(nc.semaphore("d_%s%d" % (q_, i)))

    def MM(o, lhsT, rhs, start, stop, R, W):
        return S.op("pe", lambda e: e.matmul(o, lhsT=lhsT, rhs=rhs, start=start, stop=stop,
                                             skip_group_check=True), R, W)

    def TR(o, in_, ident, R, W):
        return S.op("pe", lambda e: e.transpose(o, in_, ident), R, W)

    def ACTF(o, in_, func, R, W, scale=1.0, bias=0.0):
        return S.op("act", lambda e: e.activation(out=o, in_=in_, func=func, bias=bias, scale=scale), R, W)

    def CP(eng, o, in_, R, W):
        if eng == "act":
            return S.op("act", lambda e: e.copy(out=o, in_=in_), R, W)
        return S.op(eng, lambda e: e.tensor_copy(out=o, in_=in_), R, W)

    def TT(eng, o, in0, in1, op, R, W):
        return S.op(eng, lambda e: e.tensor_tensor(out=o, in0=in0, in1=in1, op=op), R, W)

    def TS(eng, o, in0, s1, op0, R, W, s2=None, op1=None):
        if op1 is None:
            return S.op(eng, lambda e: e.tensor_scalar(out=o, in0=in0, scalar1=s1, scalar2=None, op0=op0), R, W)
        return S.op(eng, lambda e: e.tensor_scalar(out=o, in0=in0, scalar1=s1, scalar2=s2, op0=op0, op1=op1), R, W)

    def STT(eng, o, in0, scalar, in1, op0, op1, R, W):
        return S.op(eng, lambda e: e.scalar_tensor_tensor(out=o, in0=in0, scalar=scalar, in1=in1,
                                                          op0=op0, op1=op1), R, W)

    def MEMSET(eng, o, val, W):
        return S.op(eng, lambda e: e.memset(o, val), (), W)

    def DMA(q, o, in_, R, W, stream):
        return S.op(q, lambda e: e.dma_start(out=o, in_=in_), R, W, dma=stream)

    rr = {"n": 0}

    def alt():
        rr["n"] += 1
        return "act" if rr["n"] % 2 else "dve"

    out_writes = []

    cst = A.alloc(768, F32)
    cstb = A.alloc(640, BF16)
    Bcst, Bcstb = Buf("cst"), Buf("cstb")
    DMA("sp", cst, consts, [], [Bcst], 1)
    CP("act", cstb[:, 0:384], cst[:, 0:384], [Bcst], [Bcstb])
    CP("act", cstb[:, 384:640], cst[:, 512:768], [Bcst], [Bcstb])
    onesz = [cstb[:, 384:512], cstb[:, 512:640]]
    identf, identb = cst[:, 0:128], cstb[:, 0:128]
    ltrib, onesb = cstb[:, 128:256], cstb[:, 256:384]
    eC, maskcol, mask3 = cst[:, 384:448], cst[:, 448:450], cst[:, 450:452]
    gates_all = A.alloc(128, F32).rearrange("p (t k) -> p t k", k=2)
    dest_all = A.alloc(128, I32).rearrange("p (t k) -> p t k", k=2)
    Bgates, Bdest = Buf("gates"), Buf("dest")
    runc = A.alloc(64, F32)
    Brun = Buf("run")
    MEMSET("dve", runc, 0.0, [Brun])
    persist = A.off

    w_in_b = A.alloc(8 * 2048, BF16).rearrange("p (k n) -> p k n", k=8)
    Bwin = Buf("w_in")
    xTb = [A.alloc(8 * 512, BF16).rearrange("p (k n) -> p k n", k=8) for _ in range(2)]
    BxT = [Buf("xT0"), Buf("xT1")]
    NQ, NK = 12, 12
    qz = [A.alloc(4 * NQ * 128, BF16).rearrange("p (f n) -> p f n", f=4) for _ in range(2)]
    kT = A.alloc(4 * NK * 128, BF16).rearrange("p (f n) -> p f n", f=4)
    vz = [A.alloc(NK * 512, BF16).rearrange("p (s i c) -> p s i c", s=NK, i=4) for _ in range(2)]
    Bq = [Buf("q%d" % i) for i in range(NQ)]
    Bk = [Buf("k%d" % i) for i in range(NK)]
    Bv = [Buf("v%d" % i) for i in range(NK)]
    for par in range(2):
        MEMSET("pool", qz[par], 0.0, Bq)
        MEMSET("pool", vz[par], 0.0, Bv)
    maskt = A.alloc(NCLS * 1024, BF16).rearrange("p (c n) -> p c n", c=NCLS)
    Bmask = Buf("mask")
    mst = [A.alloc(1024, F32) for _ in range(2)]
    Bmst = [Buf("mst0"), Buf("mst1")]
    ust = [A.alloc(4 * 512, BF16).rearrange("p (f n) -> p f n", f=4) for _ in range(2)]
    Bust = [Buf("ust0"), Buf("ust1")]
    NEB = 5
    Eb = [A.alloc(512, BF16) for _ in range(NEB)]
    BE = [Buf("E%d" % i) for i in range(NEB)]
    recs = [A.alloc(512, F32) for _ in range(2)]
    Brecs = [Buf(), Buf()]
    attb = [A.alloc(512, BF16) for _ in range(2)]
    Batt = [Buf("att0"), Buf("att1")]
    Bdr_u, Bdr_att = Buf("uT_d"), Buf("attT_d")

    w_in_v = w_in.rearrange("(k p) n -> p k n", p=128)
    for kc in range(8):
        DMA("pool", w_in_b[:, kc, :], w_in_v[:, kc, :], [], [Bwin], 0)
    for c in range(NCLS):
        DMA("sp", mst[c % 2], btab[c], [], [Bmst[c % 2]], 1)
        ACTF(maskt[:, c, :], mst[c % 2], AF.Exp, [Bmst[c % 2]], [Bmask])

    xT_v = xT.rearrange("(k p) t -> p k t", p=128)
    uT_dv = uT_d.rearrange("(f p) t -> p f t", p=128)
    attT_dv = attT_d.rearrange("(f p) t -> p f t", p=128)
    cnt = {"sb": 0, "e": 0, "a": 0}

    def tile_units(j):
        if j <= 1:
            kcs = list(range(4))
            cls = [5 + 4 * j + jc for jc in kcs]
        elif j >= 62:
            kcs = list(range(60, 64))
            cls = [13 + 4 * (j - 62) + (jc - 60) for jc in kcs]
        else:
            kcs = list(range(j - 2, j + 3))
            cls = list(range(5))
        us = []
        for ci, jc in enumerate(kcs):
            for par in range(2):
                us.append({"j": j, "ks": jc % NK, "cls": cls[ci], "par": par,
                           "first": ci == 0 and par == 0, "last": ci == len(kcs) - 1 and par == 1})
        return us

    def st12(u):
        sb = 2 + cnt["sb"] % 4
        cnt["sb"] += 1
        ei = cnt["e"] % NEB
        cnt["e"] += 1
        u["ei"] = ei
        qs, ks, par = u["j"] % NQ, u["ks"], u["par"]
        for i in range(4):
            MM(ps[sb][:, i * 128:(i + 1) * 128], kT[:, i, ks * 128:(ks + 1) * 128],
               qz[par][:, i, qs * 128:(qs + 1) * 128], True, True, [Bk[ks], Bq[qs]], [PB_[sb]])
        ACTF(Eb[ei], ps[sb][:, :], AF.Exp, [PB_[sb]], [BE[ei]], scale=0.125)
        mv = maskt[:, u["cls"], :].rearrange("p (i t q) -> p i t q", i=4, t=2)[:, :, par, :]
        ev = Eb[ei].rearrange("p (i q) -> p i q", i=4)
        TT("dve", ev, ev, mv, ALU.mult, [BE[ei], Bmask], [BE[ei]])

    def st3(u):
        ei, ks, par, j = u["ei"], u["ks"], u["par"], u["j"]
        po, pm = 6, 7
        for i in range(4):
            MM(ps[po][:, i * 128:(i + 1) * 128], vz[par][:, ks, i, :], Eb[ei][:, i * 128:(i + 1) * 128],
               u["first"] and i == 0, u["last"], [Bv[ks], BE[ei]], [PB_[po]])
        MM(ps[pm][:, :], onesz[par], Eb[ei], u["first"], u["last"], [Bcstb, BE[ei]], [PB_[pm]])
        if u["last"]:
            recj = recs[j % 2]
            S.op("dve", lambda e: e.reciprocal(out=recj, in_=ps[pm][:, :]), [PB_[pm]], [Brecs[j % 2]])
            ai = cnt["a"] % 2
            cnt["a"] += 1
            TT("dve", attb[ai], ps[po][:, :], recj, ALU.mult, [PB_[po], Brecs[j % 2]], [Batt[ai]])
            DMA("sp", attT_dv[:, :, j * 128:(j + 1) * 128], attb[ai].rearrange("p (f n) -> p f n", f=4),
                [Batt[ai]], [Bdr_att], 2)

    SKEW = 3

    def attn_tiles(js):
        us = [u for j in js for u in tile_units(j)]
        for t in range(len(us) + SKEW):
            if t < len(us):
                st12(us[t])
            if t >= SKEW:
                st3(us[t - SKEW])

    pcnt = {"n": 0}

    def inproj(b):
        xb, Bx = xTb[b % 2], BxT[b % 2]
        DMA("pool", xb, xT_v[:, :, b * 512:(b + 1) * 512], [], [Bx], 0)
        for fc in range(12):
            col0 = fc * 128 if fc < 8 else 1536 + (fc - 8) * 128
            bank = pcnt["n"] % 2
            pcnt["n"] += 1
            for kc in range(8):
                MM(ps[bank][:, :], w_in_b[:, kc, col0:col0 + 128], xb[:, kc, :], kc == 0, kc == 7,
                   [Bwin, Bx], [PB_[bank]])
            if fc < 4:
                s0 = (4 * b) % NQ
                W = [Bq[s0 + i] for i in range(4)]
                CP("act", qz[0][0:64, fc, s0 * 128:s0 * 128 + 512], ps[bank][0:64, :], [PB_[bank]], W)
                CP("dve", qz[1][64:128, fc, s0 * 128:s0 * 128 + 512], ps[bank][64:128, :], [PB_[bank]], W)
                continue
            elif fc < 8:
                s0 = (4 * b) % NK
                dst, W = kT[:, fc - 4, s0 * 128:s0 * 128 + 512], [Bk[s0 + i] for i in range(4)]
            else:
                dst, W = ust[b % 2][:, fc - 8, :], [Bust[b % 2]]
            CP(alt(), dst, ps[bank][:, :], [PB_[bank]], W)
        DMA("sp", uT_dv[:, :, b * 512:(b + 1) * 512], ust[b % 2], [Bust[b % 2]], [Bdr_u], 2)
        for i in range(4):
            bank = pcnt["n"] % 2
            pcnt["n"] += 1
            for kc in range(8):
                MM(ps[bank][:, :], xb[:, kc, i * 128:(i + 1) * 128], w_in_b[:, kc, 1024:1536], kc == 0, kc == 7,
                   [Bwin, Bx], [PB_[bank]])
            sl = (4 * b + i) % NK
            pv4 = ps[bank][:, :].rearrange("p (i t d) -> p i t d", i=4, t=2)
            CP("act", vz[0][:, sl, :, 0:64], pv4[:, :, 0, :], [PB_[bank]], [Bv[sl]])
            CP("dve", vz[1][:, sl, :, 64:128], pv4[:, :, 1, :], [PB_[bank]], [Bv[sl]])

    inproj(0)
    for b in range(16):
        lanes = [[], []]
        if b < 15:
            S.lane = lanes[0]
            inproj(b + 1)
        S.lane = lanes[1]
        lo = max(0, 4 * b - 2)
        hi = 4 * b + 2 if b < 15 else 64
        attn_tiles(list(range(lo, hi)))
        S.lane = None
        na, nb_ = len(lanes[0]), len(lanes[1])
        S.run_lanes(lanes, chunk=[1, max(1, nb_ // max(na, 1))])
    S.barrier()
    A.off = persist

    def finish():
        S.op("sp", lambda e: e.nop(), extra=out_writes)
        S.emit(sems)
        es.close()
        return nc

    def do_tap(src, Bsrc):
        out_writes.append(DMA("sp", tapo, src, [Bsrc], [], 9))

    if stop == 1:
        do_tap({"uT_d": uT_d, "attT_d": attT_d}[tap[0]], {"uT_d": Bdr_u, "attT_d": Bdr_att}[tap[0]])
        return finish()

    sg_d = dscr("sg_d", [512, NT], BF16)
    sg_dv = sg_d.rearrange("(f p) t -> p f t", p=128)
    Bdr_sg = Buf("sg_d")
    PBt = A.alloc(NPB, F32)
    BPB, BPA, Bglob = Buf("PB"), Buf("PA"), Buf("glob")

    def v4(ap):
        return ap.rearrange("p (d q c) -> p d q c", d=2, q=16)

    BBr, BBi, CBr, CBi = (v4(PBt[:, 96:608]), v4(PBt[:, 608:1120]), v4(PBt[:, 1120:1632]), v4(PBt[:, 1632:2144]))
    DAc = A.alloc(4, F32)
    PW = A.alloc(17 * 64, F32).rearrange("p (k r n) -> p k r n", k=17, r=2)
    SC = A.alloc(9 * 3 * 32, F32).rearrange("p (l k c n) -> p l k c n", l=1, k=9, c=3)
    BbB = A.alloc(2 * 512, F32).rearrange("p (r d q c) -> p r d q c", r=2, d=2, q=16)
    BbA = A.alloc(2 * 512, F32).rearrange("p (r n) -> p r n", r=2)
    lamA = A.alloc(2 * 512, F32).rearrange("p (r n) -> p r n", r=2)
    p2mark = A.off
    PAt = A.alloc(NPA, F32)
    DMA("sp", PBt, PBd, [], [BPB], 1)
    DMA("sp", PAt, PAd, [], [BPA], 1)
    arB, aiB, ldB = PBt[:, 0:32], PBt[:, 32:64], PBt[:, 64:96]
    arA, aiA, ldA = PAt[:, 0:512], PAt[:, 512:1024], PAt[:, 1024:1536]
    BAr, BAi = PAt[:, 1536:2048], PAt[:, 2048:2560]

    def cmul(eng, o_r, o_i, a_r, a_i, b_r, b_i, t1, t2, R, W):
        TT(eng, t1, a_r, b_r, ALU.mult, R, W)
        TT(eng, t2, a_i, b_i, ALU.mult, R, W)
        TT(eng, o_r, t1, t2, ALU.subtract, R, W)
        TT(eng, t1, a_r, b_i, ALU.mult, R, W)
        TT(eng, t2, a_i, b_r, ALU.mult, R, W)
        TT(eng, o_i, t1, t2, ALU.add, R, W)

    def lam_z(ar, ai, ld, n, Bin, lr, li, zr, zi):
        R, W = [Bin, Bglob], [Bglob]
        dt, mag, th, kk, r, sn, cs, tmp = [A.alloc(n, F32) for _ in range(8)]
        ACTF(dt, ld, AF.Exp, R, W)
        TT("dve", mag, ar, dt, ALU.mult, R, W)
        ACTF(mag, mag, AF.Exp, R, W)
        TT("dve", th, ai, dt, ALU.mult, R, W)
        TS("dve", kk, th, float(1 / (2 * np.pi)), ALU.mult, R, W, s2=MAGIC, op1=ALU.add)
        TS("dve", kk, kk, -MAGIC, ALU.add, R, W)
        STT("dve", r, kk, -C1, th, ALU.mult, ALU.add, R, W)
        STT("dve", r, kk, -C2, r, ALU.mult, ALU.add, R, W)
        TS("dve", r, r, float(np.pi), ALU.min, R, W, s2=-float(np.pi), op1=ALU.max)
        ACTF(sn, r, AF.Sin, R, W)
        TS("dve", tmp, r, -1.0, ALU.mult, R, W)
        TT("dve", tmp, tmp, r, ALU.max, R, W)
        TS("dve", tmp, tmp, -1.0, ALU.mult, R, W, s2=float(np.pi / 2), op1=ALU.add)
        ACTF(cs, tmp, AF.Sin, R, W)
        TT("dve", lr, mag, cs, ALU.mult, R, W)
        TT("dve", li, mag, sn, ALU.mult, R, W)
        TT("dve", tmp, ar, ar, ALU.mult, R, W)
        TT("dve", kk, ai, ai, ALU.mult, R, W)
        TT("dve", tmp, tmp, kk, ALU.add, R, W)
        S.op("dve", lambda e: e.reciprocal(out=tmp, in_=tmp), R, W)
        TS("dve", r, lr, -1.0, ALU.add, R, W)
        TT("dve", kk, r, ar, ALU.mult, R, W)
        TT("dve", th, li, ai, ALU.mult, R, W)
        TT("dve", kk, kk, th, ALU.add, R, W)
        TT("dve", zr, kk, tmp, ALU.mult, R, W)
        TT("dve", kk, li, ar, ALU.mult, R, W)
        TT("dve", th, r, ai, ALU.mult, R, W)
        TT("dve", kk, kk, th, ALU.subtract, R, W)
        TT("dve", zi, kk, tmp, ALU.mult, R, W)

    RG, WG = [BPB, BPA, Bglob], [Bglob]
    CP("dve", DAc, PAt[:, 2560:2564], RG, WG)
    zB = [A.alloc(32, F32) for _ in range(2)]
    lam_z(arB, aiB, ldB, 32, BPB, PW[:, 1, 0, :], PW[:, 1, 1, :], zB[0], zB[1])
    t1b, t2b = A.alloc(512, F32), A.alloc(512, F32)
    MEMSET("dve", PW[:, 0, 0, :], 1.0, WG)
    MEMSET("dve", PW[:, 0, 1, :], 0.0, WG)
    for k in range(2, 17):
        cmul("dve", PW[:, k, 0, :], PW[:, k, 1, :], PW[:, k - 1, 0, :], PW[:, k - 1, 1, :],
             PW[:, 1, 0, :], PW[:, 1, 1, :], t1b[:, 0:32], t2b[:, 0:32], RG, WG)
    CP("dve", SC[:, 0, 0, 0:2, :], PW[:, 16, 0:2, :], RG, WG)
    for k in range(1, 9):
        cmul("dve", SC[:, 0, k, 0, :], SC[:, 0, k, 1, :], SC[:, 0, k - 1, 0, :], SC[:, 0, k - 1, 1, :],
             SC[:, 0, k - 1, 0, :], SC[:, 0, k - 1, 1, :], t1b[:, 0:32], t2b[:, 0:32], RG, WG)
    TS("dve", SC[:, 0, :, 2, :], SC[:, 0, :, 1, :], -1.0, ALU.mult, RG, WG)

    def bc(ap, shape):
        return ap.to_broadcast(shape)

    zrb = bc(zB[0].unsqueeze(2), [128, 32, 16])
    zib = bc(zB[1].unsqueeze(2), [128, 32, 16])
    f3 = lambda ap: ap.rearrange("p d q c -> p (d q) c")
    f3o = lambda ap: ap.rearrange("p (n c) -> p n c", c=16)
    cmul("dve", f3(BbB[:, 0]), f3(BbB[:, 1]), zrb, zib, f3(BBr), f3(BBi), f3o(t1b), f3o(t2b), RG, WG)
    zA = [A.alloc(512, F32) for _ in range(2)]
    lam_z(arA, aiA, ldA, 512, BPA, lamA[:, 0, :], lamA[:, 1, :], zA[0], zA[1])
    cmul("dve", BbA[:, 0, :], BbA[:, 1, :], zA[0], zA[1], BAr, BAi, t1b, t2b, RG, WG)
    S.barrier()
    A.off = p2mark

    uTf = [A.alloc(NT, BF16)]
    uds = A.alloc(NT, BF16).rearrange("p (s j) -> p s j", s=16)
    Buds = Buf("uds")
    BuT = [[Buf() for _ in range(16)]]
    TXs = [A.alloc(16 * 2 * 128, BF16).rearrange("p (k r g c) -> p k r g c", k=16, r=2, g=2) for _ in range(2)]
    TX3s = [A.alloc(16 * 2 * 128, BF16).rearrange("p (k r g c) -> p k r g c", k=16, r=2, g=2) for _ in range(2)]
    BTXs = [Buf(), Buf()]
    Wb = A.alloc(2 * 2 * 4 * 17 * 16, BF16).rearrange("p (r d q t c) -> p r d q t c", r=2, d=2, q=4, t=17)
    TC = A.alloc(2 * 4 * 2 * 512, BF16).rearrange("p (d q r n) -> p d q r n", d=2, q=4, r=2)
    Bp = A.alloc(2 * 8 * 2 * 128, BF16).rearrange("p (d g r n) -> p d g r n", d=2, g=8, r=2)
    FIRt = A.alloc(2 * 16 * 128, BF16).rearrange("p (d t n) -> p d t n", d=2, t=16)
    Sf = A.alloc(2 * 4 * 2 * 514, BF16).rearrange("p (d q r n) -> p d q r n", d=2, q=4, r=2)
    BTX, BWf, BWb, BTC, BBp, BFIR, BEp = [Buf() for _ in range(7)]
    BSf = [[Buf() for _ in range(4)] for _ in range(2)]
    MEMSET("pool", TC, 0.0, [BTC])
    MEMSET("pool", Bp, 0.0, [BBp])
    MEMSET("pool", Sf, 0.0, [b for r in BSf for b in r])
    XsAll = A.alloc(4096, F32)
    Xs = [XsAll[:, q * 1024:(q + 1) * 1024].rearrange("p (r n) -> p r n", r=2) for q in range(4)]
    BXs = [Buf() for _ in range(4)]
    scr0 = A.off
    EpA = A.alloc(16 * 128, F32).rearrange("p (k r c) -> p k r c", k=16, r=2)
    VA = A.alloc(16 * 128, F32).rearrange("p (k r c) -> p k r c", k=16, r=2)
    tA = A.alloc(16 * 64, F32)
    Wf = A.alloc(2 * 4 * 17 * 16, F32).rearrange("p (r q t c) -> p r q t c", r=2, q=4, t=17)
    lpw = A.alloc(3 * 128, F32).rearrange("p (m r c) -> p m r c", m=3, r=2)
    tW = EpA.rearrange("p k r c -> p (k r c)")[:, 0:4 * 17 * 16]
    tW2 = VA.rearrange("p k r c -> p (k r c)")[:, 0:4 * 17 * 16]
    scr1 = A.off
    A.off = scr0
    YiB = [[A.alloc(16 * 128, BF16).rearrange("p (k n) -> p k n", k=16) for _ in range(2)],
           [XsAll[:, d * 1024:(d + 1) * 1024].bitcast(BF16).rearrange("p (k n) -> p k n", k=16) for d in range(2)]]
    BYiB = [[Buf(), Buf()], [Buf(), Buf()]]
    YtB = [A.alloc(2048, F32), XsAll[:, 2048:4096]]
    BYtB = [Buf(), Buf()]
    NGB = 3
    ytm = [A.alloc(512, F32) for _ in range(NGB)]
    gt1 = [A.alloc(512, F32) for _ in range(NGB)]
    sgo = [A.alloc(512, BF16) for _ in range(NGB)]
    Bytm = [Buf() for _ in range(NGB)]
    A.off = max(A.off, scr1)
    lamA4 = lamA.rearrange("p r (d f c) -> p r d f c", d=2, f=4)
    BbA4 = BbA.rearrange("p r (d f c) -> p r d f c", d=2, f=4)

    def cmac(eng, dst, src, l, k, col, R, W):
        a, b, nb = SC[:, l, k, 0, col:col + 1], SC[:, l, k, 1, col:col + 1], SC[:, l, k, 2, col:col + 1]
        STT(eng, dst[:, 0, :], src[:, 0, :], a, dst[:, 0, :], ALU.mult, ALU.add, R, W)
        STT(eng, dst[:, 0, :], src[:, 1, :], nb, dst[:, 0, :], ALU.mult, ALU.add, R, W)
        STT(eng, dst[:, 1, :], src[:, 0, :], b, dst[:, 1, :], ALU.mult, ALU.add, R, W)
        STT(eng, dst[:, 1, :], src[:, 1, :], a, dst[:, 1, :], ALU.mult, ALU.add, R, W)

    def scan(eng, X, n, l, rev, col, R, W):
        nlev = n.bit_length() - 1
        for k in range(nlev):
            st, h = 2 << k, 1 << k
            if not rev:
                cmac(eng, X[:, :, st - 1::st], X[:, :, h - 1::st], 0, k, col, R, W)
            else:
                cmac(eng, X[:, :, 0::st], X[:, :, h::st], 0, k, col, R, W)
        for k in range(nlev - 2, -1, -1):
            st, h = 2 << k, 1 << k
            if not rev:
                cmac(eng, X[:, :, st - 1 + h::st], X[:, :, st - 1:n - st:st], 0, k, col, R, W)
            else:
                cmac(eng, X[:, :, h:n - st:st], X[:, :, st::st], 0, k, col, R, W)

    pdn = {"n": 0, "pin": 0, "pt": 0, "pf": 0, "pg": 0}
    for f in range(4):
        uf, Buf_f = uTf[0], BuT[0]
        DMA("sp", uf, uT_dv[:, f, :], [Bdr_u], Buf_f, 1)
        ufv = uf.rearrange("p (j t) -> p t j", t=16)
        CP("act", uds[:, 0:8, :], ufv[:, 0:8, :], Buf_f, [Buds])
        CP("dve", uds[:, 8:16, :], ufv[:, 8:16, :], Buf_f, [Buds])
        yfir = uf.rearrange("p (b n) -> p b n", b=16)

        def fir_block(blk):
            pf = 6 + pdn["pf"] % 2
            pdn["pf"] += 1
            ub = uds[:, :, 32 * blk:32 * blk + 32]
            RB = [BFIR, Buds]
            MM(ps[pf][:, :], FIRt[:, 0, 0, :], ub, True, False, RB, [PB_[pf]])
            for tau in range(1, 16):
                MM(ps[pf][:, tau * 32:512], FIRt[:, 0, tau, :], ub[:, 0:16 - tau, :], False, False, RB, [PB_[pf]])
            for tau in range(16):
                MM(ps[pf][:, 0:(16 - tau) * 32], FIRt[:, 1, tau, :], ub[:, tau:16, :], False, tau == 15, RB,
                   [PB_[pf]])
            CP("act", yfir[:, blk, :].rearrange("p (jl s) -> p s jl", s=16),
               ps[pf][:, :].rearrange("p (s jl) -> p s jl", s=16), [PB_[pf]], [Buf_f[blk]])
        for d in range(2):
            Cr = bc(CBr[:, d, 4 * f:4 * f + 4, :].unsqueeze(2), [128, 4, 17, 16])
            Ci = bc(CBi[:, d, 4 * f:4 * f + 4, :].unsqueeze(2), [128, 4, 17, 16])
            pr = bc(PW[:, :, 0, d * 16 + 4 * f:d * 16 + 4 * f + 4].rearrange("p t q -> p q t").unsqueeze(3),
                    [128, 4, 17, 16])
            pi_ = bc(PW[:, :, 1, d * 16 + 4 * f:d * 16 + 4 * f + 4].rearrange("p t q -> p q t").unsqueeze(3),
                     [128, 4, 17, 16])
            t1v = tW.rearrange("p (q t c) -> p q t c", q=4, t=17)
            t2v = tW2.rearrange("p (q t c) -> p q t c", q=4, t=17)
            cmul("dve", Wf[:, 0], Wf[:, 1], Cr, Ci, pr, pi_, t1v, t2v, [BPB, Bglob, BWf], [BWf])
            for ri in range(2):
                CP("act", Wb[:, ri, d].rearrange("p q t c -> p (q t c)"), Wf[:, ri].rearrange("p q t c -> p (q t c)"),
                   [BWf], [BWb])
            for g in range(2):
                for ri in range(2):
                    o = TC[64 * g:64 * g + 64, d, :, ri, :].rearrange("p q (k g c) -> p q k g c", k=16, g=2)[:, :, :, g, :]
                    i_ = Wf[64 * g:64 * g + 64, ri, :, 1:17, :]
                    ACTF(o, i_, AF.Copy, [BWf], [BTC], scale=1.0 if ri == 0 else -1.0)
        for g8 in range(8):
            g, qq = g8 % 2, g8 // 2
            for d in range(2):
                for ri in range(2):
                    ACTF(Bp[64 * g:64 * g + 64, d, g8, ri, g8 * 16:(g8 + 1) * 16],
                         BbB[64 * g:64 * g + 64, ri, d, 4 * f + qq, :], AF.Copy, [Bglob], [BBp],
                         scale=1.0 if ri == 0 else -1.0)
        for d in range(2):
            for bk in range(4):
                for g8 in range(8):
                    for ri in range(2):
                        MM(ps[bk][:, g8 * 64:(g8 + 1) * 64], Bp[:, d, g8, ri, :],
                           Wb[:, ri, d, g8 // 2, 4 * bk:4 * bk + 4, :],
                           g8 == 0 and ri == 0, g8 == 7 and ri == 1, [BBp, BWb], [PB_[bk]])
                ov = ps[bk][:, :].rearrange("p (g t c) -> p t g c", g=8, t=4)
                CP(alt(), FIRt[:, d, 4 * bk:4 * bk + 4, :].rearrange("p t (g c) -> p t g c", g=8), ov,
                   [PB_[bk]], [BFIR])
                if d == 0 and bk == 0:
                    STT("dve", FIRt[:, 0, 0, :].rearrange("p (g c) -> p g c", g=8),
                        identf.rearrange("p (g c) -> p g c", g=8), DAc[:, f:f + 1],
                        ps[0][:, :].rearrange("p (g t c) -> p g t c", g=8, t=4)[:, :, 0, :], ALU.mult, ALU.add,
                        [PB_[0], Bcst, Bglob], [BFIR])
        def tx_gen(d):
            TX, TX3, BTX = TXs[d], TX3s[d], BTXs[d]
            RA, WA = [Bglob, BEp], [BEp]
            lr_ = lamA4[:, 0, d, f, :]
            li_ = lamA4[:, 1, d, f, :]
            MEMSET("dve", EpA[:, 0, 0, :], 1.0, WA)
            MEMSET("dve", EpA[:, 0, 1, :], 0.0, WA)
            CP("dve", EpA[:, 1, 0, :], lr_, RA, WA)
            CP("dve", EpA[:, 1, 1, :], li_, RA, WA)
            pr_, pi_ = lr_, li_
            for lv, m in enumerate((2, 4, 8)):
                cmul("dve", lpw[:, lv, 0, :], lpw[:, lv, 1, :], pr_, pi_, pr_, pi_, tA[:, 0:64], tA[:, 64:128], RA, WA)
                pr_, pi_ = lpw[:, lv, 0, :], lpw[:, lv, 1, :]
                t1 = tA[:, 0:m * 64].rearrange("p (k c) -> p k c", k=m)
                t2 = tA[:, 512:512 + m * 64].rearrange("p (k c) -> p k c", k=m)
                cmul("dve", EpA[:, m:2 * m, 0, :], EpA[:, m:2 * m, 1, :], EpA[:, 0:m, 0, :], EpA[:, 0:m, 1, :],
                     bc(pr_.unsqueeze(1), [128, m, 64]), bc(pi_.unsqueeze(1), [128, m, 64]), t1, t2, RA, WA)
            Br_ = bc(BbA4[:, 0, d, f, :].unsqueeze(1), [128, 16, 64])
            Bi_ = bc(BbA4[:, 1, d, f, :].unsqueeze(1), [128, 16, 64])
            tAv = tA.rearrange("p (k c) -> p k c", k=16)
            TT("dve", VA[:, :, 0, :], EpA[:, :, 0, :], Br_, ALU.mult, RA, WA)
            TT("dve", tAv, EpA[:, :, 1, :], Bi_, ALU.mult, RA, WA)
            TT("dve", VA[:, :, 0, :], VA[:, :, 0, :], tAv, ALU.subtract, RA, WA)
            TT("dve", VA[:, :, 1, :], EpA[:, :, 0, :], Bi_, ALU.mult, RA, WA)
            TT("dve", tAv, EpA[:, :, 1, :], Br_, ALU.mult, RA, WA)
            TT("dve", VA[:, :, 1, :], VA[:, :, 1, :], tAv, ALU.add, RA, WA)
            for ri in range(2):
                for g in range(2):
                    TS("dve", TX[:, :, ri, g, :], VA[:, :, ri, :], maskcol[:, g:g + 1], ALU.mult, [BEp, Bcst], [BTX])
                    ACTF(TX3[64:128, :, ri, g, :], VA[64:128, :, ri, :], AF.Identity, [BEp, Bcst], [BTX],
                         scale=mask3[64:128, g:g + 1])

        def x_mm(d):
            TX, TX3, BTX = TXs[d], TX3s[d], BTXs[d]
            for qq in range(4):
                for ri in range(2):
                    bank = 2 * qq + ri
                    for s_ in range(16):
                        k = 15 - s_ if d == 0 else s_
                        if qq < 3:
                            lt = TX[32 * qq:32 * qq + 32, k, ri, :, :].rearrange("p g c -> p (g c)")
                            rh = uds[32 * qq:32 * qq + 32, s_, :]
                        else:
                            lt = TX3[64:128, k, ri, :, :].rearrange("p g c -> p (g c)")
                            rh = uds[64:128, s_, :]
                        MM(ps[bank][:, :], lt, rh, s_ == 0, s_ == 15, [BTX, Buds], [PB_[bank]])

        def x_evac(d):
            for qq in range(4):
                for ri in range(2):
                    bank = 2 * qq + ri
                    CP(alt(), Xs[qq][:, ri, :], ps[bank][:, :], [PB_[bank]], [BXs[qq]])

        def scans(d):
            lanes = [[] for _ in range(4)]
            for qq in range(4):
                S.lane = lanes[qq]
                col = d * 16 + 4 * f + qq
                scan("dve", Xs[qq], 512, 0, d == 1, col, [BXs[qq], Bglob], [BXs[qq]])
                CP("act", Sf[:, d, qq, :, 1:513], Xs[qq], [BXs[qq]], [BSf[d][qq]])
            S.lane = None
            S.run_lanes(lanes)

        for blk in range(8):
            fir_block(blk)
        tx_gen(0)
        x_mm(0)
        x_evac(0)
        tx_gen(1)
        x_mm(1)
        scans(0)
        x_evac(1)
        for blk in range(8, 16):
            fir_block(blk)
        scans(1)
        S.barrier()
        for ct in range(4):
            Yi, BYi, Yt, BYt = YiB[ct % 2], BYiB[ct % 2], YtB[ct % 2], BYtB[ct % 2]
            for d in range(2):
                off = 128 * ct if d == 0 else 128 * ct + 2
                for qq in range(4):
                    pin = 4 + pdn["pin"] % 2
                    pdn["pin"] += 1
                    for ri in range(2):
                        MM(ps[pin][:, :], Sf[:, d, qq, ri, off:off + 128], TC[:, d, qq, ri, :], ri == 0, ri == 1,
                           [BSf[d][qq], BTC], [PB_[pin]])
                    CP(alt(), Yi[d][:, :, 32 * qq:32 * qq + 32], ps[pin][:, :].rearrange("p (k n) -> p k n", k=16),
                       [PB_[pin]], [BYi[d]])
            for tg in range(4):
                pt = 6 + pdn["pt"] % 2
                pdn["pt"] += 1
                for t4 in range(4):
                    tau = 4 * tg + t4
                    MM(ps[pt][:, t4 * 128:(t4 + 1) * 128], Yi[0][:, tau, :], identb, True, False,
                       [BYi[0], Bcstb], [PB_[pt]])
                    MM(ps[pt][:, t4 * 128:(t4 + 1) * 128], Yi[1][:, 15 - tau, :], identb, False, True,
                       [BYi[1], Bcstb], [PB_[pt]])
                CP(alt(), Yt.rearrange("p (j t) -> p t j", t=16)[:, 4 * tg:4 * tg + 4, :],
                   ps[pt][:, :].rearrange("p (t j) -> p t j", t=4), [PB_[pt]], [BYt])
            def gelu_block(tb, pf):
                blk = 4 * ct + tb
                y, g1, so, By = ytm[pf], gt1[pf], sgo[pf], Bytm[pf]
                TT("dve", y, yfir[:, blk, :], Yt[:, tb * 512:(tb + 1) * 512], ALU.add, [Buf_f[blk], BYt], [By])
                ACTF(g1, y, AF.Square, [By], [By])
                TS("dve", g1, g1, 0.044715, ALU.mult, [By], [By], s2=1.0, op1=ALU.add)
                TT("dve", g1, g1, y, ALU.mult, [By], [By])
                ACTF(g1, g1, AF.Sigmoid, [By], [By], scale=1.5957691216057308)
                TT("dve", so, y, g1, ALU.mult, [By], [By])
                DMA("sp", sg_dv[:, f, blk * 512:(blk + 1) * 512], so, [By], [Bdr_sg], 2)

            lanes = [[] for _ in range(NGB)]
            for tb in range(NGB):
                S.lane = lanes[tb]
                gelu_block(tb, tb)
            S.lane = None
            S.run_lanes(lanes)
            for tb in range(NGB, 4):
                gelu_block(tb, tb - NGB)
        S.barrier()
    A.off = persist

    if stop == 2:
        do_tap(sg_d, Bdr_sg)
        return finish()

    AX = mybir.AxisListType.X
    lnt = A.alloc(4 * DM, F32).rearrange("p (i n) -> p i n", i=4)
    Bln = Buf("ln")
    for i in range(4):
        DMA("sp", lnt[:, i, :], lnp[i].partition_broadcast(128), [], [Bln], 1)
    epsc = A.alloc(1, F32)
    MEMSET("dve", epsc, EPS, [Bln])
    persist2 = A.off
    wglu_b = A.alloc(4 * 512, BF16).rearrange("p (k n) -> p k n", k=4)
    bglu = A.alloc(4, F32)
    wout_b = A.alloc(8 * DM, BF16).rearrange("p (k n) -> p k n", k=8)
    wr_f = A.alloc(8 * 72, F32).rearrange("p (k n) -> p k n", k=8)
    brb = A.alloc(72, F32)
    Bw3 = Buf("w3")
    DMA("pool", wglu_b, w_glu.rearrange("(k p) n -> p k n", p=128), [], [Bw3], 0)
    for kc in range(8):
        DMA("pool", wout_b[:, kc, :], w_out.rearrange("(k p) n -> p k n", p=128)[:, kc, :], [], [Bw3], 0)
    DMA("sp", bglu, b_glu, [], [Bw3], 1)
    DMA("sp", wr_f, w_r.rearrange("(k p) n -> p k n", p=128), [], [Bw3], 1)
    DMA("sp", brb, b_r[0].partition_broadcast(128), [], [Bw3], 1)
    sgb = [A.alloc(4 * 512, BF16).rearrange("p (k n) -> p k n", k=4) for _ in range(2)]
    atb = [A.alloc(4 * 512, BF16).rearrange("p (k n) -> p k n", k=4) for _ in range(2)]
    sTg = [A.alloc(4 * 512, BF16).rearrange("p (k n) -> p k n", k=4) for _ in range(2)]
    gtb = [A.alloc(512, BF16) for _ in range(2)]
    Bsgb, Batb, BsTg, Bgtb = ([Buf(), Buf()] for _ in range(4))
    NL = 4
    xt = [A.alloc(DM, F32) for _ in range(NL)]
    rt = [A.alloc(DM, F32) for _ in range(NL)]
    hbt2 = [[A.alloc(DM, BF16) for _ in range(NL)] for _ in range(2)]
    lnscr = [A.alloc(16, F32) for _ in range(NL)]
    Blns = [Buf() for _ in range(NL)]
    hTs = [A.alloc(8 * 128, F32).rearrange("p (k n) -> p k n", k=8) for _ in range(NL)]
    sms = [A.alloc(512, F32) for _ in range(NL)]
    twbs = [A.alloc(64, BF16) for _ in range(NL)]
    Bxt, Brt, BhTs, Bsms = ([Buf() for _ in range(NL)] for _ in range(4))
    Bhbt2 = [[Buf() for _ in range(NL)] for _ in range(2)]
    Bhbuf, Bxbuf, Bobuf = Buf("hbuf"), Buf("xbuf"), Buf("obuf")

    def layer_norm(r, gi, R, W, scr, Bs):
        st, mv, sd, nb = scr[:, 0:12], scr[:, 12:14], scr[:, 14:15], scr[:, 15:16]
        S.op("dve", lambda e: e.bn_stats(out=st[:, 0:6], in_=r[:, 0:512]), R + [Bs], [Bs])
        S.op("dve", lambda e: e.bn_stats(out=st[:, 6:12], in_=r[:, 512:1024]), R + [Bs], [Bs])
        S.op("dve", lambda e: e.bn_aggr(out=mv, in_=st), [Bs], [Bs])
        ACTF(sd, mv[:, 1:2], AF.Sqrt, [Bs, Bln], [Bs], bias=epsc[:, 0:1])
        S.op("dve", lambda e: e.reciprocal(out=sd, in_=sd), [Bs], [Bs])
        STT("dve", nb, mv[:, 0:1], -1.0, sd, ALU.mult, ALU.mult, [Bs], [Bs])
        ACTF(r, r, AF.Identity, R + [Bs], W, scale=sd, bias=nb)
        TT("dve", r, r, lnt[:, gi, :], ALU.mult, R + [Bln], W)
        TT("dve", r, r, lnt[:, gi + 1, :], ALU.add, R + [Bln], W)

    def p3_f1a(b, i):
        bi, tt = b % 2, 4 * b + i
        for half in range(2):
            bank = half
            for kc in range(8):
                lt = atb[bi][:, kc, i * 128:(i + 1) * 128] if kc < 4 else sTg[bi][:, kc - 4, i * 128:(i + 1) * 128]
                MM(ps[bank][:, :], lt, wout_b[:, kc, half * 512:(half + 1) * 512], kc == 0, kc == 7,
                   [Batb[bi], BsTg[bi], Bw3], [PB_[bank]])
            STT("dve", rt[i][:, half * 512:(half + 1) * 512], xt[i][:, half * 512:(half + 1) * 512], ALPHA,
                ps[bank][:, :], ALU.mult, ALU.add, [Bxt[i], PB_[bank]], [Brt[i]])

    def p3_f1b(b, i):
        tt = 4 * b + i
        h = rt[i]
        layer_norm(h, 0, [Brt[i]], [Brt[i]], lnscr[i], Blns[i])
        DMA("sp", hbuf[tt * 128:(tt + 1) * 128, :], h, [Brt[i]], [Bhbuf], 2)
        CP("act", hbt2[b % 2][i], h, [Brt[i]], [Bhbt2[b % 2][i]])

    def p3_f2(b, i):
        h, hT, BhT, mb = rt[i], hTs[i], BhTs[i], 4 + i
        for kc in range(8):
            bank = 2 + kc // 4
            TR(ps[bank][:, (kc % 4) * 128:(kc % 4 + 1) * 128], h[:, kc * 128:(kc + 1) * 128], identf,
               [Brt[i], Bcst], [PB_[bank]])
        CP("act", hT[:, 0:4, :], ps[2][:, :].rearrange("p (k n) -> p k n", k=4), [PB_[2]], [BhT])
        CP("act", hT[:, 4:8, :], ps[3][:, :].rearrange("p (k n) -> p k n", k=4), [PB_[3]], [BhT])
        for kc in range(8):
            MM(ps[mb][:, 0:72], hT[:, kc, :], wr_f[:, kc, :], kc == 0, kc == 7, [BhT, Bw3], [PB_[mb]])

    def p3_router(b, i):
        tt = 4 * b + i
        sm, Bsm, twb, mb = sms[i], Bsms[i], twbs[i], 4 + i
        lg = sm[:, 0:72]
        el = sm[:, 8:72].rearrange("p (g e) -> p g e", g=8)
        gmax, ngm, gsum, gval = sm[:, 72:73], sm[:, 73:74], sm[:, 74:75], sm[:, 75:76]
        ge, ohg = sm[:, 80:88], sm[:, 88:96]
        prod = sm[:, 96:160].rearrange("p (g e) -> p g e", g=8)
        ein, oh1, e2, oh2 = sm[:, 160:168], sm[:, 168:176], sm[:, 176:184], sm[:, 184:192]
        m1, m2, dm, w1 = sm[:, 192:193], sm[:, 193:194], sm[:, 194:195], sm[:, 195:196]
        o64 = [sm[:, 200:264], sm[:, 264:328]]
        R_, W_ = [Bsm], [Bsm]
        TT("dve", lg, ps[mb][:, 0:72], brb, ALU.add, [PB_[mb], Bw3, Bsm], W_)
        S.op("dve", lambda e: e.reduce_max(out=gmax, in_=lg[:, 0:8], axis=AX), R_, W_)
        TS("dve", ngm, gmax, -1.0, ALU.mult, R_, W_)
        ACTF(ge, lg[:, 0:8], AF.Exp, R_, W_, bias=ngm)
        S.op("dve", lambda e: e.reduce_sum(out=gsum, in_=ge, axis=AX), R_, W_)
        S.op("dve", lambda e: e.reciprocal(out=gval, in_=gsum), R_, W_)
        TS("dve", ohg, lg[:, 0:8], gmax, ALU.is_equal, R_, W_)
        TT("dve", prod, el, bc(ohg.unsqueeze(2), [128, 8, 8]), ALU.mult, R_, W_)
        S.op("dve", lambda e: e.tensor_reduce(out=ein, in_=prod.rearrange("p g e -> p e g"), axis=AX,
                                              op=ALU.add), R_, W_)
        S.op("dve", lambda e: e.reduce_max(out=m1, in_=ein, axis=AX), R_, W_)
        TS("dve", oh1, ein, m1, ALU.is_equal, R_, W_)
        STT("dve", e2, oh1, -1e30, ein, ALU.mult, ALU.add, R_, W_)
        S.op("dve", lambda e: e.reduce_max(out=m2, in_=e2, axis=AX), R_, W_)
        TS("dve", oh2, e2, m2, ALU.is_equal, R_, W_)
        TT("dve", dm, m2, m1, ALU.subtract, R_, W_)
        ACTF(dm, dm, AF.Exp, R_, W_)
        TS("dve", dm, dm, 1.0, ALU.add, R_, W_)
        S.op("dve", lambda e: e.reciprocal(out=w1, in_=dm), R_, W_)
        TT("dve", gates_all[:, tt, 0:1], gval, w1, ALU.mult, R_, [Bgates])
        TT("dve", gates_all[:, tt, 1:2], gval, gates_all[:, tt, 0:1], ALU.subtract, R_ + [Bgates], [Bgates])
        for k, ohk in enumerate((oh1, oh2)):
            TT("dve", o64[k].rearrange("p (g e) -> p g e", g=8), bc(ohg.unsqueeze(2), [128, 8, 8]),
               bc(ohk.unsqueeze(1), [128, 8, 8]), ALU.mult, R_, W_)
        TT("dve", twb, o64[0], o64[1], ALU.add, R_, W_)

    def p3_dispatch(b, i):
        tt = 4 * b + i
        sm, Bsm, twb, mb = sms[i], Bsms[i], twbs[i], 4 + i
        o64 = [sm[:, 200:264], sm[:, 264:328]]
        posC, tmp64, dkf = sm[:, 328:392], sm[:, 420:484], sm[:, 392:394]
        R_, W_ = [Bsm], [Bsm]
        MM(ps[mb][:, 128:192], ltrib, twb, True, True, [Bsm, Bcstb], [PB_[mb]])
        MM(ps[mb][:, 192:256], onesb, twb, True, True, [Bsm, Bcstb], [PB_[mb]])
        TT("dve", posC, ps[mb][:, 128:192], runc, ALU.add, [PB_[mb], Brun, Bsm], W_)
        TT("dve", posC, posC, eC, ALU.add, R_ + [Bcst], W_)
        TT("dve", runc, runc, ps[mb][:, 192:256], ALU.add, [PB_[mb], Brun], [Brun])
        for k in range(2):
            TT("dve", tmp64, o64[k], posC, ALU.mult, R_, W_)
            S.op("dve", lambda e, k=k: e.reduce_sum(out=dkf[:, k:k + 1], in_=tmp64, axis=AX), R_, W_)
            CP("dve", dest_all[:, tt, k:k + 1], dkf[:, k:k + 1], R_, [Bdest])
            S.op("pool", lambda e, k=k: e.indirect_dma_start(
                out=xbuf, out_offset=bass.IndirectOffsetOnAxis(ap=dest_all[:, tt, k:k + 1], axis=0),
                in_=hbt2[b % 2][i], in_offset=None), [Bhbt2[b % 2][i], Bdest], [Bxbuf], dma=3)

    def p3_loads(b):
        bi = b % 2
        DMA("sp", sgb[bi], sg_dv[:, :, b * 512:(b + 1) * 512], [Bdr_sg], [Bsgb[bi]], 1)
        DMA("sp", atb[bi], attT_dv[:, :, b * 512:(b + 1) * 512], [Bdr_att], [Batb[bi]], 1)

    def p3_glu(b):
        bi = b % 2
        for oc in range(4):
            bank = oc % 2
            for kc in range(4):
                MM(ps[bank][:, :], wglu_b[:, kc, oc * 128:(oc + 1) * 128], sgb[bi][:, kc, :], kc == 0, kc == 3,
                   [Bw3, Bsgb[bi]], [PB_[bank]])
            ACTF(gtb[bank], ps[bank][:, :], AF.Sigmoid, [PB_[bank], Bw3], [Bgtb[bank]], bias=bglu[:, oc:oc + 1])
            TT("dve", sTg[bi][:, oc, :], sgb[bi][:, oc, :], gtb[bank], ALU.mult, [Bsgb[bi], Bgtb[bank]], [BsTg[bi]])

    def p3_xloads(b):
        for i in range(NL):
            tt_ = 4 * b + i
            DMA("sp", xt[i], x[tt_ * 128:(tt_ + 1) * 128, :], [], [Bxt[i]], 1)

    p3_loads(0)
    p3_xloads(0)
    p3_glu(0)
    for i in range(NL):
        p3_f1a(0, i)
    for b in range(16):
        if b < 15:
            p3_loads(b + 1)
            p3_xloads(b + 1)
        lanes = [[] for _ in range(NL)]
        for i in range(NL):
            S.lane = lanes[i]
            p3_f1b(b, i)
        S.lane = None
        S.run_lanes(lanes)
        if b < 15:
            p3_glu(b + 1)
        for i in range(NL):
            if b > 0:
                p3_dispatch(b - 1, i)
            p3_f2(b, i)
        lanes = [[] for _ in range(NL + 1)]
        for i in range(NL):
            S.lane = lanes[i]
            p3_router(b, i)
        if b < 15:
            S.lane = lanes[NL]
            for i in range(NL):
                p3_f1a(b + 1, i)
        S.lane = None
        S.run_lanes(lanes)
    for i in range(NL):
        p3_dispatch(15, i)
    S.barrier()
    A.off = persist2

    if stop == 3:
        do_tap(hbuf, Bhbuf)
        return finish()

    NWB = 3
    wg = [A.alloc(8 * 512, BF16).rearrange("p (k n) -> p k n", k=8) for _ in range(NWB)]
    wu = [A.alloc(8 * 512, BF16).rearrange("p (k n) -> p k n", k=8) for _ in range(NWB)]
    wd = [A.alloc(4 * DM, BF16).rearrange("p (k n) -> p k n", k=4) for _ in range(NWB)]
    xgt = [A.alloc(3 * DM, BF16).rearrange("p (s n) -> p s n", s=3) for _ in range(NWB)]
    xgT = [A.alloc(8 * CAP, BF16).rearrange("p (k n) -> p k n", k=8) for _ in range(NWB)]
    hidT = [A.alloc(4 * CAP, BF16).rearrange("p (k n) -> p k n", k=4) for _ in range(NWB)]
    sgt = [A.alloc(CAP, BF16) for _ in range(2)]
    obt = [A.alloc(DM, BF16) for _ in range(3)]
    Bwg, Bwu, Bwd, Bxgt, BxgT, BhidT, Bsgt, Bobt = ([Buf() for _ in range(3)] for _ in range(8))
    c4 = {"t": 0, "g": 0, "d": 0, "o": 0}

    def p4_load(e_):
        bi = e_ % NWB
        DMA("pool", wg[bi], w_gate[e_].rearrange("(k p) n -> p k n", p=128), [], [Bwg[bi]], 5)
        DMA("pool", wu[bi], w_up[e_].rearrange("(k p) n -> p k n", p=128), [], [Bwu[bi]], 5)
        DMA("pool", wd[bi], w_down[e_].rearrange("(k p) n -> p k n", p=128), [], [Bwd[bi]], 5)
        DMA("sp", xgt[bi], xbuf[e_ * CAP:(e_ + 1) * CAP, :].rearrange("(s p) n -> p s n", p=128),
            [Bxbuf], [Bxgt[bi]], 6)

    def p4_A(e_):
        bi = e_ % NWB
        for s_ in range(3):
            bank = c4["t"] % 2
            c4["t"] += 1
            pb16 = ps[bank][:, :].bitcast(BF16)
            for kc in range(8):
                TR(pb16[:, kc * 128:(kc + 1) * 128], xgt[bi][:, s_, kc * 128:(kc + 1) * 128], identb,
                   [Bxgt[bi], Bcstb], [PB_[bank]])
            CP(alt(), xgT[bi][:, :, s_ * 128:(s_ + 1) * 128], pb16.rearrange("p (k n) -> p k n", k=8),
               [PB_[bank]], [BxgT[bi]])

    def p4_B(e_):
        bi = e_ % NWB
        for hc in range(4):
            gi = c4["g"] % 2
            c4["g"] += 1
            bg, bu = 2 + gi, 4 + gi
            for kc in range(8):
                MM(ps[bg][:, 0:CAP], wg[bi][:, kc, hc * 128:(hc + 1) * 128], xgT[bi][:, kc, :], kc == 0, kc == 7,
                   [Bwg[bi], BxgT[bi]], [PB_[bg]])
            for kc in range(8):
                MM(ps[bu][:, 0:CAP], wu[bi][:, kc, hc * 128:(hc + 1) * 128], xgT[bi][:, kc, :], kc == 0, kc == 7,
                   [Bwu[bi], BxgT[bi]], [PB_[bu]])
            ACTF(sgt[gi], ps[bg][:, 0:CAP], AF.Silu, [PB_[bg]], [Bsgt[gi]])
            TT("dve", hidT[bi][:, hc, :], sgt[gi], ps[bu][:, 0:CAP], ALU.mult, [Bsgt[gi], PB_[bu]], [BhidT[bi]])

    def p4_C(e_):
        bi = e_ % NWB
        for s_ in range(3):
            oi = c4["o"] % 3
            c4["o"] += 1
            for half in range(2):
                bank = 6 + c4["d"] % 2
                c4["d"] += 1
                for hc in range(4):
                    MM(ps[bank][:, :], hidT[bi][:, hc, s_ * 128:(s_ + 1) * 128], wd[bi][:, hc, half * 512:(half + 1) * 512],
                       hc == 0, hc == 3, [BhidT[bi], Bwd[bi]], [PB_[bank]])
                CP(alt(), obt[oi][:, half * 512:(half + 1) * 512], ps[bank][:, :], [PB_[bank]], [Bobt[oi]])
            r0 = e_ * CAP + s_ * 128
            DMA("sp", obuf[r0:r0 + 128, :], obt[oi], [Bobt[oi]], [Bobuf], 7)

    p4_load(0)
    p4_load(1)
    p4_A(0)
    p4_B(0)
    p4_A(1)
    for e_ in range(NE):
        if e_ + 2 < NE:
            p4_load(e_ + 2)
        if e_ + 1 < NE:
            p4_B(e_ + 1)
        p4_C(e_)
        if e_ + 2 < NE:
            p4_A(e_ + 2)
    S.barrier()
    A.off = persist2

    N5 = 4
    o1 = [A.alloc(DM, BF16) for _ in range(N5)]
    o2 = [A.alloc(DM, BF16) for _ in range(N5)]
    ht = [A.alloc(DM, F32) for _ in range(N5)]
    yt5 = [A.alloc(DM, F32) for _ in range(N5)]
    Bo1, Bo2, Bht, Byt5 = ([Buf() for _ in range(N5)] for _ in range(4))
    ln5 = [A.alloc(16, F32) for _ in range(N5)]
    Bln5 = [Buf() for _ in range(N5)]
    Bout = Buf("out")
    def p5_load(tt):
        ti = tt % N5
        for k, (ob_, Bo_) in enumerate(((o1[ti], Bo1[ti]), (o2[ti], Bo2[ti]))):
            S.op("pool", lambda e, k=k, ob_=ob_: e.indirect_dma_start(
                out=ob_, out_offset=None, in_=obuf,
                in_offset=bass.IndirectOffsetOnAxis(ap=dest_all[:, tt, k:k + 1], axis=0)),
                [Bobuf, Bdest], [Bo_], dma=4)
        DMA("sp", ht[ti], hbuf[tt * 128:(tt + 1) * 128, :], [Bhbuf], [Bht[ti]], 1)

    def p5_compute(tt):
        ti = tt % N5
        y5 = yt5[ti]
        S.op("act", lambda e: e.mul(out=y5, in_=ht[ti], mul=ALPHA), [Bht[ti]], [Byt5[ti]])
        STT("dve", y5, o1[ti], gates_all[:, tt, 0:1], y5, ALU.mult, ALU.add, [Bo1[ti], Bgates, Byt5[ti]], [Byt5[ti]])
        STT("dve", y5, o2[ti], gates_all[:, tt, 1:2], y5, ALU.mult, ALU.add, [Bo2[ti], Bgates, Byt5[ti]], [Byt5[ti]])
        layer_norm(y5, 2, [Byt5[ti]], [Byt5[ti]], ln5[ti], Bln5[ti])
        out_writes.append(DMA("sp", out[tt * 128:(tt + 1) * 128, :], y5, [Byt5[ti]], [Bout], 8))

    for tt in range(3):
        p5_load(tt)
    for tt in range(64):
        if tt + 3 < 64:
            p5_load(tt + 3)
        p5_compute(tt)
    return finish()


def prep_shared(inp):
    sh = {}
    sh["w_in"] = np.ascontiguousarray(inp["w_in"][0], np.float32)
    sh["w_glu"] = np.ascontiguousarray(inp["w_glu"][0], np.float32)
    sh["b_glu"] = np.ascontiguousarray(inp["b_glu"][0].reshape(4, 128).T, np.float32)
    sh["w_out"] = np.ascontiguousarray(inp["w_out"][0], np.float32)
    sh["lnp"] = np.ascontiguousarray(np.stack([inp["ln1_g"][0], inp["ln1_b"][0], inp["ln2_g"][0], inp["ln2_b"][0]]),
                                     np.float32)
    sh["w_r"] = np.ascontiguousarray(np.concatenate([inp["w_router_group"][0], inp["w_router_expert"][0]], 1),
                                     np.float32)
    sh["b_r"] = np.ascontiguousarray(np.concatenate([inp["b_router_group"][0], inp["b_router_expert"][0]])[None],
                                     np.float32)
    sh["w_gate"] = np.ascontiguousarray(inp["w_gate"][0], np.float32)
    sh["w_up"] = np.ascontiguousarray(inp["w_up"][0], np.float32)
    sh["w_down"] = np.ascontiguousarray(inp["w_down"][0], np.float32)
    sh["btab"] = build_bias_tab(np.asarray(inp["rpb"][0], np.float32))
    sh["consts"] = build_consts()
    PB, PA = build_s5_layouts(*[np.asarray(inp[k][0], np.float32) for k in
                                ("s5_a_re", "s5_a_im", "s5_log_dt", "s5_b_re", "s5_b_im", "s5_c_re", "s5_c_im",
                                 "s5_d")])
    sh["PB"], sh["PA"] = PB, PA
    return sh


def prep_core(inp, b, sh):
    m = dict(sh)
    xb = np.asarray(inp["x"][b], np.float32)
    m["x"] = np.ascontiguousarray(xb)
    m["xT"] = np.ascontiguousarray(xb.T)
    return m


def kernel(**inputs):
    sh = prep_shared(inputs)
    nc = build()
    in_maps = [prep_core(inputs, b, sh) for b in range(8)]
    res = run_bass_kernel_spmd(nc, in_maps, core_ids=list(range(8)))
    return np.stack([np.asarray(r["out"], np.float32) for r in res.results], 0)
```
